# Optimizing a Trainium2 kernel written in Bass

```python
import math
import jax, jax.numpy as jnp
from jax import lax
import numpy as np

D_MODEL = 2048
BATCH = 32
SEQ = 256
DEPTH = 4
DEC_BATCH = 8
DEC_SEQ = 2048
PAST_LEN = 512

GRID_W = 64
CHUNK_A = 128
D_A = 1024
N_A_GROUPS = 8
A_GROUP = D_A // N_A_GROUPS
N_GLA_HEADS = 4
GLA_DK = D_MODEL // 4
GLA_DV = D_MODEL // 2
GLA_DK_HEAD = GLA_DK // N_GLA_HEADS
GLA_DV_HEAD = GLA_DV // N_GLA_HEADS
GLA_RANK = 16
GLA_TAU = 16.0
GLA_CHUNK = 64
N_BRANCH = 2
D_FF = 5632
N_EXPERTS = 8
TOP_K = 2
N_EVEN = (DEPTH + 1) // 2
N_ODD = DEPTH // 2
EPS = 1e-6
IN_SIZES = (D_A, D_A, GLA_DK, GLA_DK, GLA_DV, GLA_DV, GLA_RANK, GLA_RANK, D_MODEL, D_MODEL)
N_IN = 2 * D_A + 2 * GLA_DK + 2 * GLA_DV + 2 * GLA_RANK + N_BRANCH * D_MODEL

kernel_name = 'hybrid_gmlp_gla_moe_diffusion_step'


def rmsnorm(x, g):
    x32 = x.astype(jnp.float32)
    y = x32 * lax.rsqrt(jnp.mean(x32 * x32, axis=-1, keepdims=True) + EPS)
    return (y * g).astype(x.dtype)


def grid_pos_embed(L, dtype):
    rows = L // GRID_W
    r, col = jnp.meshgrid(jnp.arange(rows, dtype=jnp.float32), jnp.arange(GRID_W, dtype=jnp.float32), indexing='ij')
    r = r.reshape(-1)
    col = col.reshape(-1)
    quarter = D_MODEL // 4
    freq = jnp.exp(-math.log(10000.0) * jnp.arange(quarter, dtype=jnp.float32) / quarter)
    ar = r[:, None] * freq
    ac = col[:, None] * freq
    emb = jnp.concatenate([jnp.sin(ar), jnp.cos(ar), jnp.sin(ac), jnp.cos(ac)], axis=-1)
    return emb.astype(dtype)


def to_heads(t, n):
    B, L, Dt = t.shape
    return t.reshape(B, L, n, Dt // n).transpose(0, 2, 1, 3)


def chunk_sgu(u, v, ln_g, ln_b, w_s, b_s):
    B, L, _ = u.shape
    n = L // CHUNK_A
    v32 = v.astype(jnp.float32)
    mu = jnp.mean(v32, axis=-1, keepdims=True)
    var = jnp.mean(jnp.square(v32 - mu), axis=-1, keepdims=True)
    vn = ((v32 - mu) * lax.rsqrt(var + EPS) * ln_g + ln_b).astype(v.dtype)
    vn = vn.reshape(B, n, CHUNK_A, N_A_GROUPS, A_GROUP)
    f = jnp.einsum('gpq,bnqgc->bnpgc', w_s, vn) + b_s.T[None, None, :, :, None]
    return u * f.reshape(B, L, D_A)


def gla_chunked(q, k, v, log_a, s0):
    B, H, L, DK = q.shape
    DV = v.shape[-1]
    C = GLA_CHUNK
    N = L // C
    q = q.reshape(B, H, N, C, DK)
    k = k.reshape(B, H, N, C, DK)
    v = v.reshape(B, H, N, C, DV)
    b = jnp.cumsum(log_a.reshape(B, H, N, C, DK), axis=3)
    b_last = b[:, :, :, -1:, :]
    qd = q * jnp.exp(b)
    kd = k * jnp.exp(-b)
    att = jnp.einsum('bhncd,bhnsd->bhncs', qd, kd)
    mask = jnp.tril(jnp.ones((C, C), dtype=bool))
    att = jnp.where(mask, att, 0.0)
    o_intra = jnp.einsum('bhncs,bhnse->bhnce', att, v)
    k_end = k * jnp.exp(b_last - b)
    dS = jnp.einsum('bhncd,bhnce->bhnde', k_end, v)
    g = jnp.exp(b_last[:, :, :, 0, :])

    def step(S, inp):
        g_n, dS_n = inp
        return g_n[..., None] * S + dS_n, S

    s_fin, s_prev = lax.scan(step, s0, (jnp.moveaxis(g, 2, 0), jnp.moveaxis(dS, 2, 0)))
    s_prev = jnp.moveaxis(s_prev, 0, 2)
    o_inter = jnp.einsum('bhncd,bhnde->bhnce', qd, s_prev)
    return (o_intra + o_inter).reshape(B, H, L, DV), s_fin


def gla_bidir(q, k, v, la_f, la_b, s0_f, s0_b):
    o_f, s_f = gla_chunked(q, k, v, la_f, s0_f)
    fl = lambda t: jnp.flip(t, axis=2)
    o_b, s_b = gla_chunked(fl(q), fl(k), fl(v), fl(la_b), s0_b)
    return o_f + fl(o_b), s_f, s_b


def mixer(h, w_in, sgu_ln_g, sgu_ln_b, w_spatial, b_spatial, gla_a2, gla_ab, gla_norm_g,
          w_branch_a, w_branch_b, w_out, s0_f, s0_b):
    dt = h.dtype
    proj = h @ w_in
    idx = np.cumsum(IN_SIZES)[:-1].tolist()
    pu, pv, pq, pk, pvg, pr, plf, plb, pga, pgb = jnp.split(proj, idx, axis=-1)
    a = chunk_sgu(jax.nn.gelu(pu), jax.nn.gelu(pv), sgu_ln_g, sgu_ln_b, w_spatial, b_spatial)
    q = to_heads(pq.astype(jnp.float32), N_GLA_HEADS) * (GLA_DK_HEAD ** -0.5)
    k = to_heads(pk.astype(jnp.float32), N_GLA_HEADS)
    v = to_heads(pvg.astype(jnp.float32), N_GLA_HEADS)
    la_f = to_heads(jax.nn.log_sigmoid((plf @ gla_a2[0] + gla_ab[0]).astype(jnp.float32)) / GLA_TAU, N_GLA_HEADS)
    la_b = to_heads(jax.nn.log_sigmoid((plb @ gla_a2[1] + gla_ab[1]).astype(jnp.float32)) / GLA_TAU, N_GLA_HEADS)
    o, s_f, s_b = gla_bidir(q, k, v, la_f, la_b, s0_f, s0_b)
    o = o * lax.rsqrt(jnp.mean(o * o, axis=-1, keepdims=True) + EPS)
    B, H, L, E = o.shape
    o = o.transpose(0, 2, 1, 3).reshape(B, L, H * E) * gla_norm_g
    o = o.astype(dt) * jax.nn.silu(pr)
    merged = jax.nn.sigmoid(pga) * (a @ w_branch_a) + jax.nn.sigmoid(pgb) * (o @ w_branch_b)
    return merged @ w_out, s_f, s_b


def swiglu(h, w1, w3, w2):
    return (jax.nn.silu(h @ w1) * (h @ w3)) @ w2


def moe(h, router_w, router_b, w1, w3, w2):
    B, L, D = h.shape
    t = h.reshape(B * L, D)
    logits = (t @ router_w).astype(jnp.float32) + router_b
    top_v, top_i = lax.top_k(logits, TOP_K)
    probs = jax.nn.softmax(top_v, axis=-1)
    combine = jnp.sum(jax.nn.one_hot(top_i, N_EXPERTS, dtype=jnp.float32) * probs[..., None], axis=1)
    combine = combine.astype(h.dtype)
    out = jnp.zeros_like(t)
    for e in range(N_EXPERTS):
        out = out + combine[:, e:e + 1] * swiglu(t, w1[e], w3[e], w2[e])
    return out.reshape(B, L, D)


def trunk(x, cond, s_init, P):
    B = x.shape[0]
    finals = []
    for l in range(DEPTH):
        mod = (jax.nn.silu(cond) @ P['w_mod'][l] + P['b_mod'][l])[:, None, :]
        sh1, sc1, g1, sh2, sc2, g2 = jnp.split(mod, 6, axis=-1)
        h = rmsnorm(x, P['norm1_g'][l]) * (1 + sc1) + sh1
        if s_init is None:
            s0_f = jnp.zeros((B, N_GLA_HEADS, GLA_DK_HEAD, GLA_DV_HEAD), jnp.float32)
            s0_b = s0_f
        else:
            s0_f = s_init[:, l, 0].astype(jnp.float32)
            s0_b = s_init[:, l, 1].astype(jnp.float32)
        y, s_f, s_b = mixer(h, P['w_in'][l], P['sgu_ln_g'][l], P['sgu_ln_b'][l], P['w_spatial'][l],
                            P['b_spatial'][l], P['gla_a2'][l], P['gla_ab'][l], P['gla_norm_g'][l],
                            P['w_branch_a'][l], P['w_branch_b'][l], P['w_out'][l], s0_f, s0_b)
        x = x + g1 * y
        h = rmsnorm(x, P['norm2_g'][l]) * (1 + sc2) + sh2
        if l % 2 == 0:
            j = l // 2
            f = swiglu(h, P['ffn_w1'][j], P['ffn_w3'][j], P['ffn_w2'][j])
        else:
            j = l // 2
            f = moe(h, P['moe_router'][j], P['moe_router_b'][j], P['moe_w1'][j], P['moe_w3'][j], P['moe_w2'][j])
        x = x + g2 * f
        if s_init is None:
            finals.append(jnp.stack([s_f, s_b], axis=1))
    states = jnp.stack(finals, axis=1) if s_init is None else None
    return rmsnorm(x, P['final_g']), states


def setup_inputs(seed: int = 0) -> dict:
    key = jax.random.key(seed)
    ks = jax.random.split(key, 32)
    f32 = jnp.float32
    nrm = lambda k, shape, s: jax.random.normal(k, shape, f32) * s
    D = D_MODEL
    return {
        'x_prompt': nrm(ks[0], (BATCH, SEQ, D), 1.0),
        'x_sample': nrm(ks[1], (DEC_BATCH, DEC_SEQ, D), 1.0),
        'state_gla': nrm(ks[2], (DEC_BATCH, DEPTH, 2, N_GLA_HEADS, GLA_DK_HEAD, GLA_DV_HEAD), 1.0),
        'c': nrm(ks[3], (DEC_BATCH, D), 1.0),
        'c_ctx': nrm(ks[4], (D,), 1.0),
        'norm1_g': 1.0 + nrm(ks[5], (DEPTH, D), 0.01),
        'norm2_g': 1.0 + nrm(ks[6], (DEPTH, D), 0.01),
        'w_mod': nrm(ks[7], (DEPTH, D, 6 * D), 0.5 * D ** -0.5),
        'b_mod': nrm(ks[8], (DEPTH, 6 * D), 0.01),
        'w_in': nrm(ks[9], (DEPTH, D, N_IN), D ** -0.5),
        'sgu_ln_g': 1.0 + nrm(ks[10], (DEPTH, D_A), 0.01),
        'sgu_ln_b': nrm(ks[11], (DEPTH, D_A), 0.01),
        'w_spatial': nrm(ks[12], (DEPTH, N_A_GROUPS, CHUNK_A, CHUNK_A), CHUNK_A ** -0.5),
        'b_spatial': 1.0 + nrm(ks[13], (DEPTH, N_A_GROUPS, CHUNK_A), 0.01),
        'gla_a2': nrm(ks[14], (DEPTH, 2, GLA_RANK, GLA_DK), GLA_RANK ** -0.5),
        'gla_ab': nrm(ks[15], (DEPTH, 2, GLA_DK), 0.01),
        'gla_norm_g': 1.0 + nrm(ks[16], (DEPTH, GLA_DV), 0.01),
        'w_branch_a': nrm(ks[17], (DEPTH, D_A, D), D_A ** -0.5),
        'w_branch_b': nrm(ks[18], (DEPTH, GLA_DV, D), GLA_DV ** -0.5),
        'w_out': nrm(ks[19], (DEPTH, D, D), D ** -0.5),
        'ffn_w1': nrm(ks[20], (N_EVEN, D, D_FF), D ** -0.5),
        'ffn_w3': nrm(ks[21], (N_EVEN, D, D_FF), D ** -0.5),
        'ffn_w2': nrm(ks[22], (N_EVEN, D_FF, D), D_FF ** -0.5),
        'moe_router': nrm(ks[23], (N_ODD, D, N_EXPERTS), D ** -0.5),
        'moe_router_b': nrm(ks[24], (N_ODD, N_EXPERTS), 0.01),
        'moe_w1': nrm(ks[25], (N_ODD, N_EXPERTS, D, D_FF), D ** -0.5),
        'moe_w3': nrm(ks[26], (N_ODD, N_EXPERTS, D, D_FF), D ** -0.5),
        'moe_w2': nrm(ks[27], (N_ODD, N_EXPERTS, D_FF, D), D_FF ** -0.5),
        'final_g': 1.0 + nrm(ks[28], (D,), 0.01),
    }


def reference(x_prompt, x_sample, state_gla, c, c_ctx, norm1_g, norm2_g, w_mod, b_mod, w_in,
              sgu_ln_g, sgu_ln_b, w_spatial, b_spatial, gla_a2, gla_ab, gla_norm_g,
              w_branch_a, w_branch_b, w_out, ffn_w1, ffn_w3, ffn_w2,
              moe_router, moe_router_b, moe_w1, moe_w3, moe_w2, final_g):
    P = dict(norm1_g=norm1_g, norm2_g=norm2_g, w_mod=w_mod, b_mod=b_mod, w_in=w_in,
             sgu_ln_g=sgu_ln_g, sgu_ln_b=sgu_ln_b, w_spatial=w_spatial, b_spatial=b_spatial,
             gla_a2=gla_a2, gla_ab=gla_ab, gla_norm_g=gla_norm_g, w_branch_a=w_branch_a,
             w_branch_b=w_branch_b, w_out=w_out, ffn_w1=ffn_w1, ffn_w3=ffn_w3, ffn_w2=ffn_w2,
             moe_router=moe_router, moe_router_b=moe_router_b, moe_w1=moe_w1, moe_w3=moe_w3,
             moe_w2=moe_w2, final_g=final_g)
    y_prompt, new_state_gla = trunk(x_prompt, c_ctx[None, :], None, P)
    xs = x_sample + grid_pos_embed(x_sample.shape[1], x_sample.dtype)[None]
    y_sample, _ = trunk(xs, c, state_gla, P)
    return (y_prompt, y_sample, new_state_gla)
```

```python
import math
import numpy as np
from contextlib import ExitStack
import concourse.bass as bass
import concourse.mybir as mybir
from concourse.bass_utils import run_bass_kernel_spmd

F32 = mybir.dt.float32
BF16 = mybir.dt.bfloat16
I32 = mybir.dt.int32
AF = mybir.ActivationFunctionType
ALU = mybir.AluOpType

D = 2048
DEPTH = 4
T = 3072
NBLK = 6
D_A = 1024
DFF = 5632
NFF = 44
NE = 8
EPS = 1e-6
N_IN = 9248
SEQS = [(0, 2048, 0)] + [(2048 + 256 * i, 256, 1) for i in range(4)]


class SemH:
    def __init__(self, h):
        self.h = h
        self.count = 0


class Buf:
    __slots__ = ("name", "last_w", "readers", "sem")

    def __init__(self, name):
        self.name = name
        self.last_w = None
        self.readers = []
        self.sem = None


class Op:
    __slots__ = ("eng", "fn", "deps", "needed", "is_dma", "tok")

    def __init__(self, eng, fn, is_dma=False):
        self.eng = eng
        self.fn = fn
        self.deps = []
        self.needed = False
        self.is_dma = is_dma
        self.tok = None


class Sched:
    ENGS = ("pe", "act", "dve", "pool", "sp")

    def __init__(self, nc, es):
        self.nc = nc
        self.es = es
        self.ops = {e: [] for e in self.ENGS}
        self.touched = {}
        self.cur_bar = None
        self.free_sems = []
        self.nsem = 0
        self.regs = {}
        self.phase_sems = []
        self.dead = False

    def buf(self, name):
        b = Buf(name)
        b.last_w = self.cur_bar
        return b

    def reg(self, *key):
        b = self.regs.get(key)
        if b is None:
            b = self.buf(str(key))
            self.regs[key] = b
        return b

    def _getsem(self):
        if self.free_sems:
            return self.free_sems.pop(0)
        self.nsem += 1
        return SemH(self.es.enter_context(self.nc.semaphore("ds%d" % self.nsem)))

    def _add(self, op, reads, writes):
        if self.dead:
            return op
        deps = []
        for b in reads:
            if b.last_w is not None:
                deps.append(b.last_w)
        for b in writes:
            deps.extend(b.readers)
            if b.last_w is not None:
                deps.append(b.last_w)
        seen = set()
        for d in deps:
            if d is op or id(d) in seen:
                continue
            seen.add(id(d))
            if d.eng == "pe" and op.eng == "pe" and not d.is_dma and not op.is_dma:
                continue
            op.deps.append(d)
            d.needed = True
        for b in reads:
            b.readers.append(op)
            self.touched[id(b)] = b
        for b in writes:
            b.readers = []
            b.last_w = op
            self.touched[id(b)] = b
        self.ops[op.eng].append(op)
        return op

    def op(self, eng, fn, reads=(), writes=()):
        return self._add(Op(eng, fn), list(reads), list(writes))

    def dma(self, eng, out, in_, sb, reads=(), writes=(), **kw):
        def fn(e):
            return e.dma_start(out=out, in_=in_, **kw)
        o = Op(eng, fn, is_dma=True)
        o.needed = True
        if self.dead:
            return o
        if sb.sem is None:
            sb.sem = self._getsem()
            self.phase_sems.append(sb)
        sb.sem.count += 16
        o.tok = (sb.sem.h, sb.sem.count)
        return self._add(o, list(reads), list(writes))

    def barrier(self):
        if self.dead:
            return None
        allb = list(self.touched.values())
        o = self.op("sp", lambda e: e.nop(), reads=[], writes=allb)
        o.needed = True
        self.cur_bar = o
        self.touched = {}
        for b in self.phase_sems:
            self.free_sems.append(b.sem)
            b.sem = None
        self.phase_sems = []
        return o

    def emit(self):
        nc = self.nc
        engmap = {"pe": "tensor", "act": "scalar", "dve": "vector", "pool": "gpsimd", "sp": "sync"}
        ROT = 30000
        for e in self.ENGS:
            cnt = 0
            sem = None
            k = 0
            for o in self.ops[e]:
                if o.is_dma:
                    continue
                if o.needed:
                    if sem is None or cnt >= ROT:
                        sem = self.es.enter_context(nc.semaphore("es_%s_%d" % (e, k)))
                        k += 1
                        cnt = 0
                    cnt += 1
                    o.tok = (sem, cnt)
        block = self.es.enter_context(nc.Block())
        sched = self

        def make(ename):
            def body(eng):
                waited = {}
                for o in sched.ops[ename]:
                    for d in o.deps:
                        s, v = d.tok
                        k = id(s)
                        if waited.get(k, 0) >= v:
                            continue
                        waited[k] = v
                        eng.wait_ge(s, v)
                    ins = o.fn(eng)
                    if o.is_dma:
                        ins.then_inc(o.tok[0], 16)
                    elif o.needed:
                        ins.then_inc(o.tok[0], 1)
            return body

        for ename in self.ENGS:
            getattr(block, engmap[ename])(make(ename))


class _Stop(Exception):
    pass


def build(n_layers=DEPTH, stop=None, dbg=False):
    nc = bass.Bass("TRN2", target_bir_lowering=False)
    es = ExitStack()
    S = Sched(nc, es)

    def stage(name):
        if stop == name and not S.dead:
            S.barrier()
            S.dead = True

    def din(name, shape):
        return nc.dram_tensor(name, list(shape), F32, kind="ExternalInput").ap()

    def dout(name, shape):
        return nc.dram_tensor(name, list(shape), F32, kind="ExternalOutput").ap()

    def dscr(name, shape, dt):
        if dbg:
            return nc.dram_tensor(name, list(shape), dt, kind="ExternalOutput").ap()
        return nc.dram_tensor(name, list(shape), dt).ap()

    xs_in = din("xs", (2048, D))
    xp_in = din("xp", (1024, D))
    st_in = din("st", (DEPTH, 2, 4, 128, 256))
    cc_in = din("cc", (2, D))
    norm1_g = din("norm1_g", (DEPTH, D))
    norm2_g = din("norm2_g", (DEPTH, D))
    w_mod = din("w_mod", (DEPTH, D, 6 * D))
    b_mod = din("b_mod", (DEPTH, 6 * D))
    w_in = din("w_in", (DEPTH, D, N_IN))
    sgu_ln_g = din("sgu_ln_g", (DEPTH, D_A))
    sgu_ln_b = din("sgu_ln_b", (DEPTH, D_A))
    w_spatial = din("w_spatial", (DEPTH, 8, 128, 128))
    b_spatial = din("b_spatial", (DEPTH, 8, 128))
    gla_a2 = din("gla_a2", (DEPTH, 2, 16, 512))
    gla_ab = din("gla_ab", (DEPTH, 2, 512))
    gla_norm_g = din("gla_norm_g", (DEPTH, 1024))
    w_branch_a = din("w_branch_a", (DEPTH, 1024, D))
    w_branch_b = din("w_branch_b", (DEPTH, 1024, D))
    w_out = din("w_out", (DEPTH, D, D))
    ffn_w1 = din("ffn_w1", (2, D, DFF))
    ffn_w3 = din("ffn_w3", (2, D, DFF))
    ffn_w2 = din("ffn_w2", (2, DFF, D))
    moe_router = din("moe_router", (2, D, NE))
    moe_router_b = din("moe_router_b", (2, NE))
    moe_w1 = din("moe_w1", (2, NE, D, DFF))
    moe_w3 = din("moe_w3", (2, NE, D, DFF))
    moe_w2 = din("moe_w2", (2, NE, DFF, D))
    final_g = din("final_g", (D,))
    ys_out = dout("ys", (2048, D))
    yp_out = dout("yp", (1024, D))
    ns_out = dout("ns", (4, DEPTH, 2, 4, 128, 256))

    xT = dscr("xT", (16, 128, T), F32)
    puT = dscr("puT", (8, 128, T), BF16)
    vn_d = dscr("vn", (T, 1024), BF16)
    qT_d = dscr("qT", (4, 128, T), F32)
    kT_d = dscr("kT", (4, 128, T), F32)
    ktm_d = dscr("ktm", (T, 512), BF16)
    vtm_d = dscr("vtm", (T, 1024), BF16)
    rtm_d = dscr("rtm", (T, 1024), BF16)
    latm_d = dscr("latm", (2, T, 512), F32)
    gaT_d = dscr("gaT", (16, 128, T), BF16)
    gbT_d = dscr("gbT", (16, 128, T), BF16)
    aT_d = dscr("aT", (8, 128, T), BF16)
    oT_d = dscr("oT", (8, 128, T), BF16)
    of_d = dscr("of", (T, 1024), F32)

    _uid = [0]

    def sbt(stack, name, shape, dt):
        _uid[0] += 1
        name = "%s_%d" % (name, _uid[0])
        t = stack.enter_context(nc.sbuf_tensor(name, list(shape), dt))
        return t, S.buf(name)

    pbk = []
    for i in range(8):
        t = es.enter_context(nc.psum_tensor("pb%d" % i, [128, 512], F32))
        pbk.append((t, S.buf("pb%d" % i)))
    bank_i = [0]

    def bank():
        r = pbk[bank_i[0] % 8]
        bank_i[0] += 1
        return r

    identb, b_identb = sbt(es, "identb", (128, 128), BF16)
    identf, b_identf = sbt(es, "identf", (128, 128), F32)
    onesb, b_onesb = sbt(es, "onesb", (128, 128), BF16)
    onesf, b_onesf = sbt(es, "onesf", (128, 128), F32)
    triS, b_triS = sbt(es, "triS", (64, 4, 64), F32)
    triM, b_triM = sbt(es, "triM", (64, 2, 4, 64), F32)
    selE, b_selE = sbt(es, "selE", (8, 8, 128), BF16)
    modT, b_modT = sbt(es, "modT", (128, DEPTH, 96, 2), F32)
    bmodT, b_bmodT = sbt(es, "bmodT", (128, DEPTH, 96), F32)
    nT, b_nT = sbt(es, "nT", (128, 2, DEPTH, 16), F32)
    fgT, b_fgT = sbt(es, "fgT", (128, 16), F32)
    gsc, b_gsc = sbt(es, "gsc", (128, DEPTH, 2, 16, 2), F32)
    scT, b_scT = sbt(es, "scT", (128, 16, 2), BF16)

    def mk_tri(dst, pattern_sign, cm, op, scale_val):
        S.op("pool", lambda e: e.memset(dst, scale_val), writes=[])
        S.op("pool", lambda e: e.affine_select(out=dst, in_=dst, pattern=[[pattern_sign, 64]], compare_op=op,
                                              fill=0.0, base=0, channel_multiplier=cm), reads=[], writes=[])

    def cst(fn, w):
        S.op("pool", fn, reads=w, writes=w)

    cst(lambda e: e.memset(identf[:], 0.0), [b_identf])
    cst(lambda e: e.affine_select(out=identf[:], in_=identf[:], pattern=[[-1, 128]], compare_op=ALU.not_equal,
                                  fill=1.0, base=0, channel_multiplier=1), [b_identf])
    S.op("dve", lambda e: e.tensor_copy(identb[:], identf[:]), reads=[b_identf], writes=[b_identb])
    cst(lambda e: e.memset(onesf[:], 1.0), [b_onesf])
    cst(lambda e: e.memset(onesb[:], 1.0), [b_onesb])
    sc16 = -1.0 / 16.0
    cst(lambda e: e.memset(triS[:], sc16), [b_triS])
    cst(lambda e: e.affine_select(out=triS[:, 0, :], in_=triS[:, 0, :], pattern=[[1, 64]], compare_op=ALU.is_ge, fill=0.0, base=0, channel_multiplier=-1), [b_triS])
    cst(lambda e: e.affine_select(out=triS[:, 1, :], in_=triS[:, 1, :], pattern=[[-1, 64]], compare_op=ALU.is_ge, fill=0.0, base=0, channel_multiplier=1), [b_triS])
    cst(lambda e: e.affine_select(out=triS[:, 2, :], in_=triS[:, 2, :], pattern=[[-1, 64]], compare_op=ALU.is_gt, fill=0.0, base=0, channel_multiplier=1), [b_triS])
    cst(lambda e: e.affine_select(out=triS[:, 3, :], in_=triS[:, 3, :], pattern=[[1, 64]], compare_op=ALU.is_gt, fill=0.0, base=0, channel_multiplier=-1), [b_triS])
    triSb, b_triSb = sbt(es, "triSb", (64, 4, 64), BF16)
    S.op("dve", lambda e: e.tensor_copy(triSb[:], triS[:]), reads=[b_triS], writes=[b_triSb])
    cst(lambda e: e.memset(triM[:], 1.0), [b_triM])
    for h in range(4):
        cst(lambda e, h=h: e.affine_select(out=triM[:, 0, h, :], in_=triM[:, 0, h, :], pattern=[[1, 64]], compare_op=ALU.is_ge, fill=0.0, base=0, channel_multiplier=-1), [b_triM])
        cst(lambda e, h=h: e.affine_select(out=triM[:, 1, h, :], in_=triM[:, 1, h, :], pattern=[[-1, 64]], compare_op=ALU.is_ge, fill=0.0, base=0, channel_multiplier=1), [b_triM])
    cst(lambda e: e.memset(selE[:], 1.0), [b_selE])
    cst(lambda e: e.affine_select(out=selE[:], in_=selE[:], pattern=[[-1, 8], [0, 128]], compare_op=ALU.is_equal, fill=0.0, base=0, channel_multiplier=1), [b_selE])

    wA = [sbt(es, "wA%d" % i, (128, 16, 256), BF16) for i in range(4)]
    wA_i = [0]

    def next_wA():
        r = wA[wA_i[0] % 4]
        wA_i[0] += 1
        return r

    wS = [sbt(es, "wS%d" % i, (128, 16, 256), F32) for i in range(2)]
    wS_i = [0]
    cast_rr = [0]

    def load_w(dst, src, bufobj, kind="a", eng=None):
        if kind == "hb":
            stg, b_stg = wS[(wS_i[0] - 1) % 2]
        else:
            stg, b_stg = wS[wS_i[0] % 2]
            wS_i[0] += 1
        if kind == "a":
            view = stg[:]
        elif kind == "lf":
            view = stg[:, :, 0:32]
        elif kind == "w2":
            view = stg[:].rearrange("p a b -> p (a b)")[:, 0:1024].rearrange("p (f n) -> p f n", n=512)
        elif kind == "ha":
            view = stg[:, 0:8, :]
        else:
            view = stg[:, 8:16, :]
        S.dma("sp", view, src, b_stg, writes=[b_stg])
        if kind == "ha":
            return
        if kind == "hb":
            view = stg[:]
        if eng is None:
            eng = ("dve", "act")[cast_rr[0] % 2]
            cast_rr[0] += 1
        if eng == "act":
            S.op("act", lambda e, dst=dst, view=view: e.activation(dst, view, AF.Copy), reads=[b_stg], writes=[bufobj])
        else:
            S.op(eng, lambda e, dst=dst, view=view: e.tensor_copy(dst, view), reads=[b_stg], writes=[bufobj])

    with nc.allow_non_contiguous_dma(reason="small one-time parameter layouts"):
        with ExitStack() as ph:
            ccT, b_ccT = sbt(ph, "ccT", (128, 16, 2), F32)
            for g_ in range(2):
                S.dma("sp", ccT[:, :, g_], cc_in[g_].rearrange("(kc p) -> p kc", p=128), b_ccT, writes=[b_ccT])
            S.op("act", lambda e: e.activation(scT[:], ccT[:], AF.Silu), reads=[b_ccT], writes=[b_scT])
            for l_ in range(DEPTH):
                S.dma("sp", bmodT[:, l_, :], b_mod[l_].rearrange("(j p) -> p j", p=128), b_bmodT, writes=[b_bmodT])
                S.dma("sp", nT[:, 0, l_, :], norm1_g[l_].rearrange("(c p) -> p c", p=128), b_nT, writes=[b_nT])
                S.dma("sp", nT[:, 1, l_, :], norm2_g[l_].rearrange("(c p) -> p c", p=128), b_nT, writes=[b_nT])
            S.dma("sp", fgT[:], final_g.rearrange("(c p) -> p c", p=128), b_fgT, writes=[b_fgT])
            for l in range(n_layers):
                pm, b_pm = bank()
                for ct in range(48):
                    wt, b_wt = next_wA()
                    load_w(wt[:], w_mod[l][:, ct * 256:(ct + 1) * 256].rearrange("(kc p) n -> p kc n", p=128), b_wt)
                    for j in range(2):
                        jj = ct * 2 + j
                        for kc in range(16):
                            S.op("pe", lambda e, wt=wt, j=j, kc=kc, jj=jj, pm=pm: e.matmul(
                                pm[:, jj * 2:jj * 2 + 2], wt[:, kc, j * 128:(j + 1) * 128], scT[:, kc, :],
                                start=(kc == 0), stop=(kc == 15)), reads=[b_wt, b_scT], writes=[b_pm])
                for g in range(2):
                    S.op("dve", lambda e, l=l, g=g, pm=pm: e.tensor_tensor(
                        out=modT[:, l, :, g], in0=pm[:, 0:192].rearrange("p (j g) -> p j g", g=2)[:, :, g],
                        in1=bmodT[:, l, :], op=ALU.add), reads=[b_pm, b_bmodT], writes=[b_modT])
                for which in range(2):
                    for g in range(2):
                        sc0 = 16 + 48 * which
                        S.op("dve", lambda e, l=l, g=g, which=which, sc0=sc0: e.scalar_tensor_tensor(
                            out=gsc[:, l, which, :, g], in0=modT[:, l, sc0:sc0 + 16, g], scalar=1.0,
                            in1=nT[:, which, l, :], op0=ALU.add, op1=ALU.mult),
                            reads=[b_modT, b_nT], writes=[b_gsc])
            if dbg:
                d_modT = nc.dram_tensor("d_modT", [128, DEPTH, 96, 2], F32, kind="ExternalOutput").ap()
                d_gsc = nc.dram_tensor("d_gsc", [128, DEPTH, 2, 16, 2], F32, kind="ExternalOutput").ap()
                S.dma("sp", d_modT, modT[:], b_modT, reads=[b_modT])
                S.dma("sp", d_gsc, gsc[:], b_gsc, reads=[b_gsc])
            S.barrier()
            stage("pro")

        with ExitStack() as ph:
            freq, b_freq = sbt(ph, "freq", (128, 512), F32)
            ii, b_ii = sbt(ph, "ii", (128, 512), I32)
            pidx, b_pidx = sbt(ph, "pidx", (128, 2), I32)
            pv2, b_pv2 = sbt(ph, "pv2", (128, 2), F32)
            rv, b_rv = sbt(ph, "rv", (128, 1), F32)
            posc, b_posc = sbt(ph, "posc", (128, 1024), F32)
            posr, b_posr = sbt(ph, "posr", (128, 1024), F32)
            uu, b_uu = sbt(ph, "uu", (128, 512), F32)
            ki, b_ki = sbt(ph, "ki", (128, 512), I32)
            kf, b_kf = sbt(ph, "kf", (128, 512), F32)
            mm, b_mm = sbt(ph, "mm", (128, 512), F32)
            negpi, b_negpi = sbt(ph, "negpi", (128, 1), F32)
            xin = [sbt(ph, "xin%d" % i, (128, D), F32) for i in range(2)]
            xtt = [sbt(ph, "xtt%d" % i, (128, 16, 128), F32) for i in range(2)]
            inv2pi = 1.0 / (2.0 * math.pi)
            S.op("pool", lambda e: e.memset(negpi[:], -math.pi), writes=[b_negpi])
            S.op("pool", lambda e: e.iota(ii[:], pattern=[[1, 512]], base=0, channel_multiplier=0), writes=[b_ii])
            S.op("pool", lambda e: e.iota(pidx[:, 0:1], pattern=[[0, 1]], base=0, channel_multiplier=1), writes=[b_pidx])
            S.op("dve", lambda e: e.tensor_single_scalar(out=pidx[:, 1:2], in_=pidx[:, 0:1], scalar=6, op=ALU.arith_shift_right), reads=[b_pidx], writes=[b_pidx])
            S.op("dve", lambda e: e.tensor_single_scalar(out=pidx[:, 0:1], in_=pidx[:, 0:1], scalar=63, op=ALU.bitwise_and), reads=[b_pidx], writes=[b_pidx])
            S.op("dve", lambda e: e.tensor_copy(pv2[:], pidx[:]), reads=[b_pidx], writes=[b_pv2])
            S.op("dve", lambda e: e.tensor_copy(freq[:], ii[:]), reads=[b_ii], writes=[b_freq])
            S.op("act", lambda e: e.activation(freq[:], freq[:], AF.Exp, scale=-math.log(10000.0) / 512.0), reads=[b_freq], writes=[b_freq])

            def sincos(dst, b_dst, vcol, b_v, off):
                S.op("dve", lambda e: e.tensor_scalar(out=uu[:], in0=freq[:], scalar1=vcol, scalar2=off, op0=ALU.mult, op1=ALU.add),
                     reads=[b_freq, b_v], writes=[b_uu])
                S.op("dve", lambda e: e.tensor_copy(ki[:], uu[:]), reads=[b_uu], writes=[b_ki])
                S.op("dve", lambda e: e.tensor_copy(kf[:], ki[:]), reads=[b_ki], writes=[b_kf])
                S.op("dve", lambda e: e.tensor_tensor(out=uu[:], in0=uu[:], in1=kf[:], op=ALU.subtract), reads=[b_uu, b_kf], writes=[b_uu])
                S.op("dve", lambda e: e.tensor_single_scalar(out=mm[:], in_=uu[:], scalar=0.0, op=ALU.is_lt), reads=[b_uu], writes=[b_mm])
                S.op("dve", lambda e: e.tensor_tensor(out=uu[:], in0=uu[:], in1=mm[:], op=ALU.add), reads=[b_uu, b_mm], writes=[b_uu])
                S.op("act", lambda e: e.activation(dst, uu[:], AF.Sin, scale=2.0 * math.pi, bias=negpi[:]), reads=[b_uu, b_negpi], writes=[b_dst])

            S.op("dve", lambda e: e.tensor_scalar(out=pv2[:], in0=pv2[:], scalar1=inv2pi, scalar2=None, op0=ALU.mult), reads=[b_pv2], writes=[b_pv2])
            sincos(posc[:, 0:512], b_posc, pv2[:, 0:1], b_pv2, 0.5)
            sincos(posc[:, 512:1024], b_posc, pv2[:, 0:1], b_pv2, 0.75)
            for i in range(24):
                xt_, b_xt = xin[i % 2]
                xo, b_xo = xtt[i % 2]
                src = xs_in[i * 128:(i + 1) * 128, :] if i < 16 else xp_in[(i - 16) * 128:(i - 15) * 128, :]
                S.dma("sp", xt_[:], src, b_xt, writes=[b_xt])
                if i < 16:
                    S.op("dve", lambda e, i=i: e.tensor_scalar(out=rv[:], in0=pv2[:, 1:2], scalar1=2.0 * i * inv2pi, scalar2=None, op0=ALU.add),
                         reads=[b_pv2], writes=[b_rv])
                    sincos(posr[:, 0:512], b_posr, rv[:, 0:1], b_rv, 0.5)
                    sincos(posr[:, 512:1024], b_posr, rv[:, 0:1], b_rv, 0.75)
                    S.op("dve", lambda e, xt_=xt_: e.tensor_tensor(out=xt_[:, 0:1024], in0=xt_[:, 0:1024], in1=posr[:], op=ALU.add), reads=[b_xt, b_posr], writes=[b_xt])
                    S.op("dve", lambda e, xt_=xt_: e.tensor_tensor(out=xt_[:, 1024:2048], in0=xt_[:, 1024:2048], in1=posc[:], op=ALU.add), reads=[b_xt, b_posc], writes=[b_xt])
                for q4 in range(4):
                    pt, b_pt = bank()
                    for j in range(4):
                        c = q4 * 4 + j
                        S.op("pe", lambda e, pt=pt, j=j, c=c, xt_=xt_: e.transpose(pt[:, j * 128:(j + 1) * 128], xt_[:, c * 128:(c + 1) * 128], identf[:]),
                             reads=[b_xt, b_identf], writes=[b_pt])
                    eng = "act" if q4 % 2 else "dve"
                    if eng == "act":
                        S.op("act", lambda e, pt=pt, q4=q4, xo=xo: e.activation(xo[:, q4 * 4:(q4 + 1) * 4, :], pt[:].rearrange("p (j t) -> p j t", t=128), AF.Copy), reads=[b_pt], writes=[b_xo])
                    else:
                        S.op("dve", lambda e, pt=pt, q4=q4, xo=xo: e.tensor_copy(xo[:, q4 * 4:(q4 + 1) * 4, :], pt[:].rearrange("p (j t) -> p j t", t=128)), reads=[b_pt], writes=[b_xo])
                S.dma("sp", xT[:, :, i * 128:(i + 1) * 128].rearrange("c p t -> p c t"), xo[:], b_xo, reads=[b_xo], writes=[S.reg("xT", i // 4)])
            S.barrier()
            stage("x")

        def norm_mod(ph_bufs, xTb, b_xTb, hT, b_hT, l, which, g, h32cb=None):
            sq, rstd, b_rstd, tmps = ph_bufs
            pss, b_pss = bank()
            for c in range(16):
                sqt, b_sq = sq[c % 2]
                S.op("act", lambda e, c=c, sqt=sqt: e.activation(sqt[:], xTb[:, c, :], AF.Square), reads=[b_xTb], writes=[b_sq])
                S.op("pe", lambda e, c=c, sqt=sqt, pss=pss: e.matmul(pss[:], onesb[:], sqt[:], start=(c == 0), stop=(c == 15)),
                     reads=[b_sq, b_onesb], writes=[b_pss])
            S.op("act", lambda e, pss=pss: e.activation(rstd[:], pss[:], AF.Sqrt, scale=1.0 / D, bias=epsb[:]), reads=[b_pss, b_epsb], writes=[b_rstd])
            S.op("dve", lambda e: e.reciprocal(rstd[:], rstd[:]), reads=[b_rstd], writes=[b_rstd])
            sh0 = 0 if which == 0 else 48
            for c in range(16):
                tmp, b_tmp = tmps[c % 2]
                S.op("dve", lambda e, c=c, tmp=tmp: e.tensor_tensor(out=tmp[:], in0=xTb[:, c, :], in1=rstd[:], op=ALU.mult), reads=[b_xTb, b_rstd], writes=[b_tmp])
                if h32cb is None:
                    S.op("act", lambda e, c=c, tmp=tmp, l=l, g=g, which=which: e.activation(hT[:, c, :], tmp[:], AF.Identity, scale=gsc[:, l, which, c, g:g + 1],
                                                                     bias=modT[:, l, sh0 + c, g:g + 1]), reads=[b_tmp, b_gsc, b_modT], writes=[b_hT])
                else:
                    S.op("act", lambda e, c=c, tmp=tmp, l=l, g=g, which=which: e.activation(tmp[:], tmp[:], AF.Identity, scale=gsc[:, l, which, c, g:g + 1],
                                                                     bias=modT[:, l, sh0 + c, g:g + 1]), reads=[b_tmp, b_gsc, b_modT], writes=[b_tmp])
                    h32cb(c, tmp, b_tmp)

        epsb, b_epsb = sbt(es, "epsb", (128, 1), F32)
        S.op("pool", lambda e: e.memset(epsb[:], EPS), writes=[b_epsb])

        for l in range(n_layers):
            with ExitStack() as ph:
                xTb, b_xTb = sbt(ph, "xTb", (128, 16, 512), F32)
                hT, b_hT = sbt(ph, "hT", (128, 16, 512), BF16)
                sq = [sbt(ph, "sq%d" % i, (128, 512), BF16) for i in range(2)]
                rstd, b_rstd = sbt(ph, "rstd", (128, 512), F32)
                tmps = [sbt(ph, "tmp%d" % i, (128, 512), F32) for i in range(2)]
                stb = [sbt(ph, "stb%d" % i, (128, 512), BF16) for i in range(3)]
                stf = [sbt(ph, "stf%d" % i, (128, 512), F32) for i in range(3)]
                stt = [sbt(ph, "stt%d" % i, (128, 256), BF16) for i in range(3)]
                pvs = [sbt(ph, "pvs%d" % i, (128, 1024), F32) for i in range(4)]
                vnb = [sbt(ph, "vnb%d" % i, (128, 1024), BF16) for i in range(2)]
                lng, b_lng = sbt(ph, "lng", (128, 1024), F32)
                lnb, b_lnb = sbt(ph, "lnb", (128, 1024), F32)
                bst, b_bst = sbt(ph, "bst", (128, 2, 6), F32)
                bag, b_bag = sbt(ph, "bag", (128, 2), F32)
                lfh, b_lfh = sbt(ph, "lfh", (32, 512), BF16)
                lfl, b_lfl = sbt(ph, "lfl", (32, 512), BF16)
                a2s, b_a2s = sbt(ph, "a2s", (32, 2, 512), F32)
                a2hi, b_a2hi = sbt(ph, "a2hi", (32, 2, 512), BF16)
                a2lo, b_a2lo = sbt(ph, "a2lo", (32, 2, 512), BF16)
                abb, b_abb = sbt(ph, "abb", (128, 2, 512), F32)
                lt = [sbt(ph, "lt%d" % i, (128, 512), F32) for i in range(4)]
                wlf, b_wlf = sbt(ph, "wlf", (128, 16, 32), BF16)
                cnt = {"b": 0, "f": 0, "t": 0}

                S.dma("sp", lng[:], sgu_ln_g[l].partition_broadcast(128), b_lng, writes=[b_lng])
                S.dma("sp", lnb[:], sgu_ln_b[l].partition_broadcast(128), b_lnb, writes=[b_lnb])
                S.op("pool", lambda e: e.memset(a2s[:], 0.0), writes=[b_a2s])
                S.dma("sp", a2s[0:16, 0, :], gla_a2[l, 0], b_a2s, writes=[b_a2s])
                S.dma("sp", a2s[16:32, 1, :], gla_a2[l, 1], b_a2s, writes=[b_a2s])
                S.op("dve", lambda e: e.tensor_copy(a2hi[:], a2s[:]), reads=[b_a2s], writes=[b_a2hi])
                S.op("dve", lambda e: e.tensor_tensor(out=a2lo[:], in0=a2s[:], in1=a2hi[:], op=ALU.subtract), reads=[b_a2s, b_a2hi], writes=[b_a2lo])
                for d_ in range(2):
                    S.dma("sp", abb[:, d_, :], gla_ab[l, d_].partition_broadcast(128), b_abb, writes=[b_abb])

                for b in range(NBLK):
                    g = 0 if b < 4 else 1
                    t0 = b * 512
                    S.dma("sp", xTb[:], xT[:, :, t0:t0 + 512].rearrange("c p t -> p c t"), b_xTb, reads=[S.reg("xT", b)], writes=[b_xTb])
                    norm_mod((sq, rstd, b_rstd, tmps), xTb, b_xTb, hT, b_hT, l, 0, g)
                    stage("p1a")

                    def fm_group(col0, ncols, dst, name, func, scale=1.0, fp32=False):
                        for ct in range(ncols // 256):
                            wt, b_wt = next_wA()
                            load_w(wt[:], w_in[l][:, col0 + ct * 256:col0 + (ct + 1) * 256].rearrange("(kc p) n -> p kc n", p=128), b_wt)
                            for j in range(2):
                                ch = ct * 2 + j
                                pp, b_pp = bank()
                                for kc in range(16):
                                    S.op("pe", lambda e, wt=wt, j=j, kc=kc, pp=pp: e.matmul(pp[:], wt[:, kc, j * 128:(j + 1) * 128], hT[:, kc, :],
                                                                                         start=(kc == 0), stop=(kc == 15)), reads=[b_wt, b_hT], writes=[b_pp])
                                if fp32:
                                    st_, b_st = stf[cnt["f"] % 3]; cnt["f"] += 1
                                else:
                                    st_, b_st = stb[cnt["b"] % 3]; cnt["b"] += 1
                                S.op("act", lambda e, st_=st_, pp=pp: e.activation(st_[:], pp[:], func, scale=scale), reads=[b_pp], writes=[b_st])
                                S.dma("sp", dst[ch, :, t0:t0 + 512], st_[:], b_st, reads=[b_st], writes=[S.reg(name, ch, b)])

                    def tm_group(col0, ncols, evac):
                        for ct in range(ncols // 256):
                            wt, b_wt = next_wA()
                            load_w(wt[:], w_in[l][:, col0 + ct * 256:col0 + (ct + 1) * 256].rearrange("(kc p) n -> p kc n", p=128), b_wt)
                            for ts in range(4):
                                pp, b_pp = bank()
                                for kc in range(16):
                                    S.op("pe", lambda e, wt=wt, ts=ts, kc=kc, pp=pp: e.matmul(pp[:, 0:256], hT[:, kc, ts * 128:(ts + 1) * 128], wt[:, kc, :],
                                                                                          start=(kc == 0), stop=(kc == 15)), reads=[b_wt, b_hT], writes=[b_pp])
                                evac(ct, ts, pp, b_pp)

                    def tm_store(dst, name, func):
                        def evac(ct, ts, pp, b_pp):
                            st_, b_st = stt[cnt["t"] % 3]; cnt["t"] += 1
                            S.op("act", lambda e, st_=st_, pp=pp: e.activation(st_[:], pp[:, 0:256], func), reads=[b_pp], writes=[b_st])
                            S.dma("sp", dst[t0 + ts * 128:t0 + (ts + 1) * 128, ct * 256:(ct + 1) * 256], st_[:], b_st, reads=[b_st],
                                  writes=[S.reg(name, ct, b * 4 + ts)])
                        return evac

                    fm_group(0, 1024, puT, "puT", AF.Gelu_apprx_tanh)
                    stage("p1b")

                    def evac_pv(ct, ts, pp, b_pp):
                        pvt, b_pvt = pvs[ts]
                        S.op("act", lambda e, pvt=pvt, pp=pp, ct=ct: e.activation(pvt[:, ct * 256:(ct + 1) * 256], pp[:, 0:256], AF.Gelu_apprx_tanh), reads=[b_pp], writes=[b_pvt])
                    tm_group(1024, 1024, evac_pv)
                    for ts in range(4):
                        pvt, b_pvt = pvs[ts]
                        vb_, b_vb = vnb[ts % 2]
                        for hh in range(2):
                            S.op("dve", lambda e, pvt=pvt, hh=hh: e.bn_stats(bst[:, hh, :], pvt[:, hh * 512:(hh + 1) * 512]), reads=[b_pvt], writes=[b_bst])
                        S.op("dve", lambda e: e.bn_aggr(bag[:], bst[:].rearrange("p a s -> p (a s)")), reads=[b_bst], writes=[b_bag])
                        S.op("act", lambda e: e.activation(bag[:, 1:2], bag[:, 1:2], AF.Sqrt, bias=epsb[:]), reads=[b_bag, b_epsb], writes=[b_bag])
                        S.op("dve", lambda e: e.reciprocal(bag[:, 1:2], bag[:, 1:2]), reads=[b_bag], writes=[b_bag])
                        S.op("dve", lambda e, pvt=pvt: e.tensor_scalar(out=pvt[:], in0=pvt[:], scalar1=bag[:, 0:1], scalar2=bag[:, 1:2], op0=ALU.subtract, op1=ALU.mult),
                             reads=[b_pvt, b_bag], writes=[b_pvt])
                        S.op("dve", lambda e, pvt=pvt: e.tensor_tensor(out=pvt[:], in0=pvt[:], in1=lng[:], op=ALU.mult), reads=[b_pvt, b_lng], writes=[b_pvt])
                        S.op("dve", lambda e, pvt=pvt, vb_=vb_: e.tensor_tensor(out=vb_[:], in0=pvt[:], in1=lnb[:], op=ALU.add), reads=[b_pvt, b_lnb], writes=[b_vb])
                        S.dma("sp", vn_d[t0 + ts * 128:t0 + (ts + 1) * 128, :], vb_[:], b_vb, reads=[b_vb], writes=[S.reg("vn", b * 4 + ts)])

                    stage("p1c")
                    fm_group(2048, 512, qT_d, "qT", AF.Copy, scale=128.0 ** -0.5, fp32=True)
                    fm_group(2560, 512, kT_d, "kT", AF.Copy, fp32=True)
                    tm_group(2560, 512, tm_store(ktm_d, "ktm", AF.Copy))
                    tm_group(3072, 1024, tm_store(vtm_d, "vtm", AF.Copy))
                    tm_group(4096, 1024, tm_store(rtm_d, "rtm", AF.Silu))

                    stage("p1d")
                    load_w(wlf[:], w_in[l][:, 5120:5152].rearrange("(kc p) n -> p kc n", p=128), b_wlf, kind="lf")
                    pp, b_pp = bank()
                    for kc in range(16):
                        S.op("pe", lambda e, kc=kc, pp=pp: e.matmul(pp[0:32, :], wlf[:, kc, :], hT[:, kc, :], start=(kc == 0), stop=(kc == 15)),
                             reads=[b_wlf, b_hT], writes=[b_pp])
                    S.op("dve", lambda e, pp=pp: e.tensor_copy(lfh[:], pp[0:32, :]), reads=[b_pp], writes=[b_lfh])
                    S.op("dve", lambda e, pp=pp: e.tensor_tensor(out=lfl[:], in0=pp[0:32, :], in1=lfh[:], op=ALU.subtract), reads=[b_pp, b_lfh], writes=[b_lfl])
                    for d_ in range(2):
                        for ts in range(4):
                            pp2, b_pp2 = bank()
                            S.op("pe", lambda e, pp2=pp2, ts=ts, d_=d_: e.matmul(pp2[:], lfh[:, ts * 128:(ts + 1) * 128], a2hi[:, d_, :], start=True, stop=False),
                                 reads=[b_lfh, b_a2hi], writes=[b_pp2])
                            S.op("pe", lambda e, pp2=pp2, ts=ts, d_=d_: e.matmul(pp2[:], lfl[:, ts * 128:(ts + 1) * 128], a2hi[:, d_, :], start=False, stop=False),
                                 reads=[b_lfl, b_a2hi], writes=[b_pp2])
                            S.op("pe", lambda e, pp2=pp2, ts=ts, d_=d_: e.matmul(pp2[:], lfh[:, ts * 128:(ts + 1) * 128], a2lo[:, d_, :], start=False, stop=True),
                                 reads=[b_lfh, b_a2lo], writes=[b_pp2])
                            l0, b_l0 = lt[(d_ * 4 + ts) % 2]
                            l2, b_l2 = lt[2 + (d_ * 4 + ts) % 2]
                            S.op("dve", lambda e, pp2=pp2, l0=l0, d_=d_: e.tensor_tensor(out=l0[:], in0=pp2[:], in1=abb[:, d_, :], op=ALU.add), reads=[b_pp2, b_abb], writes=[b_l0])
                            S.op("act", lambda e, l0=l0: e.activation(l0[:], l0[:], AF.Exp, scale=-1.0), reads=[b_l0], writes=[b_l0])
                            S.op("act", lambda e, l0=l0, l2=l2: e.activation(l2[:], l0[:], AF.Ln, bias=onesf[:, 0:1]), reads=[b_l0, b_onesf], writes=[b_l2])
                            S.dma("sp", latm_d[d_, t0 + ts * 128:t0 + (ts + 1) * 128, :], l2[:], b_l2, reads=[b_l2], writes=[S.reg("latm", d_, b * 4 + ts)])

                    stage("p1e")
                    fm_group(5152, 2048, gaT_d, "gaT", AF.Sigmoid)
                    fm_group(7200, 2048, gbT_d, "gbT", AF.Sigmoid)
                S.barrier()
                stage("p1_%d" % l)

            with ExitStack() as ph:
                wsr, b_wsr = sbt(ph, "wsr", (128, 8, 128), F32)
                wsT, b_wsT = sbt(ph, "wsT", (128, 8, 128), BF16)
                bsf, b_bsf = sbt(ph, "bsf", (128, 1024), F32)
                ftm = [sbt(ph, "ftm%d" % i, (128, 4, 128), F32) for i in range(2)]
                vnc = [sbt(ph, "vnc%d" % i, (128, 1024), BF16) for i in range(2)]
                puc = [sbt(ph, "puc%d" % i, (128, 8, 128), BF16) for i in range(2)]
                atc = [sbt(ph, "atc%d" % i, (128, 8, 128), BF16) for i in range(2)]
                S.dma("sp", wsr[:], w_spatial[l].rearrange("g p q -> p g q"), b_wsr, writes=[b_wsr])
                S.dma("sp", bsf[:], b_spatial[l].rearrange("g p -> (g p)").partition_broadcast(128), b_bsf, writes=[b_bsf])
                for g2 in range(2):
                    pt, b_pt = bank()
                    for j in range(4):
                        gg = g2 * 4 + j
                        S.op("pe", lambda e, pt=pt, j=j, gg=gg: e.transpose(pt[:, j * 128:(j + 1) * 128], wsr[:, gg, :], identf[:]), reads=[b_wsr, b_identf], writes=[b_pt])
                    S.op("dve", lambda e, pt=pt, g2=g2: e.tensor_copy(wsT[:, g2 * 4:(g2 + 1) * 4, :], pt[:].rearrange("p (j t) -> p j t", t=128)), reads=[b_pt], writes=[b_wsT])
                for i in range(24):
                    vc, b_vc = vnc[i % 2]
                    pc, b_pc = puc[i % 2]
                    ac, b_ac = atc[i % 2]
                    S.dma("sp", vc[:], vn_d[i * 128:(i + 1) * 128, :], b_vc, reads=[S.reg("vn", i)], writes=[b_vc])
                    S.dma("sp", pc[:], puT[:, :, i * 128:(i + 1) * 128].rearrange("c p t -> p c t"), b_pc, reads=[S.reg("puT", c, i // 4) for c in range(8)], writes=[b_pc])
                    for g2 in range(2):
                        pp, b_pp = bank()
                        for j in range(4):
                            gg = g2 * 4 + j
                            S.op("pe", lambda e, pp=pp, j=j, gg=gg, vc=vc: e.matmul(pp[:, j * 128:(j + 1) * 128], vc[:, gg * 128:(gg + 1) * 128], wsT[:, gg, :], start=True, stop=True),
                                 reads=[b_vc, b_wsT], writes=[b_pp])
                        ft_, b_ft = ftm[g2]
                        S.op("dve", lambda e, pp=pp, g2=g2, ft_=ft_: e.tensor_tensor(out=ft_[:], in0=pp[:].rearrange("p (j t) -> p j t", t=128),
                                                                             in1=bsf[:, g2 * 512:(g2 + 1) * 512].rearrange("p (j t) -> p j t", t=128), op=ALU.add), reads=[b_pp, b_bsf], writes=[b_ft])
                        S.op("pool", lambda e, g2=g2, pc=pc, ac=ac, ft_=ft_: e.tensor_tensor(out=ac[:, g2 * 4:(g2 + 1) * 4, :], in0=ft_[:],
                                                                                    in1=pc[:, g2 * 4:(g2 + 1) * 4, :], op=ALU.mult), reads=[b_ft, b_pc], writes=[b_ac])
                    S.dma("sp", aT_d[:, :, i * 128:(i + 1) * 128].rearrange("c p t -> p c t"), ac[:], b_ac, reads=[b_ac], writes=[S.reg("aT", i)])
                S.barrier()
                stage("p2_%d" % l)

            with ExitStack() as ph:
                Sf, b_Sf = sbt(ph, "Sf", (128, 4, 256), F32)
                Sb, b_Sb = sbt(ph, "Sb", (128, 4, 256), BF16)
                gng, b_gng = sbt(ph, "gng", (64, 1024), F32)
                qTc = [sbt(ph, "qTc%d" % i, (128, 4, 64), F32) for i in range(2)]
                kTc = [sbt(ph, "kTc%d" % i, (128, 4, 64), F32) for i in range(2)]
                ktc = [sbt(ph, "ktc%d" % i, (64, 512), BF16) for i in range(2)]
                vtc = [sbt(ph, "vtc%d" % i, (64, 1024), BF16) for i in range(2)]
                lac = [sbt(ph, "lac%d" % i, (64, 512), F32) for i in range(2)]
                lah = [sbt(ph, "lah%d" % i, (64, 512), BF16) for i in range(2)]
                lal = [sbt(ph, "lal%d" % i, (64, 512), BF16) for i in range(2)]
                rtc = [sbt(ph, "rtc%d" % i, (64, 1024), BF16) for i in range(2)]
                ofc = [sbt(ph, "ofc%d" % i, (64, 1024), F32) for i in range(2)]
                E1 = [sbt(ph, "E1%d" % i, (128, 4, 64), F32) for i in range(2)]
                E2 = [sbt(ph, "E2%d" % i, (128, 4, 64), F32) for i in range(2)]
                qd = [sbt(ph, "qd%d" % i, (128, 4, 64), BF16) for i in range(2)]
                kd = [sbt(ph, "kd%d" % i, (128, 4, 64), BF16) for i in range(2)]
                EK = [sbt(ph, "EK%d" % i, (64, 512), F32) for i in range(2)]
                kend = [sbt(ph, "kend%d" % i, (64, 512), BF16) for i in range(2)]
                attm = [sbt(ph, "attm%d" % i, (64, 4, 64), BF16) for i in range(2)]
                osb = [sbt(ph, "osb%d" % i, (64, 1024), F32) for i in range(2)]
                ot2 = [sbt(ph, "ot2%d" % i, (64, 1024), F32) for i in range(2)]
                ogb = [sbt(ph, "ogb%d" % i, (64, 1024), BF16) for i in range(2)]
                oTc = [sbt(ph, "oTc%d" % i, (128, 8, 64), BF16) for i in range(2)]
                ssq = [sbt(ph, "ssq%d" % i, (64, 4), F32) for i in range(2)]
                junk, b_junk = sbt(ph, "junk", (64, 256), BF16)
                S.dma("sp", gng[:], gla_norm_g[l].partition_broadcast(64), b_gng, writes=[b_gng])
                it = [0]
                for si, (s0, L, grp) in enumerate(SEQS):
                    N = L // 64
                    for dr in range(2):
                        if grp == 0:
                            S.dma("sp", Sf[:], st_in[l, dr].rearrange("h d e -> d h e"), b_Sf, writes=[b_Sf])
                        else:
                            S.op("pool", lambda e: e.memset(Sf[:], 0.0), writes=[b_Sf])
                        S.op("act", lambda e: e.activation(Sb[:], Sf[:], AF.Copy), reads=[b_Sf], writes=[b_Sb])
                        order = range(N) if dr == 0 else range(N - 1, -1, -1)
                        for n in order:
                            k_ = it[0] % 2
                            it[0] += 1
                            tk = s0 + n * 64
                            c64 = tk // 64
                            b5 = tk // 512
                            t128 = tk // 128
                            q_, b_q = qTc[k_]; kk_, b_k = kTc[k_]; kt_, b_kt = ktc[k_]; vt_, b_vt = vtc[k_]; la_, b_la = lac[k_]
                            S.dma("sp", q_[:], qT_d[:, :, tk:tk + 64].rearrange("h p t -> p h t"), b_q, reads=[S.reg("qT", h, b5) for h in range(4)], writes=[b_q])
                            S.dma("sp", kk_[:], kT_d[:, :, tk:tk + 64].rearrange("h p t -> p h t"), b_k, reads=[S.reg("kT", h, b5) for h in range(4)], writes=[b_k])
                            S.dma("sp", kt_[:], ktm_d[tk:tk + 64, :], b_kt, reads=[S.reg("ktm", c, t128) for c in range(2)], writes=[b_kt])
                            S.dma("sp", vt_[:], vtm_d[tk:tk + 64, :], b_vt, reads=[S.reg("vtm", c, t128) for c in range(4)], writes=[b_vt])
                            S.dma("sp", la_[:], latm_d[dr, tk:tk + 64, :], b_la, reads=[S.reg("latm", dr, t128)], writes=[b_la])
                            lah_, b_lah = lah[k_]; lal_, b_lal = lal[k_]
                            S.op("dve", lambda e, lah_=lah_, la_=la_: e.tensor_copy(lah_[:], la_[:]), reads=[b_la], writes=[b_lah])
                            S.op("pool", lambda e, lal_=lal_, lah_=lah_, la_=la_: e.tensor_tensor(out=lal_[:], in0=la_[:], in1=lah_[:], op=ALU.subtract), reads=[b_la, b_lah], writes=[b_lal])
                            p1, b_p1 = bank()
                            for h in range(4):
                                S.op("pe", lambda e, p1=p1, h=h, lah_=lah_, dr=dr: e.matmul(p1[:, h * 64:(h + 1) * 64], lah_[:, h * 128:(h + 1) * 128], triSb[:, dr, :], start=True, stop=False),
                                     reads=[b_lah, b_triSb], writes=[b_p1])
                                S.op("pe", lambda e, p1=p1, h=h, lal_=lal_, dr=dr: e.matmul(p1[:, h * 64:(h + 1) * 64], lal_[:, h * 128:(h + 1) * 128], triSb[:, dr, :], start=False, stop=True),
                                     reads=[b_lal, b_triSb], writes=[b_p1])
                            p2, b_p2 = bank()
                            S.op("pe", lambda e, p2=p2, lah_=lah_, dr=dr: e.matmul(p2[0:64, :], triSb[:, 2 + dr, :], lah_[:], start=True, stop=False), reads=[b_lah, b_triSb], writes=[b_p2])
                            S.op("pe", lambda e, p2=p2, lal_=lal_, dr=dr: e.matmul(p2[0:64, :], triSb[:, 2 + dr, :], lal_[:], start=False, stop=True), reads=[b_lal, b_triSb], writes=[b_p2])
                            e1, b_e1 = E1[k_]; e2, b_e2 = E2[k_]; ek, b_ek = EK[k_]
                            S.op("act", lambda e, p1=p1, e1=e1: e.activation(e1[:], p1[:, 0:256].rearrange("p (h c) -> p h c", c=64), AF.Exp), reads=[b_p1], writes=[b_e1])
                            S.op("act", lambda e, p1=p1, e2=e2: e.activation(e2[:], p1[:, 0:256].rearrange("p (h c) -> p h c", c=64), AF.Exp, scale=-1.0), reads=[b_p1], writes=[b_e2])
                            S.op("act", lambda e, p2=p2, ek=ek: e.activation(ek[:], p2[0:64, :], AF.Exp), reads=[b_p2], writes=[b_ek])
                            qd_, b_qd = qd[k_]; kd_, b_kd = kd[k_]; ke_, b_ke = kend[k_]
                            S.op("dve", lambda e, qd_=qd_, q_=q_, e1=e1: e.tensor_tensor(out=qd_[:], in0=q_[:], in1=e1[:], op=ALU.mult), reads=[b_q, b_e1], writes=[b_qd])
                            S.op("dve", lambda e, kd_=kd_, kk_=kk_, e2=e2: e.tensor_tensor(out=kd_[:], in0=kk_[:], in1=e2[:], op=ALU.mult), reads=[b_k, b_e2], writes=[b_kd])
                            S.op("pool", lambda e, ke_=ke_, kt_=kt_, ek=ek: e.tensor_tensor(out=ke_[:], in0=kt_[:], in1=ek[:], op=ALU.mult), reads=[b_kt, b_ek], writes=[b_ke])
                            p3, b_p3 = bank()
                            for h in range(4):
                                S.op("pe", lambda e, p3=p3, h=h, kd_=kd_, qd_=qd_: e.matmul(p3[0:64, h * 64:(h + 1) * 64], kd_[:, h, :], qd_[:, h, :], start=True, stop=True),
                                     reads=[b_kd, b_qd], writes=[b_p3])
                            am_, b_am = attm[k_]
                            S.op("dve", lambda e, p3=p3, am_=am_, dr=dr: e.tensor_tensor(out=am_[:], in0=p3[0:64, 0:256].rearrange("p (h c) -> p h c", c=64), in1=triM[:, dr, :, :], op=ALU.mult),
                                 reads=[b_p3, b_triM], writes=[b_am])
                            po = [bank(), bank()]
                            for h in range(4):
                                pt_, b_pt_ = po[h // 2]
                                hh = h % 2
                                S.op("pe", lambda e, pt_=pt_, hh=hh, h=h, am_=am_, vt_=vt_: e.matmul(pt_[0:64, hh * 256:(hh + 1) * 256], am_[:, h, :], vt_[:, h * 256:(h + 1) * 256], start=True, stop=False),
                                     reads=[b_am, b_vt], writes=[b_pt_])
                                S.op("pe", lambda e, pt_=pt_, hh=hh, h=h, qd_=qd_: e.matmul(pt_[0:64, hh * 256:(hh + 1) * 256], qd_[:, h, :], Sb[:, h, :], start=False, stop=True),
                                     reads=[b_qd, b_Sb], writes=[b_pt_])
                            pd = [bank(), bank()]
                            for h in range(4):
                                pt_, b_pt_ = pd[h // 2]
                                hh = h % 2
                                S.op("pe", lambda e, pt_=pt_, hh=hh, h=h, ke_=ke_, vt_=vt_: e.matmul(pt_[:, hh * 256:(hh + 1) * 256], ke_[:, h * 128:(h + 1) * 128], vt_[:, h * 256:(h + 1) * 256], start=True, stop=True),
                                     reads=[b_ke, b_vt], writes=[b_pt_])
                            gcol = 63 if dr == 0 else 0
                            for h in range(4):
                                pt_, b_pt_ = pd[h // 2]
                                hh = h % 2
                                S.op("dve", lambda e, pt_=pt_, hh=hh, h=h, e1=e1, gcol=gcol: e.scalar_tensor_tensor(out=Sf[:, h, :], in0=Sf[:, h, :], scalar=e1[:, h, gcol:gcol + 1],
                                                                                                     in1=pt_[:, hh * 256:(hh + 1) * 256], op0=ALU.mult, op1=ALU.add),
                                     reads=[b_Sf, b_e1, b_pt_], writes=[b_Sf])
                            S.op("act", lambda e: e.activation(Sb[:], Sf[:], AF.Copy), reads=[b_Sf], writes=[b_Sb])
                            if dbg and it[0] == 1 and l == 0:
                                for nm, t_, b_, shp, dt_ in (("g_e1", e1, b_e1, [128, 4, 64], F32), ("g_ek", ek, b_ek, [64, 512], F32), ("g_kend", ke_, b_ke, [64, 512], BF16),
                                                             ("g_attm", am_, b_am, [64, 4, 64], BF16), ("g_qd", qd_, b_qd, [128, 4, 64], BF16), ("g_kd", kd_, b_kd, [128, 4, 64], BF16),
                                                             ("g_S", Sf, b_Sf, [128, 4, 256], F32), ("g_la", la_, b_la, [64, 512], F32),
                                                             ("g_tri", triS, b_triS, [64, 4, 64], F32), ("g_trib", triSb, b_triSb, [64, 4, 64], BF16),
                                                             ("g_lah", lah_, b_lah, [64, 512], BF16), ("g_lal", lal_, b_lal, [64, 512], BF16), ("g_triM", triM, b_triM, [64, 2, 4, 64], F32)):
                                    dd = nc.dram_tensor(nm, shp, dt_, kind="ExternalOutput").ap()
                                    S.dma("sp", dd, t_[:], b_, reads=[b_])
                            if dr == 0:
                                os_, b_os = osb[k_]
                                for hb in range(2):
                                    pt_, b_pt_ = po[hb]
                                    S.op("act" if hb else "dve", (lambda e, pt_=pt_, hb=hb, os_=os_: e.activation(os_[:, hb * 512:(hb + 1) * 512], pt_[0:64, :], AF.Copy)) if hb else
                                         (lambda e, pt_=pt_, hb=hb, os_=os_: e.tensor_copy(os_[:, hb * 512:(hb + 1) * 512], pt_[0:64, :])), reads=[b_pt_], writes=[b_os])
                                S.dma("sp", of_d[tk:tk + 64, :], os_[:], b_os, reads=[b_os], writes=[S.reg("of", c64)])
                            else:
                                of_, b_of = ofc[k_]; rt_, b_rt = rtc[k_]
                                S.dma("sp", of_[:], of_d[tk:tk + 64, :], b_of, reads=[S.reg("of", c64)], writes=[b_of])
                                S.dma("sp", rt_[:], rtm_d[tk:tk + 64, :], b_rt, reads=[S.reg("rtm", c, t128) for c in range(4)], writes=[b_rt])
                                os_, b_os = osb[k_]
                                for hb in range(2):
                                    pt_, b_pt_ = po[hb]
                                    S.op("dve", lambda e, pt_=pt_, hb=hb, os_=os_, of_=of_: e.tensor_tensor(out=os_[:, hb * 512:(hb + 1) * 512], in0=pt_[0:64, :], in1=of_[:, hb * 512:(hb + 1) * 512], op=ALU.add),
                                         reads=[b_pt_, b_of], writes=[b_os])
                                ss_, b_ss = ssq[k_]
                                for h in range(4):
                                    S.op("act", lambda e, os_=os_, h=h, ss_=ss_: e.activation(junk[:], os_[:, h * 256:(h + 1) * 256], AF.Square, accum_out=ss_[:, h:h + 1]),
                                         reads=[b_os], writes=[b_junk, b_ss])
                                S.op("act", lambda e, ss_=ss_: e.activation(ss_[:], ss_[:], AF.Sqrt, scale=1.0 / 256.0, bias=epsb[0:64, :]), reads=[b_ss, b_epsb], writes=[b_ss])
                                S.op("dve", lambda e, ss_=ss_: e.reciprocal(ss_[:], ss_[:]), reads=[b_ss], writes=[b_ss])
                                o2_, b_o2 = ot2[k_]
                                for h in range(4):
                                    S.op("dve", lambda e, os_=os_, h=h, ss_=ss_, o2_=o2_: e.scalar_tensor_tensor(out=o2_[:, h * 256:(h + 1) * 256], in0=os_[:, h * 256:(h + 1) * 256], scalar=ss_[:, h:h + 1],
                                                                                                         in1=gng[:, h * 256:(h + 1) * 256], op0=ALU.mult, op1=ALU.mult),
                                         reads=[b_os, b_ss, b_gng], writes=[b_o2])
                                og_, b_og = ogb[k_]
                                S.op("pool", lambda e, og_=og_, o2_=o2_, rt_=rt_: e.tensor_tensor(out=og_[:], in0=o2_[:], in1=rt_[:], op=ALU.mult), reads=[b_o2, b_rt], writes=[b_og])
                                ptr, b_ptr = bank()
                                ptb = ptr[:].bitcast(BF16)
                                for c in range(8):
                                    S.op("pe", lambda e, ptb=ptb, c=c, og_=og_: e.transpose(ptb[:, c * 64:(c + 1) * 64], og_[:, c * 128:(c + 1) * 128], identb[0:64, 0:64]),
                                         reads=[b_og, b_identb], writes=[b_ptr])
                                oc_, b_oc = oTc[k_]
                                S.op("dve", lambda e, ptb=ptb, oc_=oc_: e.tensor_copy(oc_[:], ptb[:, 0:512].rearrange("p (c t) -> p c t", t=64)), reads=[b_ptr], writes=[b_oc])
                                S.dma("sp", oT_d[:, :, tk:tk + 64].rearrange("c p t -> p c t"), oc_[:], b_oc, reads=[b_oc], writes=[S.reg("oT", c64)])
                        if grp == 1:
                            S.dma("sp", ns_out[si - 1, l, dr].rearrange("h d e -> d h e"), Sf[:], b_Sf, reads=[b_Sf])
                S.barrier()
                stage("p3_%d" % l)

            with ExitStack() as ph:
                is_moe = (l % 2 == 1)
                jl = l // 2
                xTb, b_xTb = sbt(ph, "xTb", (128, 16, 512), F32)
                hT, b_hT = sbt(ph, "hT", (128, 16, 512), BF16)
                gT, b_gT = sbt(ph, "gT", (128, NFF, 512), BF16)
                mTb, b_mTb = gT, b_gT
                aTb, b_aTb = hT[:, 0:8, :], b_hT
                oTb, b_oTb = hT[:, 8:16, :], b_hT
                gab = [sbt(ph, "gab%d" % i, (128, 512), BF16) for i in range(2)]
                gbb = [sbt(ph, "gbb%d" % i, (128, 512), BF16) for i in range(2)]
                sq = [sbt(ph, "sq%d" % i, (128, 512), BF16) for i in range(2)]
                rstd, b_rstd = sbt(ph, "rstd", (128, 512), F32)
                tmps = [sbt(ph, "tmp%d" % i, (128, 512), F32) for i in range(2)]
                t1 = [sbt(ph, "t1%d" % i, (128, 512), BF16) for i in range(2)]
                t2 = [sbt(ph, "t2%d" % i, (128, 512), BF16) for i in range(2)]
                sil = [sbt(ph, "sil%d" % i, (128, 512), BF16) for i in range(2)]
                u3s = [sbt(ph, "u3s%d" % i, (128, 512), BF16) for i in range(2)]
                w2t = [sbt(ph, "w2t%d" % i, (128, 2, 512), BF16) for i in range(3)]
                if is_moe:
                    rwf, b_rwf = sbt(ph, "rwf", (128, 16, NE), F32)
                    rwh, b_rwh = sbt(ph, "rwh", (128, 16, NE), BF16)
                    rwl, b_rwl = sbt(ph, "rwl", (128, 16, NE), BF16)
                    hlo = [sbt(ph, "hlo%d" % i, (128, 512), BF16) for i in range(2)]
                    selEb, b_selEb = selE, b_selE
                    cmbTh, b_cmbTh = sbt(ph, "cmbTh", (NE, 512), BF16)
                    rbb, b_rbb = sbt(ph, "rbb", (128, NE), F32)
                    lgT, b_lgT = sbt(ph, "lgT", (NE, 512), F32)
                    lg, b_lg = sbt(ph, "lg", (128, NE), F32)
                    mk1, b_mk1 = sbt(ph, "mk1", (128, NE), F32)
                    mk2, b_mk2 = sbt(ph, "mk2", (128, NE), F32)
                    m12, b_m12 = sbt(ph, "m12", (128, 4), F32)
                    cmb, b_cmb = sbt(ph, "cmb", (128, NE), F32)
                    cmbT, b_cmbT = sbt(ph, "cmbT", (NE, 512), F32)
                    cbes = [sbt(ph, "cbe%d" % i, (128, 512), BF16) for i in range(2)]
                    S.dma("sp", rwf[:], moe_router[jl].rearrange("(kc p) e -> p kc e", p=128), b_rwf, writes=[b_rwf])
                    S.dma("sp", rbb[:], moe_router_b[jl].partition_broadcast(128), b_rbb, writes=[b_rbb])
                    S.op("dve", lambda e: e.tensor_copy(rwh[:], rwf[:]), reads=[b_rwf], writes=[b_rwh])
                    S.op("dve", lambda e: e.tensor_tensor(out=rwl[:], in0=rwf[:], in1=rwh[:], op=ALU.subtract), reads=[b_rwf, b_rwh], writes=[b_rwl])

                w2i = [0]
                for b in range(NBLK):
                    g = 0 if b < 4 else 1
                    t0 = b * 512
                    S.dma("sp", xTb[:], xT[:, :, t0:t0 + 512].rearrange("c p t -> p c t"), b_xTb, reads=[S.reg("xT", b)], writes=[b_xTb])
                    S.dma("sp", aTb, aT_d[:, :, t0:t0 + 512].rearrange("c p t -> p c t"), b_aTb, reads=[S.reg("aT", 4 * b + i) for i in range(4)], writes=[b_aTb])
                    S.dma("sp", oTb, oT_d[:, :, t0:t0 + 512].rearrange("c p t -> p c t"), b_oTb, reads=[S.reg("oT", 8 * b + i) for i in range(8)], writes=[b_oTb])
                    for ct in range(8):
                        wt, b_wt = next_wA()
                        load_w(None, w_branch_a[l][:, ct * 256:(ct + 1) * 256].rearrange("(kc p) n -> p kc n", p=128), b_wt, kind="ha")
                        load_w(wt[:], w_branch_b[l][:, ct * 256:(ct + 1) * 256].rearrange("(kc p) n -> p kc n", p=128), b_wt, kind="hb")
                        for j in range(2):
                            oc = ct * 2 + j
                            ga_, b_ga = gab[oc % 2]; gb_, b_gb = gbb[oc % 2]
                            S.dma("sp", ga_[:], gaT_d[oc, :, t0:t0 + 512], b_ga, reads=[S.reg("gaT", oc, b)], writes=[b_ga])
                            S.dma("sp", gb_[:], gbT_d[oc, :, t0:t0 + 512], b_gb, reads=[S.reg("gbT", oc, b)], writes=[b_gb])
                            pa, b_pa = bank()
                            pb_, b_pb = bank()
                            for kc in range(8):
                                S.op("pe", lambda e, pa=pa, wt=wt, kc=kc, j=j: e.matmul(pa[:], wt[:, kc, j * 128:(j + 1) * 128], aTb[:, kc, :], start=(kc == 0), stop=(kc == 7)),
                                     reads=[b_wt, b_aTb], writes=[b_pa])
                            for kc in range(8):
                                S.op("pe", lambda e, pb_=pb_, wt=wt, kc=kc, j=j: e.matmul(pb_[:], wt[:, 8 + kc, j * 128:(j + 1) * 128], oTb[:, kc, :], start=(kc == 0), stop=(kc == 7)),
                                     reads=[b_wt, b_oTb], writes=[b_pb])
                            ta, b_ta = t1[oc % 2]; tb_, b_tb = t2[oc % 2]
                            S.op("dve", lambda e, ta=ta, pa=pa, ga_=ga_: e.tensor_tensor(out=ta[:], in0=pa[:], in1=ga_[:], op=ALU.mult), reads=[b_pa, b_ga], writes=[b_ta])
                            S.op("dve", lambda e, tb_=tb_, pb_=pb_, gb_=gb_: e.tensor_tensor(out=tb_[:], in0=pb_[:], in1=gb_[:], op=ALU.mult), reads=[b_pb, b_gb], writes=[b_tb])
                            S.op("pool", lambda e, ta=ta, tb_=tb_, oc=oc: e.tensor_tensor(out=mTb[:, oc, :], in0=ta[:], in1=tb_[:], op=ALU.add), reads=[b_ta, b_tb], writes=[b_mTb])
                    for ct in range(8):
                        wt, b_wt = next_wA()
                        load_w(wt[:], w_out[l][:, ct * 256:(ct + 1) * 256].rearrange("(kc p) n -> p kc n", p=128), b_wt)
                        for j in range(2):
                            oc = ct * 2 + j
                            pp, b_pp = bank()
                            for kc in range(16):
                                S.op("pe", lambda e, pp=pp, wt=wt, kc=kc, j=j: e.matmul(pp[:], wt[:, kc, j * 128:(j + 1) * 128], mTb[:, kc, :], start=(kc == 0), stop=(kc == 15)),
                                     reads=[b_wt, b_mTb], writes=[b_pp])
                            S.op("dve", lambda e, pp=pp, oc=oc, l=l, g=g: e.scalar_tensor_tensor(out=xTb[:, oc, :], in0=pp[:], scalar=modT[:, l, 32 + oc, g:g + 1], in1=xTb[:, oc, :], op0=ALU.mult, op1=ALU.add),
                                 reads=[b_pp, b_modT, b_xTb], writes=[b_xTb])
                    if is_moe:
                        plg, b_plg = bank()

                        def h32cb(c, tmp, b_tmp, plg=plg, b_plg=b_plg):
                            hl_, b_hl = hlo[c % 2]
                            S.op("dve", lambda e, c=c, tmp=tmp: e.tensor_copy(hT[:, c, :], tmp[:]), reads=[b_tmp], writes=[b_hT])
                            S.op("dve", lambda e, c=c, tmp=tmp, hl_=hl_: e.tensor_tensor(out=hl_[:], in0=tmp[:], in1=hT[:, c, :], op=ALU.subtract), reads=[b_tmp, b_hT], writes=[b_hl])
                            S.op("pe", lambda e, c=c: e.matmul(plg[0:NE, :], rwh[:, c, :], hT[:, c, :], start=(c == 0), stop=False), reads=[b_hT, b_rwh], writes=[b_plg])
                            S.op("pe", lambda e, c=c, hl_=hl_: e.matmul(plg[0:NE, :], rwh[:, c, :], hl_[:], start=False, stop=False), reads=[b_hl, b_rwh], writes=[b_plg])
                            S.op("pe", lambda e, c=c: e.matmul(plg[0:NE, :], rwl[:, c, :], hT[:, c, :], start=False, stop=(c == 15)), reads=[b_hT, b_rwl], writes=[b_plg])
                        norm_mod((sq, rstd, b_rstd, tmps), xTb, b_xTb, hT, b_hT, l, 1, g, h32cb)
                        S.op("dve", lambda e, plg=plg: e.tensor_copy(lgT[:], plg[0:NE, :]), reads=[b_plg], writes=[b_lgT])
                        for ts in range(4):
                            pq, b_pq = bank()
                            S.op("pe", lambda e, pq=pq, ts=ts: e.transpose(pq[:, 0:NE], lgT[:, ts * 128:(ts + 1) * 128], identf[0:NE, 0:NE]), reads=[b_lgT, b_identf], writes=[b_pq])
                            S.op("dve", lambda e, pq=pq: e.tensor_tensor(out=lg[:], in0=pq[:, 0:NE], in1=rbb[:], op=ALU.add), reads=[b_pq, b_rbb], writes=[b_lg])
                            S.op("dve", lambda e: e.tensor_reduce(out=m12[:, 0:1], in_=lg[:], axis=mybir.AxisListType.X, op=ALU.max), reads=[b_lg], writes=[b_m12])
                            S.op("dve", lambda e: e.tensor_scalar(out=mk1[:], in0=lg[:], scalar1=m12[:, 0:1], scalar2=None, op0=ALU.is_equal), reads=[b_lg, b_m12], writes=[b_mk1])
                            S.op("dve", lambda e: e.scalar_tensor_tensor(out=lg[:], in0=mk1[:], scalar=-1e30, in1=lg[:], op0=ALU.mult, op1=ALU.add), reads=[b_mk1, b_lg], writes=[b_lg])
                            S.op("dve", lambda e: e.tensor_reduce(out=m12[:, 1:2], in_=lg[:], axis=mybir.AxisListType.X, op=ALU.max), reads=[b_lg], writes=[b_m12])
                            S.op("dve", lambda e: e.tensor_scalar(out=mk2[:], in0=lg[:], scalar1=m12[:, 1:2], scalar2=None, op0=ALU.is_equal), reads=[b_lg, b_m12], writes=[b_mk2])
                            S.op("dve", lambda e: e.tensor_tensor(out=m12[:, 2:3], in0=m12[:, 1:2], in1=m12[:, 0:1], op=ALU.subtract), reads=[b_m12], writes=[b_m12])
                            S.op("act", lambda e: e.activation(m12[:, 2:3], m12[:, 2:3], AF.Exp), reads=[b_m12], writes=[b_m12])
                            S.op("dve", lambda e: e.tensor_scalar(out=m12[:, 2:3], in0=m12[:, 2:3], scalar1=1.0, scalar2=None, op0=ALU.add), reads=[b_m12], writes=[b_m12])
                            S.op("dve", lambda e: e.reciprocal(m12[:, 2:3], m12[:, 2:3]), reads=[b_m12], writes=[b_m12])
                            S.op("dve", lambda e: e.tensor_scalar(out=m12[:, 3:4], in0=m12[:, 2:3], scalar1=-1.0, scalar2=1.0, op0=ALU.mult, op1=ALU.add), reads=[b_m12], writes=[b_m12])
                            S.op("dve", lambda e: e.tensor_scalar(out=mk1[:], in0=mk1[:], scalar1=m12[:, 2:3], scalar2=None, op0=ALU.mult), reads=[b_mk1, b_m12], writes=[b_mk1])
                            S.op("dve", lambda e: e.scalar_tensor_tensor(out=cmb[:], in0=mk2[:], scalar=m12[:, 3:4], in1=mk1[:], op0=ALU.mult, op1=ALU.add), reads=[b_mk1, b_mk2, b_m12], writes=[b_cmb])
                            pq2, b_pq2 = bank()
                            S.op("pe", lambda e, pq2=pq2: e.transpose(pq2[0:NE, 0:128], cmb[:], identf[:]), reads=[b_cmb, b_identf], writes=[b_pq2])
                            S.op("dve", lambda e, pq2=pq2, ts=ts: e.tensor_copy(cmbT[:, ts * 128:(ts + 1) * 128], pq2[0:NE, 0:128]), reads=[b_pq2], writes=[b_cmbT])
                        S.op("dve", lambda e: e.tensor_copy(cmbTh[:], cmbT[:]), reads=[b_cmbT], writes=[b_cmbTh])
                        experts = [(moe_w1[jl, ex], moe_w3[jl, ex], moe_w2[jl, ex], ex) for ex in range(NE)]
                    else:
                        norm_mod((sq, rstd, b_rstd, tmps), xTb, b_xTb, hT, b_hT, l, 1, g)
                        experts = [(ffn_w1[jl], ffn_w3[jl], ffn_w2[jl], None)]
                    for (W1, W3, W2, ex) in experts:
                        if ex is not None:
                            cbe, b_cbe = cbes[ex % 2]
                            pq3, b_pq3 = bank()
                            S.op("pe", lambda e, pq3=pq3, ex=ex: e.matmul(pq3[:], selEb[:, ex, :], cmbTh[:], start=True, stop=True), reads=[b_selEb, b_cmbTh], writes=[b_pq3])
                            S.op("act", lambda e, pq3=pq3, cbe=cbe: e.activation(cbe[:], pq3[:], AF.Copy), reads=[b_pq3], writes=[b_cbe])
                        for ct in range(NFF // 2):
                            w1t, b_w1t = next_wA()
                            w3t, b_w3t = next_wA()
                            load_w(w1t[:], W1[:, ct * 256:(ct + 1) * 256].rearrange("(kc p) n -> p kc n", p=128), b_w1t, eng="dve")
                            load_w(w3t[:], W3[:, ct * 256:(ct + 1) * 256].rearrange("(kc p) n -> p kc n", p=128), b_w3t, eng="act")
                            for j in range(2):
                                fc = ct * 2 + j
                                pu1, b_pu1 = bank()
                                pu3, b_pu3 = bank()
                                for kc in range(16):
                                    S.op("pe", lambda e, pu1=pu1, w1t=w1t, kc=kc, j=j: e.matmul(pu1[:], w1t[:, kc, j * 128:(j + 1) * 128], hT[:, kc, :], start=(kc == 0), stop=(kc == 15)),
                                         reads=[b_w1t, b_hT], writes=[b_pu1])
                                for kc in range(16):
                                    S.op("pe", lambda e, pu3=pu3, w3t=w3t, kc=kc, j=j: e.matmul(pu3[:], w3t[:, kc, j * 128:(j + 1) * 128], hT[:, kc, :], start=(kc == 0), stop=(kc == 15)),
                                         reads=[b_w3t, b_hT], writes=[b_pu3])
                                sl, b_sl = sil[fc % 2]
                                S.op("act", lambda e, sl=sl, pu1=pu1: e.activation(sl[:], pu1[:], AF.Silu), reads=[b_pu1], writes=[b_sl])
                                if ex is None:
                                    S.op("dve", lambda e, sl=sl, pu3=pu3, fc=fc: e.tensor_tensor(out=gT[:, fc, :], in0=pu3[:], in1=sl[:], op=ALU.mult), reads=[b_pu3, b_sl], writes=[b_gT])
                                else:
                                    u3_, b_u3 = u3s[fc % 2]
                                    S.op("dve", lambda e, u3_=u3_, pu3=pu3, cbe=cbe: e.tensor_tensor(out=u3_[:], in0=pu3[:], in1=cbe[:], op=ALU.mult), reads=[b_pu3, b_cbe], writes=[b_u3])
                                    S.op("pool", lambda e, u3_=u3_, sl=sl, fc=fc: e.tensor_tensor(out=gT[:, fc, :], in0=u3_[:], in1=sl[:], op=ALU.mult), reads=[b_u3, b_sl], writes=[b_gT])
                        for pq_ in range(4):
                            accs = [bank() for _ in range(4)]
                            for fc2 in range(NFF // 2):
                                w2_, b_w2 = w2t[w2i[0] % 3]
                                w2i[0] += 1
                                load_w(w2_[:], W2[fc2 * 256:(fc2 + 1) * 256, pq_ * 512:(pq_ + 1) * 512].rearrange("(f p) n -> p f n", p=128), b_w2, kind="w2", eng="pool")
                                for f_ in range(2):
                                    fc = fc2 * 2 + f_
                                    for j in range(4):
                                        pp, b_pp = accs[j]
                                        S.op("pe", lambda e, pp=pp, w2_=w2_, fc=fc, f_=f_, j=j: e.matmul(pp[:], w2_[:, f_, j * 128:(j + 1) * 128], gT[:, fc, :], start=(fc == 0), stop=(fc == NFF - 1)),
                                             reads=[b_w2, b_gT], writes=[b_pp])
                            for j in range(4):
                                oc = pq_ * 4 + j
                                pp, b_pp = accs[j]
                                S.op("dve", lambda e, pp=pp, oc=oc, l=l, g=g: e.scalar_tensor_tensor(out=xTb[:, oc, :], in0=pp[:], scalar=modT[:, l, 80 + oc, g:g + 1], in1=xTb[:, oc, :], op0=ALU.mult, op1=ALU.add),
                                     reads=[b_pp, b_modT, b_xTb], writes=[b_xTb])
                    S.dma("sp", xT[:, :, t0:t0 + 512].rearrange("c p t -> p c t"), xTb[:], b_xTb, reads=[b_xTb], writes=[S.reg("xT", b)])
                S.barrier()
                stage("p45_%d" % l)

        with ExitStack() as ph:
            xTb, b_xTb = sbt(ph, "xTb", (128, 16, 512), F32)
            sq = [sbt(ph, "sq%d" % i, (128, 512), BF16) for i in range(2)]
            rstd, b_rstd = sbt(ph, "rstd", (128, 512), F32)
            yo = [sbt(ph, "yo%d" % i, (128, D), F32) for i in range(2)]
            for b in range(NBLK):
                t0 = b * 512
                S.dma("sp", xTb[:], xT[:, :, t0:t0 + 512].rearrange("c p t -> p c t"), b_xTb, reads=[S.reg("xT", b)], writes=[b_xTb])
                pss, b_pss = bank()
                for c in range(16):
                    sqt, b_sq = sq[c % 2]
                    S.op("act", lambda e, c=c, sqt=sqt: e.activation(sqt[:], xTb[:, c, :], AF.Square), reads=[b_xTb], writes=[b_sq])
                    S.op("pe", lambda e, c=c, sqt=sqt, pss=pss: e.matmul(pss[:], onesb[:], sqt[:], start=(c == 0), stop=(c == 15)), reads=[b_sq, b_onesb], writes=[b_pss])
                S.op("act", lambda e, pss=pss: e.activation(rstd[:], pss[:], AF.Sqrt, scale=1.0 / D, bias=epsb[:]), reads=[b_pss, b_epsb], writes=[b_rstd])
                S.op("dve", lambda e: e.reciprocal(rstd[:], rstd[:]), reads=[b_rstd], writes=[b_rstd])
                for c in range(16):
                    S.op("dve", lambda e, c=c: e.scalar_tensor_tensor(out=xTb[:, c, :], in0=xTb[:, c, :], scalar=fgT[:, c:c + 1], in1=rstd[:], op0=ALU.mult, op1=ALU.mult),
                         reads=[b_xTb, b_fgT, b_rstd], writes=[b_xTb])
                for ts in range(4):
                    yo_, b_yo = yo[ts % 2]
                    for q4 in range(4):
                        pt, b_pt = bank()
                        for j in range(4):
                            c = q4 * 4 + j
                            S.op("pe", lambda e, pt=pt, j=j, c=c, ts=ts: e.transpose(pt[:, j * 128:(j + 1) * 128], xTb[:, c, ts * 128:(ts + 1) * 128], identf[:]),
                                 reads=[b_xTb, b_identf], writes=[b_pt])
                        if q4 % 2:
                            S.op("act", lambda e, pt=pt, q4=q4, yo_=yo_: e.activation(yo_[:, q4 * 512:(q4 + 1) * 512], pt[:], AF.Copy), reads=[b_pt], writes=[b_yo])
                        else:
                            S.op("dve", lambda e, pt=pt, q4=q4, yo_=yo_: e.tensor_copy(yo_[:, q4 * 512:(q4 + 1) * 512], pt[:]), reads=[b_pt], writes=[b_yo])
                    tok = t0 + ts * 128
                    dst = ys_out[tok:tok + 128, :] if tok < 2048 else yp_out[tok - 2048:tok - 2048 + 128, :]
                    S.dma("sp", dst, yo_[:], b_yo, reads=[b_yo])
            S.barrier()
        S.emit()
    return nc, es


_CACHE = {}


def kernel(**inputs):
    n = 8
    if "nc" not in _CACHE:
        _CACHE["nc"] = build()
    nc, _es = _CACHE["nc"]
    f = lambda a: np.ascontiguousarray(np.asarray(a, dtype=np.float32))
    xs = f(inputs["x_sample"])
    xp = f(inputs["x_prompt"])
    st = f(inputs["state_gla"])
    c = f(inputs["c"])
    c_ctx = f(inputs["c_ctx"])
    wnames = ["norm1_g", "norm2_g", "w_mod", "b_mod", "w_in", "sgu_ln_g", "sgu_ln_b", "w_spatial", "b_spatial", "gla_a2", "gla_ab",
              "gla_norm_g", "w_branch_a", "w_branch_b", "w_out", "ffn_w1", "ffn_w3", "ffn_w2", "moe_router", "moe_router_b",
              "moe_w1", "moe_w3", "moe_w2", "final_g"]
    wd = {k: f(inputs[k]) for k in wnames}
    in_maps = []
    for i in range(n):
        m = dict(wd)
        m["xs"] = xs[i]
        m["xp"] = np.ascontiguousarray(xp[4 * i:4 * i + 4].reshape(1024, D))
        m["st"] = st[i]
        m["cc"] = np.ascontiguousarray(np.stack([c[i], c_ctx], axis=0))
        in_maps.append(m)
    res = run_bass_kernel_spmd(nc, in_maps, core_ids=list(range(n)))
    R = res.results
    y_sample = np.stack([R[i]["ys"] for i in range(n)], axis=0)
    y_prompt = np.concatenate([R[i]["yp"].reshape(4, 256, D) for i in range(n)], axis=0)
    new_state = np.concatenate([R[i]["ns"] for i in range(n)], axis=0)
    return (y_prompt.astype(np.float32), y_sample.astype(np.float32), new_state.astype(np.float32))
```

```python
import math
import numpy as np
from contextlib import ExitStack
import concourse.bass as bass
import concourse.mybir as mybir
from concourse.bass_utils import run_bass_kernel_spmd

F32 = mybir.dt.float32
BF16 = mybir.dt.bfloat16
I32 = mybir.dt.int32
AF = mybir.ActivationFunctionType
ALU = mybir.AluOpType

D = 2048
DEPTH = 4
T = 3072
NBLK = 6
D_A = 1024
DFF = 5632
NFF = 44
NE = 8
EPS = 1e-6
N_IN = 9248
SEQS = [(0, 2048, 0)] + [(2048 + 256 * i, 256, 1) for i in range(4)]


class SemH:
    def __init__(self, h):
        self.h = h
        self.count = 0


class Buf:
    __slots__ = ("name", "last_w", "readers", "sem")

    def __init__(self, name):
        self.name = name
        self.last_w = None
        self.readers = []
        self.sem = None


class Op:
    __slots__ = ("eng", "fn", "deps", "needed", "is_dma", "tok")

    def __init__(self, eng, fn, is_dma=False):
        self.eng = eng
        self.fn = fn
        self.deps = []
        self.needed = False
        self.is_dma = is_dma
        self.tok = None


class Sched:
    ENGS = ("pe", "act", "dve", "pool", "sp")

    def __init__(self, nc, es):
        self.nc = nc
        self.es = es
        self.ops = {e: [] for e in self.ENGS}
        self.touched = {}
        self.cur_bar = None
        self.free_sems = []
        self.nsem = 0
        self.regs = {}
        self.phase_sems = []
        self.dead = False

    def buf(self, name):
        b = Buf(name)
        b.last_w = self.cur_bar
        return b

    def reg(self, *key):
        b = self.regs.get(key)
        if b is None:
            b = self.buf(str(key))
            self.regs[key] = b
        return b

    def _getsem(self):
        if self.free_sems:
            return self.free_sems.pop(0)
        self.nsem += 1
        return SemH(self.es.enter_context(self.nc.semaphore("ds%d" % self.nsem)))

    def _add(self, op, reads, writes):
        if self.dead:
            return op
        deps = []
        for b in reads:
            if b.last_w is not None:
                deps.append(b.last_w)
        for b in writes:
            deps.extend(b.readers)
            if b.last_w is not None:
                deps.append(b.last_w)
        seen = set()
        for d in deps:
            if d is op or id(d) in seen:
                continue
            seen.add(id(d))
            if d.eng == "pe" and op.eng == "pe" and not d.is_dma and not op.is_dma:
                continue
            op.deps.append(d)
            d.needed = True
        for b in reads:
            b.readers.append(op)
            self.touched[id(b)] = b
        for b in writes:
            b.readers = []
            b.last_w = op
            self.touched[id(b)] = b
        self.ops[op.eng].append(op)
        return op

    def op(self, eng, fn, reads=(), writes=()):
        return self._add(Op(eng, fn), list(reads), list(writes))

    def dma(self, eng, out, in_, sb, reads=(), writes=(), **kw):
        def fn(e):
            return e.dma_start(out=out, in_=in_, **kw)
        o = Op(eng, fn, is_dma=True)
        o.needed = True
        if self.dead:
            return o
        if sb.sem is None:
            sb.sem = self._getsem()
            self.phase_sems.append(sb)
        sb.sem.count += 16
        o.tok = (sb.sem.h, sb.sem.count)
        return self._add(o, list(reads), list(writes))

    def barrier(self):
        if self.dead:
            return None
        allb = list(self.touched.values())
        o = self.op("sp", lambda e: e.nop(), reads=[], writes=allb)
        o.needed = True
        self.cur_bar = o
        self.touched = {}
        for b in self.phase_sems:
            self.free_sems.append(b.sem)
            b.sem = None
        self.phase_sems = []
        return o

    def emit(self):
        nc = self.nc
        engmap = {"pe": "tensor", "act": "scalar", "dve": "vector", "pool": "gpsimd", "sp": "sync"}
        ROT = 30000
        for e in self.ENGS:
            cnt = 0
            sem = None
            k = 0
            for o in self.ops[e]:
                if o.is_dma:
                    continue
                if o.needed:
                    if sem is None or cnt >= ROT:
                        sem = self.es.enter_context(nc.semaphore("es_%s_%d" % (e, k)))
                        k += 1
                        cnt = 0
                    cnt += 1
                    o.tok = (sem, cnt)
        block = self.es.enter_context(nc.Block())
        sched = self

        def make(ename):
            def body(eng):
                waited = {}
                for o in sched.ops[ename]:
                    for d in o.deps:
                        s, v = d.tok
                        k = id(s)
                        if waited.get(k, 0) >= v:
                            continue
                        waited[k] = v
                        eng.wait_ge(s, v)
                    ins = o.fn(eng)
                    if o.is_dma:
                        ins.then_inc(o.tok[0], 16)
                    elif o.needed:
                        ins.then_inc(o.tok[0], 1)
            return body

        for ename in self.ENGS:
            getattr(block, engmap[ename])(make(ename))


class _Stop(Exception):
    pass


def build(n_layers=DEPTH, stop=None, dbg=False):
    nc = bass.Bass("TRN2", target_bir_lowering=False)
    es = ExitStack()
    S = Sched(nc, es)

    def stage(name):
        if stop == name and not S.dead:
            S.barrier()
            S.dead = True

    def din(name, shape):
        return nc.dram_tensor(name, list(shape), F32, kind="ExternalInput").ap()

    def dout(name, shape):
        return nc.dram_tensor(name, list(shape), F32, kind="ExternalOutput").ap()

    def dscr(name, shape, dt):
        if dbg:
            return nc.dram_tensor(name, list(shape), dt, kind="ExternalOutput").ap()
        return nc.dram_tensor(name, list(shape), dt).ap()

    xs_in = din("xs", (2048, D))
    xp_in = din("xp", (1024, D))
    st_in = din("st", (DEPTH, 2, 4, 128, 256))
    cc_in = din("cc", (2, D))
    norm1_g = din("norm1_g", (DEPTH, D))
    norm2_g = din("norm2_g", (DEPTH, D))
    w_mod = din("w_mod", (DEPTH, D, 6 * D))
    b_mod = din("b_mod", (DEPTH, 6 * D))
    w_in = din("w_in", (DEPTH, D, N_IN))
    sgu_ln_g = din("sgu_ln_g", (DEPTH, D_A))
    sgu_ln_b = din("sgu_ln_b", (DEPTH, D_A))
    w_spatial = din("w_spatial", (DEPTH, 8, 128, 128))
    b_spatial = din("b_spatial", (DEPTH, 8, 128))
    gla_a2 = din("gla_a2", (DEPTH, 2, 16, 512))
    gla_ab = din("gla_ab", (DEPTH, 2, 512))
    gla_norm_g = din("gla_norm_g", (DEPTH, 1024))
    w_branch_a = din("w_branch_a", (DEPTH, 1024, D))
    w_branch_b = din("w_branch_b", (DEPTH, 1024, D))
    w_out = din("w_out", (DEPTH, D, D))
    ffn_w1 = din("ffn_w1", (2, D, DFF))
    ffn_w3 = din("ffn_w3", (2, D, DFF))
    ffn_w2 = din("ffn_w2", (2, DFF, D))
    moe_router = din("moe_router", (2, D, NE))
    moe_router_b = din("moe_router_b", (2, NE))
    moe_w1 = din("moe_w1", (2, NE, D, DFF))
    moe_w3 = din("moe_w3", (2, NE, D, DFF))
    moe_w2 = din("moe_w2", (2, NE, DFF, D))
    final_g = din("final_g", (D,))
    ys_out = dout("ys", (2048, D))
    yp_out = dout("yp", (1024, D))
    ns_out = dout("ns", (4, DEPTH, 2, 4, 128, 256))

    xT = dscr("xT", (16, 128, T), F32)
    puT = dscr("puT", (8, 128, T), BF16)
    vn_d = dscr("vn", (T, 1024), BF16)
    qT_d = dscr("qT", (4, 128, T), F32)
    kT_d = dscr("kT", (4, 128, T), F32)
    ktm_d = dscr("ktm", (T, 512), BF16)
    vtm_d = dscr("vtm", (T, 1024), BF16)
    rtm_d = dscr("rtm", (T, 1024), BF16)
    latm_d = dscr("latm", (2, T, 512), F32)
    gaT_d = dscr("gaT", (16, 128, T), BF16)
    gbT_d = dscr("gbT", (16, 128, T), BF16)
    aT_d = dscr("aT", (8, 128, T), BF16)
    oT_d = dscr("oT", (8, 128, T), BF16)
    of_d = dscr("of", (T, 1024), F32)

    _uid = [0]

    def sbt(stack, name, shape, dt):
        _uid[0] += 1
        name = "%s_%d" % (name, _uid[0])
        t = stack.enter_context(nc.sbuf_tensor(name, list(shape), dt))
        return t, S.buf(name)

    pbk = []
    for i in range(8):
        t = es.enter_context(nc.psum_tensor("pb%d" % i, [128, 512], F32))
        pbk.append((t, S.buf("pb%d" % i)))
    bank_i = [0]

    def bank():
        r = pbk[bank_i[0] % 8]
        bank_i[0] += 1
        return r

    identb, b_identb = sbt(es, "identb", (128, 128), BF16)
    identf, b_identf = sbt(es, "identf", (128, 128), F32)
    onesb, b_onesb = sbt(es, "onesb", (128, 128), BF16)
    onesf, b_onesf = sbt(es, "onesf", (128, 128), F32)
    triS, b_triS = sbt(es, "triS", (64, 4, 64), F32)
    triM, b_triM = sbt(es, "triM", (64, 2, 4, 64), F32)
    selE, b_selE = sbt(es, "selE", (8, 8, 128), BF16)
    modT, b_modT = sbt(es, "modT", (128, DEPTH, 96, 2), F32)
    bmodT, b_bmodT = sbt(es, "bmodT", (128, DEPTH, 96), F32)
    nT, b_nT = sbt(es, "nT", (128, 2, DEPTH, 16), F32)
    fgT, b_fgT = sbt(es, "fgT", (128, 16), F32)
    gsc, b_gsc = sbt(es, "gsc", (128, DEPTH, 2, 16, 2), F32)
    scT, b_scT = sbt(es, "scT", (128, 16, 2), BF16)

    def mk_tri(dst, pattern_sign, cm, op, scale_val):
        S.op("pool", lambda e: e.memset(dst, scale_val), writes=[])
        S.op("pool", lambda e: e.affine_select(out=dst, in_=dst, pattern=[[pattern_sign, 64]], compare_op=op,
                                              fill=0.0, base=0, channel_multiplier=cm), reads=[], writes=[])

    def cst(fn, w):
        S.op("pool", fn, reads=w, writes=w)

    cst(lambda e: e.memset(identf[:], 0.0), [b_identf])
    cst(lambda e: e.affine_select(out=identf[:], in_=identf[:], pattern=[[-1, 128]], compare_op=ALU.not_equal,
                                  fill=1.0, base=0, channel_multiplier=1), [b_identf])
    S.op("dve", lambda e: e.tensor_copy(identb[:], identf[:]), reads=[b_identf], writes=[b_identb])
    cst(lambda e: e.memset(onesf[:], 1.0), [b_onesf])
    cst(lambda e: e.memset(onesb[:], 1.0), [b_onesb])
    sc16 = -1.0 / 16.0
    cst(lambda e: e.memset(triS[:], sc16), [b_triS])
    cst(lambda e: e.affine_select(out=triS[:, 0, :], in_=triS[:, 0, :], pattern=[[1, 64]], compare_op=ALU.is_ge, fill=0.0, base=0, channel_multiplier=-1), [b_triS])
    cst(lambda e: e.affine_select(out=triS[:, 1, :], in_=triS[:, 1, :], pattern=[[-1, 64]], compare_op=ALU.is_ge, fill=0.0, base=0, channel_multiplier=1), [b_triS])
    cst(lambda e: e.affine_select(out=triS[:, 2, :], in_=triS[:, 2, :], pattern=[[-1, 64]], compare_op=ALU.is_gt, fill=0.0, base=0, channel_multiplier=1), [b_triS])
    cst(lambda e: e.affine_select(out=triS[:, 3, :], in_=triS[:, 3, :], pattern=[[1, 64]], compare_op=ALU.is_gt, fill=0.0, base=0, channel_multiplier=-1), [b_triS])
    triSb, b_triSb = sbt(es, "triSb", (64, 4, 64), BF16)
    S.op("dve", lambda e: e.tensor_copy(triSb[:], triS[:]), reads=[b_triS], writes=[b_triSb])
    cst(lambda e: e.memset(triM[:], 1.0), [b_triM])
    for h in range(4):
        cst(lambda e, h=h: e.affine_select(out=triM[:, 0, h, :], in_=triM[:, 0, h, :], pattern=[[1, 64]], compare_op=ALU.is_ge, fill=0.0, base=0, channel_multiplier=-1), [b_triM])
        cst(lambda e, h=h: e.affine_select(out=triM[:, 1, h, :], in_=triM[:, 1, h, :], pattern=[[-1, 64]], compare_op=ALU.is_ge, fill=0.0, base=0, channel_multiplier=1), [b_triM])
    cst(lambda e: e.memset(selE[:], 1.0), [b_selE])
    cst(lambda e: e.affine_select(out=selE[:], in_=selE[:], pattern=[[-1, 8], [0, 128]], compare_op=ALU.is_equal, fill=0.0, base=0, channel_multiplier=1), [b_selE])

    wA = [sbt(es, "wA%d" % i, (128, 16, 512), BF16) for i in range(3)]
    wA_i = [0]

    def next_wA():
        r = wA[wA_i[0] % 3]
        wA_i[0] += 1
        return r

    def load_w(dst, src, bufobj):
        return S.dma("pool", dst, src, bufobj, writes=[bufobj])

    with nc.allow_non_contiguous_dma(reason="small one-time parameter layouts"):
        with ExitStack() as ph:
            ccT, b_ccT = sbt(ph, "ccT", (128, 16, 2), F32)
            for g_ in range(2):
                S.dma("sp", ccT[:, :, g_], cc_in[g_].rearrange("(kc p) -> p kc", p=128), b_ccT, writes=[b_ccT])
            S.op("act", lambda e: e.activation(scT[:], ccT[:], AF.Silu), reads=[b_ccT], writes=[b_scT])
            for l_ in range(DEPTH):
                S.dma("sp", bmodT[:, l_, :], b_mod[l_].rearrange("(j p) -> p j", p=128), b_bmodT, writes=[b_bmodT])
                S.dma("sp", nT[:, 0, l_, :], norm1_g[l_].rearrange("(c p) -> p c", p=128), b_nT, writes=[b_nT])
                S.dma("sp", nT[:, 1, l_, :], norm2_g[l_].rearrange("(c p) -> p c", p=128), b_nT, writes=[b_nT])
            S.dma("sp", fgT[:], final_g.rearrange("(c p) -> p c", p=128), b_fgT, writes=[b_fgT])
            for l in range(n_layers):
                pm, b_pm = bank()
                for ct in range(48):
                    wt, b_wt = next_wA()
                    load_w(wt[:, :, 0:256], w_mod[l][:, ct * 256:(ct + 1) * 256].rearrange("(kc p) n -> p kc n", p=128), b_wt)
                    for j in range(2):
                        jj = ct * 2 + j
                        for kc in range(16):
                            S.op("pe", lambda e, wt=wt, j=j, kc=kc, jj=jj, pm=pm: e.matmul(
                                pm[:, jj * 2:jj * 2 + 2], wt[:, kc, j * 128:(j + 1) * 128], scT[:, kc, :],
                                start=(kc == 0), stop=(kc == 15)), reads=[b_wt, b_scT], writes=[b_pm])
                for g in range(2):
                    S.op("dve", lambda e, l=l, g=g, pm=pm: e.tensor_tensor(
                        out=modT[:, l, :, g], in0=pm[:, 0:192].rearrange("p (j g) -> p j g", g=2)[:, :, g],
                        in1=bmodT[:, l, :], op=ALU.add), reads=[b_pm, b_bmodT], writes=[b_modT])
                for which in range(2):
                    for g in range(2):
                        sc0 = 16 + 48 * which
                        S.op("dve", lambda e, l=l, g=g, which=which, sc0=sc0: e.scalar_tensor_tensor(
                            out=gsc[:, l, which, :, g], in0=modT[:, l, sc0:sc0 + 16, g], scalar=1.0,
                            in1=nT[:, which, l, :], op0=ALU.add, op1=ALU.mult),
                            reads=[b_modT, b_nT], writes=[b_gsc])
            if dbg:
                d_modT = nc.dram_tensor("d_modT", [128, DEPTH, 96, 2], F32, kind="ExternalOutput").ap()
                d_gsc = nc.dram_tensor("d_gsc", [128, DEPTH, 2, 16, 2], F32, kind="ExternalOutput").ap()
                S.dma("sp", d_modT, modT[:], b_modT, reads=[b_modT])
                S.dma("sp", d_gsc, gsc[:], b_gsc, reads=[b_gsc])
            S.barrier()
            stage("pro")

        with ExitStack() as ph:
            freq, b_freq = sbt(ph, "freq", (128, 512), F32)
            ii, b_ii = sbt(ph, "ii", (128, 512), I32)
            pidx, b_pidx = sbt(ph, "pidx", (128, 2), I32)
            pv2, b_pv2 = sbt(ph, "pv2", (128, 2), F32)
            rv, b_rv = sbt(ph, "rv", (128, 1), F32)
            posc, b_posc = sbt(ph, "posc", (128, 1024), F32)
            posr, b_posr = sbt(ph, "posr", (128, 1024), F32)
            uu, b_uu = sbt(ph, "uu", (128, 512), F32)
            ki, b_ki = sbt(ph, "ki", (128, 512), I32)
            kf, b_kf = sbt(ph, "kf", (128, 512), F32)
            mm, b_mm = sbt(ph, "mm", (128, 512), F32)
            negpi, b_negpi = sbt(ph, "negpi", (128, 1), F32)
            xin = [sbt(ph, "xin%d" % i, (128, D), F32) for i in range(2)]
            xtt = [sbt(ph, "xtt%d" % i, (128, 16, 128), F32) for i in range(2)]
            inv2pi = 1.0 / (2.0 * math.pi)
            S.op("pool", lambda e: e.memset(negpi[:], -math.pi), writes=[b_negpi])
            S.op("pool", lambda e: e.iota(ii[:], pattern=[[1, 512]], base=0, channel_multiplier=0), writes=[b_ii])
            S.op("pool", lambda e: e.iota(pidx[:, 0:1], pattern=[[0, 1]], base=0, channel_multiplier=1), writes=[b_pidx])
            S.op("dve", lambda e: e.tensor_single_scalar(out=pidx[:, 1:2], in_=pidx[:, 0:1], scalar=6, op=ALU.arith_shift_right), reads=[b_pidx], writes=[b_pidx])
            S.op("dve", lambda e: e.tensor_single_scalar(out=pidx[:, 0:1], in_=pidx[:, 0:1], scalar=63, op=ALU.bitwise_and), reads=[b_pidx], writes=[b_pidx])
            S.op("dve", lambda e: e.tensor_copy(pv2[:], pidx[:]), reads=[b_pidx], writes=[b_pv2])
            S.op("dve", lambda e: e.tensor_copy(freq[:], ii[:]), reads=[b_ii], writes=[b_freq])
            S.op("act", lambda e: e.activation(freq[:], freq[:], AF.Exp, scale=-math.log(10000.0) / 512.0), reads=[b_freq], writes=[b_freq])

            def sincos(dst, b_dst, vcol, b_v, off):
                S.op("dve", lambda e: e.tensor_scalar(out=uu[:], in0=freq[:], scalar1=vcol, scalar2=off, op0=ALU.mult, op1=ALU.add),
                     reads=[b_freq, b_v], writes=[b_uu])
                S.op("dve", lambda e: e.tensor_copy(ki[:], uu[:]), reads=[b_uu], writes=[b_ki])
                S.op("dve", lambda e: e.tensor_copy(kf[:], ki[:]), reads=[b_ki], writes=[b_kf])
                S.op("dve", lambda e: e.tensor_tensor(out=uu[:], in0=uu[:], in1=kf[:], op=ALU.subtract), reads=[b_uu, b_kf], writes=[b_uu])
                S.op("dve", lambda e: e.tensor_single_scalar(out=mm[:], in_=uu[:], scalar=0.0, op=ALU.is_lt), reads=[b_uu], writes=[b_mm])
                S.op("dve", lambda e: e.tensor_tensor(out=uu[:], in0=uu[:], in1=mm[:], op=ALU.add), reads=[b_uu, b_mm], writes=[b_uu])
                S.op("act", lambda e: e.activation(dst, uu[:], AF.Sin, scale=2.0 * math.pi, bias=negpi[:]), reads=[b_uu, b_negpi], writes=[b_dst])

            S.op("dve", lambda e: e.tensor_scalar(out=pv2[:], in0=pv2[:], scalar1=inv2pi, scalar2=None, op0=ALU.mult), reads=[b_pv2], writes=[b_pv2])
            sincos(posc[:, 0:512], b_posc, pv2[:, 0:1], b_pv2, 0.5)
            sincos(posc[:, 512:1024], b_posc, pv2[:, 0:1], b_pv2, 0.75)
            for i in range(24):
                xt_, b_xt = xin[i % 2]
                xo, b_xo = xtt[i % 2]
                src = xs_in[i * 128:(i + 1) * 128, :] if i < 16 else xp_in[(i - 16) * 128:(i - 15) * 128, :]
                S.dma("sp", xt_[:], src, b_xt, writes=[b_xt])
                if i < 16:
                    S.op("dve", lambda e, i=i: e.tensor_scalar(out=rv[:], in0=pv2[:, 1:2], scalar1=2.0 * i * inv2pi, scalar2=None, op0=ALU.add),
                         reads=[b_pv2], writes=[b_rv])
                    sincos(posr[:, 0:512], b_posr, rv[:, 0:1], b_rv, 0.5)
                    sincos(posr[:, 512:1024], b_posr, rv[:, 0:1], b_rv, 0.75)
                    S.op("dve", lambda e, xt_=xt_: e.tensor_tensor(out=xt_[:, 0:1024], in0=xt_[:, 0:1024], in1=posr[:], op=ALU.add), reads=[b_xt, b_posr], writes=[b_xt])
                    S.op("dve", lambda e, xt_=xt_: e.tensor_tensor(out=xt_[:, 1024:2048], in0=xt_[:, 1024:2048], in1=posc[:], op=ALU.add), reads=[b_xt, b_posc], writes=[b_xt])
                for q4 in range(4):
                    pt, b_pt = bank()
                    for j in range(4):
                        c = q4 * 4 + j
                        S.op("pe", lambda e, pt=pt, j=j, c=c, xt_=xt_: e.transpose(pt[:, j * 128:(j + 1) * 128], xt_[:, c * 128:(c + 1) * 128], identf[:]),
                             reads=[b_xt, b_identf], writes=[b_pt])
                    eng = "act" if q4 % 2 else "dve"
                    if eng == "act":
                        S.op("act", lambda e, pt=pt, q4=q4, xo=xo: e.activation(xo[:, q4 * 4:(q4 + 1) * 4, :], pt[:].rearrange("p (j t) -> p j t", t=128), AF.Copy), reads=[b_pt], writes=[b_xo])
                    else:
                        S.op("dve", lambda e, pt=pt, q4=q4, xo=xo: e.tensor_copy(xo[:, q4 * 4:(q4 + 1) * 4, :], pt[:].rearrange("p (j t) -> p j t", t=128)), reads=[b_pt], writes=[b_xo])
                S.dma("sp", xT[:, :, i * 128:(i + 1) * 128].rearrange("c p t -> p c t"), xo[:], b_xo, reads=[b_xo], writes=[S.reg("xT", i // 4)])
            S.barrier()
            stage("x")

        def norm_mod(ph_bufs, xTb, b_xTb, hT, b_hT, l, which, g, h32cb=None):
            sq, rstd, b_rstd, tmps = ph_bufs
            pss, b_pss = bank()
            for c in range(16):
                sqt, b_sq = sq[c % 2]
                S.op("act", lambda e, c=c, sqt=sqt: e.activation(sqt[:], xTb[:, c, :], AF.Square), reads=[b_xTb], writes=[b_sq])
                S.op("pe", lambda e, c=c, sqt=sqt, pss=pss: e.matmul(pss[:], onesb[:], sqt[:], start=(c == 0), stop=(c == 15)),
                     reads=[b_sq, b_onesb], writes=[b_pss])
            S.op("act", lambda e, pss=pss: e.activation(rstd[:], pss[:], AF.Sqrt, scale=1.0 / D, bias=epsb[:]), reads=[b_pss, b_epsb], writes=[b_rstd])
            S.op("dve", lambda e: e.reciprocal(rstd[:], rstd[:]), reads=[b_rstd], writes=[b_rstd])
            sh0 = 0 if which == 0 else 48
            for c in range(16):
                tmp, b_tmp = tmps[c % 2]
                S.op("dve", lambda e, c=c, tmp=tmp: e.tensor_tensor(out=tmp[:], in0=xTb[:, c, :], in1=rstd[:], op=ALU.mult), reads=[b_xTb, b_rstd], writes=[b_tmp])
                if h32cb is None:
                    S.op("act", lambda e, c=c, tmp=tmp, l=l, g=g, which=which: e.activation(hT[:, c, :], tmp[:], AF.Identity, scale=gsc[:, l, which, c, g:g + 1],
                                                                     bias=modT[:, l, sh0 + c, g:g + 1]), reads=[b_tmp, b_gsc, b_modT], writes=[b_hT])
                else:
                    S.op("act", lambda e, c=c, tmp=tmp, l=l, g=g, which=which: e.activation(tmp[:], tmp[:], AF.Identity, scale=gsc[:, l, which, c, g:g + 1],
                                                                     bias=modT[:, l, sh0 + c, g:g + 1]), reads=[b_tmp, b_gsc, b_modT], writes=[b_tmp])
                    h32cb(c, tmp, b_tmp)

        epsb, b_epsb = sbt(es, "epsb", (128, 1), F32)
        S.op("pool", lambda e: e.memset(epsb[:], EPS), writes=[b_epsb])

        for l in range(n_layers):
            with ExitStack() as ph:
                xTb, b_xTb = sbt(ph, "xTb", (128, 16, 512), F32)
                hT, b_hT = sbt(ph, "hT", (128, 16, 512), BF16)
                sq = [sbt(ph, "sq%d" % i, (128, 512), BF16) for i in range(2)]
                rstd, b_rstd = sbt(ph, "rstd", (128, 512), F32)
                tmps = [sbt(ph, "tmp%d" % i, (128, 512), F32) for i in range(2)]
                stb = [sbt(ph, "stb%d" % i, (128, 512), BF16) for i in range(3)]
                stf = [sbt(ph, "stf%d" % i, (128, 512), F32) for i in range(3)]
                stt = [sbt(ph, "stt%d" % i, (128, 256), BF16) for i in range(3)]
                pvs = [sbt(ph, "pvs%d" % i, (128, 1024), F32) for i in range(4)]
                vnb = [sbt(ph, "vnb%d" % i, (128, 1024), BF16) for i in range(2)]
                lng, b_lng = sbt(ph, "lng", (128, 1024), F32)
                lnb, b_lnb = sbt(ph, "lnb", (128, 1024), F32)
                bst, b_bst = sbt(ph, "bst", (128, 2, 6), F32)
                bag, b_bag = sbt(ph, "bag", (128, 2), F32)
                lfh, b_lfh = sbt(ph, "lfh", (32, 512), BF16)
                lfl, b_lfl = sbt(ph, "lfl", (32, 512), BF16)
                a2s, b_a2s = sbt(ph, "a2s", (32, 2, 512), F32)
                a2hi, b_a2hi = sbt(ph, "a2hi", (32, 2, 512), BF16)
                a2lo, b_a2lo = sbt(ph, "a2lo", (32, 2, 512), BF16)
                abb, b_abb = sbt(ph, "abb", (128, 2, 512), F32)
                lt = [sbt(ph, "lt%d" % i, (128, 512), F32) for i in range(4)]
                wlf, b_wlf = sbt(ph, "wlf", (128, 16, 32), BF16)
                cnt = {"b": 0, "f": 0, "t": 0}

                S.dma("sp", lng[:], sgu_ln_g[l].partition_broadcast(128), b_lng, writes=[b_lng])
                S.dma("sp", lnb[:], sgu_ln_b[l].partition_broadcast(128), b_lnb, writes=[b_lnb])
                S.op("pool", lambda e: e.memset(a2s[:], 0.0), writes=[b_a2s])
                S.dma("sp", a2s[0:16, 0, :], gla_a2[l, 0], b_a2s, writes=[b_a2s])
                S.dma("sp", a2s[16:32, 1, :], gla_a2[l, 1], b_a2s, writes=[b_a2s])
                S.op("dve", lambda e: e.tensor_copy(a2hi[:], a2s[:]), reads=[b_a2s], writes=[b_a2hi])
                S.op("dve", lambda e: e.tensor_tensor(out=a2lo[:], in0=a2s[:], in1=a2hi[:], op=ALU.subtract), reads=[b_a2s, b_a2hi], writes=[b_a2lo])
                for d_ in range(2):
                    S.dma("sp", abb[:, d_, :], gla_ab[l, d_].partition_broadcast(128), b_abb, writes=[b_abb])

                for b in range(NBLK):
                    g = 0 if b < 4 else 1
                    t0 = b * 512
                    S.dma("sp", xTb[:], xT[:, :, t0:t0 + 512].rearrange("c p t -> p c t"), b_xTb, reads=[S.reg("xT", b)], writes=[b_xTb])
                    norm_mod((sq, rstd, b_rstd, tmps), xTb, b_xTb, hT, b_hT, l, 0, g)
                    stage("p1a")

                    def fm_group(col0, ncols, dst, name, func, scale=1.0, fp32=False):
                        for ct in range(ncols // 256):
                            wt, b_wt = next_wA()
                            load_w(wt[:, :, 0:256], w_in[l][:, col0 + ct * 256:col0 + (ct + 1) * 256].rearrange("(kc p) n -> p kc n", p=128), b_wt)
                            for j in range(2):
                                ch = ct * 2 + j
                                pp, b_pp = bank()
                                for kc in range(16):
                                    S.op("pe", lambda e, wt=wt, j=j, kc=kc, pp=pp: e.matmul(pp[:], wt[:, kc, j * 128:(j + 1) * 128], hT[:, kc, :],
                                                                                         start=(kc == 0), stop=(kc == 15)), reads=[b_wt, b_hT], writes=[b_pp])
                                if fp32:
                                    st_, b_st = stf[cnt["f"] % 3]; cnt["f"] += 1
                                else:
                                    st_, b_st = stb[cnt["b"] % 3]; cnt["b"] += 1
                                S.op("act", lambda e, st_=st_, pp=pp: e.activation(st_[:], pp[:], func, scale=scale), reads=[b_pp], writes=[b_st])
                                S.dma("sp", dst[ch, :, t0:t0 + 512], st_[:], b_st, reads=[b_st], writes=[S.reg(name, ch, b)])

                    def tm_group(col0, ncols, evac):
                        for ct in range(ncols // 256):
                            wt, b_wt = next_wA()
                            load_w(wt[:, :, 0:256], w_in[l][:, col0 + ct * 256:col0 + (ct + 1) * 256].rearrange("(kc p) n -> p kc n", p=128), b_wt)
                            for ts in range(4):
                                pp, b_pp = bank()
                                for kc in range(16):
                                    S.op("pe", lambda e, wt=wt, ts=ts, kc=kc, pp=pp: e.matmul(pp[:, 0:256], hT[:, kc, ts * 128:(ts + 1) * 128], wt[:, kc, 0:256],
                                                                                          start=(kc == 0), stop=(kc == 15)), reads=[b_wt, b_hT], writes=[b_pp])
                                evac(ct, ts, pp, b_pp)

                    def tm_store(dst, name, func):
                        def evac(ct, ts, pp, b_pp):
                            st_, b_st = stt[cnt["t"] % 3]; cnt["t"] += 1
                            S.op("act", lambda e, st_=st_, pp=pp: e.activation(st_[:], pp[:, 0:256], func), reads=[b_pp], writes=[b_st])
                            S.dma("sp", dst[t0 + ts * 128:t0 + (ts + 1) * 128, ct * 256:(ct + 1) * 256], st_[:], b_st, reads=[b_st],
                                  writes=[S.reg(name, ct, b * 4 + ts)])
                        return evac

                    fm_group(0, 1024, puT, "puT", AF.Gelu_apprx_tanh)
                    stage("p1b")

                    def evac_pv(ct, ts, pp, b_pp):
                        pvt, b_pvt = pvs[ts]
                        S.op("act", lambda e, pvt=pvt, pp=pp, ct=ct: e.activation(pvt[:, ct * 256:(ct + 1) * 256], pp[:, 0:256], AF.Gelu_apprx_tanh), reads=[b_pp], writes=[b_pvt])
                    tm_group(1024, 1024, evac_pv)
                    for ts in range(4):
                        pvt, b_pvt = pvs[ts]
                        vb_, b_vb = vnb[ts % 2]
                        for hh in range(2):
                            S.op("dve", lambda e, pvt=pvt, hh=hh: e.bn_stats(bst[:, hh, :], pvt[:, hh * 512:(hh + 1) * 512]), reads=[b_pvt], writes=[b_bst])
                        S.op("dve", lambda e: e.bn_aggr(bag[:], bst[:].rearrange("p a s -> p (a s)")), reads=[b_bst], writes=[b_bag])
                        S.op("act", lambda e: e.activation(bag[:, 1:2], bag[:, 1:2], AF.Sqrt, bias=epsb[:]), reads=[b_bag, b_epsb], writes=[b_bag])
                        S.op("dve", lambda e: e.reciprocal(bag[:, 1:2], bag[:, 1:2]), reads=[b_bag], writes=[b_bag])
                        S.op("dve", lambda e, pvt=pvt: e.tensor_scalar(out=pvt[:], in0=pvt[:], scalar1=bag[:, 0:1], scalar2=bag[:, 1:2], op0=ALU.subtract, op1=ALU.mult),
                             reads=[b_pvt, b_bag], writes=[b_pvt])
                        S.op("dve", lambda e, pvt=pvt: e.tensor_tensor(out=pvt[:], in0=pvt[:], in1=lng[:], op=ALU.mult), reads=[b_pvt, b_lng], writes=[b_pvt])
                        S.op("dve", lambda e, pvt=pvt, vb_=vb_: e.tensor_tensor(out=vb_[:], in0=pvt[:], in1=lnb[:], op=ALU.add), reads=[b_pvt, b_lnb], writes=[b_vb])
                        S.dma("sp", vn_d[t0 + ts * 128:t0 + (ts + 1) * 128, :], vb_[:], b_vb, reads=[b_vb], writes=[S.reg("vn", b * 4 + ts)])

                    stage("p1c")
                    fm_group(2048, 512, qT_d, "qT", AF.Copy, scale=128.0 ** -0.5, fp32=True)
                    fm_group(2560, 512, kT_d, "kT", AF.Copy, fp32=True)
                    tm_group(2560, 512, tm_store(ktm_d, "ktm", AF.Copy))
                    tm_group(3072, 1024, tm_store(vtm_d, "vtm", AF.Copy))
                    tm_group(4096, 1024, tm_store(rtm_d, "rtm", AF.Silu))

                    stage("p1d")
                    load_w(wlf[:], w_in[l][:, 5120:5152].rearrange("(kc p) n -> p kc n", p=128), b_wlf)
                    pp, b_pp = bank()
                    for kc in range(16):
                        S.op("pe", lambda e, kc=kc, pp=pp: e.matmul(pp[0:32, :], wlf[:, kc, :], hT[:, kc, :], start=(kc == 0), stop=(kc == 15)),
                             reads=[b_wlf, b_hT], writes=[b_pp])
                    S.op("dve", lambda e, pp=pp: e.tensor_copy(lfh[:], pp[0:32, :]), reads=[b_pp], writes=[b_lfh])
                    S.op("dve", lambda e, pp=pp: e.tensor_tensor(out=lfl[:], in0=pp[0:32, :], in1=lfh[:], op=ALU.subtract), reads=[b_pp, b_lfh], writes=[b_lfl])
                    for d_ in range(2):
                        for ts in range(4):
                            pp2, b_pp2 = bank()
                            S.op("pe", lambda e, pp2=pp2, ts=ts, d_=d_: e.matmul(pp2[:], lfh[:, ts * 128:(ts + 1) * 128], a2hi[:, d_, :], start=True, stop=False),
                                 reads=[b_lfh, b_a2hi], writes=[b_pp2])
                            S.op("pe", lambda e, pp2=pp2, ts=ts, d_=d_: e.matmul(pp2[:], lfl[:, ts * 128:(ts + 1) * 128], a2hi[:, d_, :], start=False, stop=False),
                                 reads=[b_lfl, b_a2hi], writes=[b_pp2])
                            S.op("pe", lambda e, pp2=pp2, ts=ts, d_=d_: e.matmul(pp2[:], lfh[:, ts * 128:(ts + 1) * 128], a2lo[:, d_, :], start=False, stop=True),
                                 reads=[b_lfh, b_a2lo], writes=[b_pp2])
                            l0, b_l0 = lt[(d_ * 4 + ts) % 2]
                            l2, b_l2 = lt[2 + (d_ * 4 + ts) % 2]
                            S.op("dve", lambda e, pp2=pp2, l0=l0, d_=d_: e.tensor_tensor(out=l0[:], in0=pp2[:], in1=abb[:, d_, :], op=ALU.add), reads=[b_pp2, b_abb], writes=[b_l0])
                            S.op("act", lambda e, l0=l0: e.activation(l0[:], l0[:], AF.Exp, scale=-1.0), reads=[b_l0], writes=[b_l0])
                            S.op("act", lambda e, l0=l0, l2=l2: e.activation(l2[:], l0[:], AF.Ln, bias=onesf[:, 0:1]), reads=[b_l0, b_onesf], writes=[b_l2])
                            S.dma("sp", latm_d[d_, t0 + ts * 128:t0 + (ts + 1) * 128, :], l2[:], b_l2, reads=[b_l2], writes=[S.reg("latm", d_, b * 4 + ts)])

                    stage("p1e")
                    fm_group(5152, 2048, gaT_d, "gaT", AF.Sigmoid)
                    fm_group(7200, 2048, gbT_d, "gbT", AF.Sigmoid)
                S.barrier()
                stage("p1_%d" % l)

            with ExitStack() as ph:
                wsr, b_wsr = sbt(ph, "wsr", (128, 8, 128), F32)
                wsT, b_wsT = sbt(ph, "wsT", (128, 8, 128), BF16)
                bsf, b_bsf = sbt(ph, "bsf", (128, 1024), F32)
                ftm = [sbt(ph, "ftm%d" % i, (128, 4, 128), F32) for i in range(2)]
                vnc = [sbt(ph, "vnc%d" % i, (128, 1024), BF16) for i in range(2)]
                puc = [sbt(ph, "puc%d" % i, (128, 8, 128), BF16) for i in range(2)]
                atc = [sbt(ph, "atc%d" % i, (128, 8, 128), BF16) for i in range(2)]
                S.dma("sp", wsr[:], w_spatial[l].rearrange("g p q -> p g q"), b_wsr, writes=[b_wsr])
                S.dma("sp", bsf[:], b_spatial[l].rearrange("g p -> (g p)").partition_broadcast(128), b_bsf, writes=[b_bsf])
                for g2 in range(2):
                    pt, b_pt = bank()
                    for j in range(4):
                        gg = g2 * 4 + j
                        S.op("pe", lambda e, pt=pt, j=j, gg=gg: e.transpose(pt[:, j * 128:(j + 1) * 128], wsr[:, gg, :], identf[:]), reads=[b_wsr, b_identf], writes=[b_pt])
                    S.op("dve", lambda e, pt=pt, g2=g2: e.tensor_copy(wsT[:, g2 * 4:(g2 + 1) * 4, :], pt[:].rearrange("p (j t) -> p j t", t=128)), reads=[b_pt], writes=[b_wsT])
                for i in range(24):
                    vc, b_vc = vnc[i % 2]
                    pc, b_pc = puc[i % 2]
                    ac, b_ac = atc[i % 2]
                    S.dma("sp", vc[:], vn_d[i * 128:(i + 1) * 128, :], b_vc, reads=[S.reg("vn", i)], writes=[b_vc])
                    S.dma("sp", pc[:], puT[:, :, i * 128:(i + 1) * 128].rearrange("c p t -> p c t"), b_pc, reads=[S.reg("puT", c, i // 4) for c in range(8)], writes=[b_pc])
                    for g2 in range(2):
                        pp, b_pp = bank()
                        for j in range(4):
                            gg = g2 * 4 + j
                            S.op("pe", lambda e, pp=pp, j=j, gg=gg, vc=vc: e.matmul(pp[:, j * 128:(j + 1) * 128], vc[:, gg * 128:(gg + 1) * 128], wsT[:, gg, :], start=True, stop=True),
                                 reads=[b_vc, b_wsT], writes=[b_pp])
                        ft_, b_ft = ftm[g2]
                        S.op("dve", lambda e, pp=pp, g2=g2, ft_=ft_: e.tensor_tensor(out=ft_[:], in0=pp[:].rearrange("p (j t) -> p j t", t=128),
                                                                             in1=bsf[:, g2 * 512:(g2 + 1) * 512].rearrange("p (j t) -> p j t", t=128), op=ALU.add), reads=[b_pp, b_bsf], writes=[b_ft])
                        S.op("pool", lambda e, g2=g2, pc=pc, ac=ac, ft_=ft_: e.tensor_tensor(out=ac[:, g2 * 4:(g2 + 1) * 4, :], in0=ft_[:],
                                                                                    in1=pc[:, g2 * 4:(g2 + 1) * 4, :], op=ALU.mult), reads=[b_ft, b_pc], writes=[b_ac])
                    S.dma("sp", aT_d[:, :, i * 128:(i + 1) * 128].rearrange("c p t -> p c t"), ac[:], b_ac, reads=[b_ac], writes=[S.reg("aT", i)])
                S.barrier()
                stage("p2_%d" % l)

            with ExitStack() as ph:
                Sf, b_Sf = sbt(ph, "Sf", (128, 4, 256), F32)
                Sb, b_Sb = sbt(ph, "Sb", (128, 4, 256), BF16)
                gng, b_gng = sbt(ph, "gng", (64, 1024), F32)
                qTc = [sbt(ph, "qTc%d" % i, (128, 4, 64), F32) for i in range(2)]
                kTc = [sbt(ph, "kTc%d" % i, (128, 4, 64), F32) for i in range(2)]
                ktc = [sbt(ph, "ktc%d" % i, (64, 512), BF16) for i in range(2)]
                vtc = [sbt(ph, "vtc%d" % i, (64, 1024), BF16) for i in range(2)]
                lac = [sbt(ph, "lac%d" % i, (64, 512), F32) for i in range(2)]
                lah = [sbt(ph, "lah%d" % i, (64, 512), BF16) for i in range(2)]
                lal = [sbt(ph, "lal%d" % i, (64, 512), BF16) for i in range(2)]
                rtc = [sbt(ph, "rtc%d" % i, (64, 1024), BF16) for i in range(2)]
                ofc = [sbt(ph, "ofc%d" % i, (64, 1024), F32) for i in range(2)]
                E1 = [sbt(ph, "E1%d" % i, (128, 4, 64), F32) for i in range(2)]
                E2 = [sbt(ph, "E2%d" % i, (128, 4, 64), F32) for i in range(2)]
                qd = [sbt(ph, "qd%d" % i, (128, 4, 64), BF16) for i in range(2)]
                kd = [sbt(ph, "kd%d" % i, (128, 4, 64), BF16) for i in range(2)]
                EK = [sbt(ph, "EK%d" % i, (64, 512), F32) for i in range(2)]
                kend = [sbt(ph, "kend%d" % i, (64, 512), BF16) for i in range(2)]
                attm = [sbt(ph, "attm%d" % i, (64, 4, 64), BF16) for i in range(2)]
                osb = [sbt(ph, "osb%d" % i, (64, 1024), F32) for i in range(2)]
                ot2 = [sbt(ph, "ot2%d" % i, (64, 1024), F32) for i in range(2)]
                ogb = [sbt(ph, "ogb%d" % i, (64, 1024), BF16) for i in range(2)]
                oTc = [sbt(ph, "oTc%d" % i, (128, 8, 64), BF16) for i in range(2)]
                ssq = [sbt(ph, "ssq%d" % i, (64, 4), F32) for i in range(2)]
                junk, b_junk = sbt(ph, "junk", (64, 256), BF16)
                S.dma("sp", gng[:], gla_norm_g[l].partition_broadcast(64), b_gng, writes=[b_gng])
                it = [0]
                for si, (s0, L, grp) in enumerate(SEQS):
                    N = L // 64
                    for dr in range(2):
                        if grp == 0:
                            S.dma("sp", Sf[:], st_in[l, dr].rearrange("h d e -> d h e"), b_Sf, writes=[b_Sf])
                        else:
                            S.op("pool", lambda e: e.memset(Sf[:], 0.0), writes=[b_Sf])
                        S.op("act", lambda e: e.activation(Sb[:], Sf[:], AF.Copy), reads=[b_Sf], writes=[b_Sb])
                        order = range(N) if dr == 0 else range(N - 1, -1, -1)
                        for n in order:
                            k_ = it[0] % 2
                            it[0] += 1
                            tk = s0 + n * 64
                            c64 = tk // 64
                            b5 = tk // 512
                            t128 = tk // 128
                            q_, b_q = qTc[k_]; kk_, b_k = kTc[k_]; kt_, b_kt = ktc[k_]; vt_, b_vt = vtc[k_]; la_, b_la = lac[k_]
                            S.dma("sp", q_[:], qT_d[:, :, tk:tk + 64].rearrange("h p t -> p h t"), b_q, reads=[S.reg("qT", h, b5) for h in range(4)], writes=[b_q])
                            S.dma("sp", kk_[:], kT_d[:, :, tk:tk + 64].rearrange("h p t -> p h t"), b_k, reads=[S.reg("kT", h, b5) for h in range(4)], writes=[b_k])
                            S.dma("sp", kt_[:], ktm_d[tk:tk + 64, :], b_kt, reads=[S.reg("ktm", c, t128) for c in range(2)], writes=[b_kt])
                            S.dma("sp", vt_[:], vtm_d[tk:tk + 64, :], b_vt, reads=[S.reg("vtm", c, t128) for c in range(4)], writes=[b_vt])
                            S.dma("sp", la_[:], latm_d[dr, tk:tk + 64, :], b_la, reads=[S.reg("latm", dr, t128)], writes=[b_la])
                            lah_, b_lah = lah[k_]; lal_, b_lal = lal[k_]
                            S.op("dve", lambda e, lah_=lah_, la_=la_: e.tensor_copy(lah_[:], la_[:]), reads=[b_la], writes=[b_lah])
                            S.op("pool", lambda e, lal_=lal_, lah_=lah_, la_=la_: e.tensor_tensor(out=lal_[:], in0=la_[:], in1=lah_[:], op=ALU.subtract), reads=[b_la, b_lah], writes=[b_lal])
                            p1, b_p1 = bank()
                            for h in range(4):
                                S.op("pe", lambda e, p1=p1, h=h, lah_=lah_, dr=dr: e.matmul(p1[:, h * 64:(h + 1) * 64], lah_[:, h * 128:(h + 1) * 128], triSb[:, dr, :], start=True, stop=False),
                                     reads=[b_lah, b_triSb], writes=[b_p1])
                                S.op("pe", lambda e, p1=p1, h=h, lal_=lal_, dr=dr: e.matmul(p1[:, h * 64:(h + 1) * 64], lal_[:, h * 128:(h + 1) * 128], triSb[:, dr, :], start=False, stop=True),
                                     reads=[b_lal, b_triSb], writes=[b_p1])
                            p2, b_p2 = bank()
                            S.op("pe", lambda e, p2=p2, lah_=lah_, dr=dr: e.matmul(p2[0:64, :], triSb[:, 2 + dr, :], lah_[:], start=True, stop=False), reads=[b_lah, b_triSb], writes=[b_p2])
                            S.op("pe", lambda e, p2=p2, lal_=lal_, dr=dr: e.matmul(p2[0:64, :], triSb[:, 2 + dr, :], lal_[:], start=False, stop=True), reads=[b_lal, b_triSb], writes=[b_p2])
                            e1, b_e1 = E1[k_]; e2, b_e2 = E2[k_]; ek, b_ek = EK[k_]
                            S.op("act", lambda e, p1=p1, e1=e1: e.activation(e1[:], p1[:, 0:256].rearrange("p (h c) -> p h c", c=64), AF.Exp), reads=[b_p1], writes=[b_e1])
                            S.op("act", lambda e, p1=p1, e2=e2: e.activation(e2[:], p1[:, 0:256].rearrange("p (h c) -> p h c", c=64), AF.Exp, scale=-1.0), reads=[b_p1], writes=[b_e2])
                            S.op("act", lambda e, p2=p2, ek=ek: e.activation(ek[:], p2[0:64, :], AF.Exp), reads=[b_p2], writes=[b_ek])
                            qd_, b_qd = qd[k_]; kd_, b_kd = kd[k_]; ke_, b_ke = kend[k_]
                            S.op("dve", lambda e, qd_=qd_, q_=q_, e1=e1: e.tensor_tensor(out=qd_[:], in0=q_[:], in1=e1[:], op=ALU.mult), reads=[b_q, b_e1], writes=[b_qd])
                            S.op("dve", lambda e, kd_=kd_, kk_=kk_, e2=e2: e.tensor_tensor(out=kd_[:], in0=kk_[:], in1=e2[:], op=ALU.mult), reads=[b_k, b_e2], writes=[b_kd])
                            S.op("pool", lambda e, ke_=ke_, kt_=kt_, ek=ek: e.tensor_tensor(out=ke_[:], in0=kt_[:], in1=ek[:], op=ALU.mult), reads=[b_kt, b_ek], writes=[b_ke])
                            p3, b_p3 = bank()
                            for h in range(4):
                                S.op("pe", lambda e, p3=p3, h=h, kd_=kd_, qd_=qd_: e.matmul(p3[0:64, h * 64:(h + 1) * 64], kd_[:, h, :], qd_[:, h, :], start=True, stop=True),
                                     reads=[b_kd, b_qd], writes=[b_p3])
                            am_, b_am = attm[k_]
                            S.op("dve", lambda e, p3=p3, am_=am_, dr=dr: e.tensor_tensor(out=am_[:], in0=p3[0:64, 0:256].rearrange("p (h c) -> p h c", c=64), in1=triM[:, dr, :, :], op=ALU.mult),
                                 reads=[b_p3, b_triM], writes=[b_am])
                            po = [bank(), bank()]
                            for h in range(4):
                                pt_, b_pt_ = po[h // 2]
                                hh = h % 2
                                S.op("pe", lambda e, pt_=pt_, hh=hh, h=h, am_=am_, vt_=vt_: e.matmul(pt_[0:64, hh * 256:(hh + 1) * 256], am_[:, h, :], vt_[:, h * 256:(h + 1) * 256], start=True, stop=False),
                                     reads=[b_am, b_vt], writes=[b_pt_])
                                S.op("pe", lambda e, pt_=pt_, hh=hh, h=h, qd_=qd_: e.matmul(pt_[0:64, hh * 256:(hh + 1) * 256], qd_[:, h, :], Sb[:, h, :], start=False, stop=True),
                                     reads=[b_qd, b_Sb], writes=[b_pt_])
                            pd = [bank(), bank()]
                            for h in range(4):
                                pt_, b_pt_ = pd[h // 2]
                                hh = h % 2
                                S.op("pe", lambda e, pt_=pt_, hh=hh, h=h, ke_=ke_, vt_=vt_: e.matmul(pt_[:, hh * 256:(hh + 1) * 256], ke_[:, h * 128:(h + 1) * 128], vt_[:, h * 256:(h + 1) * 256], start=True, stop=True),
                                     reads=[b_ke, b_vt], writes=[b_pt_])
                            gcol = 63 if dr == 0 else 0
                            for h in range(4):
                                pt_, b_pt_ = pd[h // 2]
                                hh = h % 2
                                S.op("dve", lambda e, pt_=pt_, hh=hh, h=h, e1=e1, gcol=gcol: e.scalar_tensor_tensor(out=Sf[:, h, :], in0=Sf[:, h, :], scalar=e1[:, h, gcol:gcol + 1],
                                                                                                     in1=pt_[:, hh * 256:(hh + 1) * 256], op0=ALU.mult, op1=ALU.add),
                                     reads=[b_Sf, b_e1, b_pt_], writes=[b_Sf])
                            S.op("act", lambda e: e.activation(Sb[:], Sf[:], AF.Copy), reads=[b_Sf], writes=[b_Sb])
                            if dbg and it[0] == 1 and l == 0:
                                for nm, t_, b_, shp, dt_ in (("g_e1", e1, b_e1, [128, 4, 64], F32), ("g_ek", ek, b_ek, [64, 512], F32), ("g_kend", ke_, b_ke, [64, 512], BF16),
                                                             ("g_attm", am_, b_am, [64, 4, 64], BF16), ("g_qd", qd_, b_qd, [128, 4, 64], BF16), ("g_kd", kd_, b_kd, [128, 4, 64], BF16),
                                                             ("g_S", Sf, b_Sf, [128, 4, 256], F32), ("g_la", la_, b_la, [64, 512], F32),
                                                             ("g_tri", triS, b_triS, [64, 4, 64], F32), ("g_trib", triSb, b_triSb, [64, 4, 64], BF16),
                                                             ("g_lah", lah_, b_lah, [64, 512], BF16), ("g_lal", lal_, b_lal, [64, 512], BF16), ("g_triM", triM, b_triM, [64, 2, 4, 64], F32)):
                                    dd = nc.dram_tensor(nm, shp, dt_, kind="ExternalOutput").ap()
                                    S.dma("sp", dd, t_[:], b_, reads=[b_])
                            if dr == 0:
                                os_, b_os = osb[k_]
                                for hb in range(2):
                                    pt_, b_pt_ = po[hb]
                                    S.op("act" if hb else "dve", (lambda e, pt_=pt_, hb=hb, os_=os_: e.activation(os_[:, hb * 512:(hb + 1) * 512], pt_[0:64, :], AF.Copy)) if hb else
                                         (lambda e, pt_=pt_, hb=hb, os_=os_: e.tensor_copy(os_[:, hb * 512:(hb + 1) * 512], pt_[0:64, :])), reads=[b_pt_], writes=[b_os])
                                S.dma("sp", of_d[tk:tk + 64, :], os_[:], b_os, reads=[b_os], writes=[S.reg("of", c64)])
                            else:
                                of_, b_of = ofc[k_]; rt_, b_rt = rtc[k_]
                                S.dma("sp", of_[:], of_d[tk:tk + 64, :], b_of, reads=[S.reg("of", c64)], writes=[b_of])
                                S.dma("sp", rt_[:], rtm_d[tk:tk + 64, :], b_rt, reads=[S.reg("rtm", c, t128) for c in range(4)], writes=[b_rt])
                                os_, b_os = osb[k_]
                                for hb in range(2):
                                    pt_, b_pt_ = po[hb]
                                    S.op("dve", lambda e, pt_=pt_, hb=hb, os_=os_, of_=of_: e.tensor_tensor(out=os_[:, hb * 512:(hb + 1) * 512], in0=pt_[0:64, :], in1=of_[:, hb * 512:(hb + 1) * 512], op=ALU.add),
                                         reads=[b_pt_, b_of], writes=[b_os])
                                ss_, b_ss = ssq[k_]
                                for h in range(4):
                                    S.op("act", lambda e, os_=os_, h=h, ss_=ss_: e.activation(junk[:], os_[:, h * 256:(h + 1) * 256], AF.Square, accum_out=ss_[:, h:h + 1]),
                                         reads=[b_os], writes=[b_junk, b_ss])
                                S.op("act", lambda e, ss_=ss_: e.activation(ss_[:], ss_[:], AF.Sqrt, scale=1.0 / 256.0, bias=epsb[0:64, :]), reads=[b_ss, b_epsb], writes=[b_ss])
                                S.op("dve", lambda e, ss_=ss_: e.reciprocal(ss_[:], ss_[:]), reads=[b_ss], writes=[b_ss])
                                o2_, b_o2 = ot2[k_]
                                for h in range(4):
                                    S.op("dve", lambda e, os_=os_, h=h, ss_=ss_, o2_=o2_: e.scalar_tensor_tensor(out=o2_[:, h * 256:(h + 1) * 256], in0=os_[:, h * 256:(h + 1) * 256], scalar=ss_[:, h:h + 1],
                                                                                                         in1=gng[:, h * 256:(h + 1) * 256], op0=ALU.mult, op1=ALU.mult),
                                         reads=[b_os, b_ss, b_gng], writes=[b_o2])
                                og_, b_og = ogb[k_]
                                S.op("pool", lambda e, og_=og_, o2_=o2_, rt_=rt_: e.tensor_tensor(out=og_[:], in0=o2_[:], in1=rt_[:], op=ALU.mult), reads=[b_o2, b_rt], writes=[b_og])
                                ptr, b_ptr = bank()
                                ptb = ptr[:].bitcast(BF16)
                                for c in range(8):
                                    S.op("pe", lambda e, ptb=ptb, c=c, og_=og_: e.transpose(ptb[:, c * 64:(c + 1) * 64], og_[:, c * 128:(c + 1) * 128], identb[0:64, 0:64]),
                                         reads=[b_og, b_identb], writes=[b_ptr])
                                oc_, b_oc = oTc[k_]
                                S.op("dve", lambda e, ptb=ptb, oc_=oc_: e.tensor_copy(oc_[:], ptb[:, 0:512].rearrange("p (c t) -> p c t", t=64)), reads=[b_ptr], writes=[b_oc])
                                S.dma("sp", oT_d[:, :, tk:tk + 64].rearrange("c p t -> p c t"), oc_[:], b_oc, reads=[b_oc], writes=[S.reg("oT", c64)])
                        if grp == 1:
                            S.dma("sp", ns_out[si - 1, l, dr].rearrange("h d e -> d h e"), Sf[:], b_Sf, reads=[b_Sf])
                S.barrier()
                stage("p3_%d" % l)

            with ExitStack() as ph:
                is_moe = (l % 2 == 1)
                jl = l // 2
                xTb, b_xTb = sbt(ph, "xTb", (128, 16, 512), F32)
                hT, b_hT = sbt(ph, "hT", (128, 16, 512), BF16)
                gT, b_gT = sbt(ph, "gT", (128, NFF, 512), BF16)
                mTb, b_mTb = gT, b_gT
                aTb, b_aTb = hT[:, 0:8, :], b_hT
                oTb, b_oTb = hT[:, 8:16, :], b_hT
                gab = [sbt(ph, "gab%d" % i, (128, 512), BF16) for i in range(2)]
                gbb = [sbt(ph, "gbb%d" % i, (128, 512), BF16) for i in range(2)]
                sq = [sbt(ph, "sq%d" % i, (128, 512), BF16) for i in range(2)]
                rstd, b_rstd = sbt(ph, "rstd", (128, 512), F32)
                tmps = [sbt(ph, "tmp%d" % i, (128, 512), F32) for i in range(2)]
                t1 = [sbt(ph, "t1%d" % i, (128, 512), BF16) for i in range(2)]
                t2 = [sbt(ph, "t2%d" % i, (128, 512), BF16) for i in range(2)]
                sil = [sbt(ph, "sil%d" % i, (128, 512), BF16) for i in range(2)]
                u3s = [sbt(ph, "u3s%d" % i, (128, 512), BF16) for i in range(2)]
                w2t = [sbt(ph, "w2t%d" % i, (128, 2, 512), BF16) for i in range(3)]
                if is_moe:
                    rwf, b_rwf = sbt(ph, "rwf", (128, 16, NE), F32)
                    rwh, b_rwh = sbt(ph, "rwh", (128, 16, NE), BF16)
                    rwl, b_rwl = sbt(ph, "rwl", (128, 16, NE), BF16)
                    hlo = [sbt(ph, "hlo%d" % i, (128, 512), BF16) for i in range(2)]
                    selEb, b_selEb = selE, b_selE
                    cmbTh, b_cmbTh = sbt(ph, "cmbTh", (NE, 512), BF16)
                    rbb, b_rbb = sbt(ph, "rbb", (128, NE), F32)
                    lgT, b_lgT = sbt(ph, "lgT", (NE, 512), F32)
                    lg, b_lg = sbt(ph, "lg", (128, NE), F32)
                    mk1, b_mk1 = sbt(ph, "mk1", (128, NE), F32)
                    mk2, b_mk2 = sbt(ph, "mk2", (128, NE), F32)
                    m12, b_m12 = sbt(ph, "m12", (128, 4), F32)
                    cmb, b_cmb = sbt(ph, "cmb", (128, NE), F32)
                    cmbT, b_cmbT = sbt(ph, "cmbT", (NE, 512), F32)
                    cbes = [sbt(ph, "cbe%d" % i, (128, 512), BF16) for i in range(2)]
                    S.dma("sp", rwf[:], moe_router[jl].rearrange("(kc p) e -> p kc e", p=128), b_rwf, writes=[b_rwf])
                    S.dma("sp", rbb[:], moe_router_b[jl].partition_broadcast(128), b_rbb, writes=[b_rbb])
                    S.op("dve", lambda e: e.tensor_copy(rwh[:], rwf[:]), reads=[b_rwf], writes=[b_rwh])
                    S.op("dve", lambda e: e.tensor_tensor(out=rwl[:], in0=rwf[:], in1=rwh[:], op=ALU.subtract), reads=[b_rwf, b_rwh], writes=[b_rwl])

                w2i = [0]
                for b in range(NBLK):
                    g = 0 if b < 4 else 1
                    t0 = b * 512
                    S.dma("sp", xTb[:], xT[:, :, t0:t0 + 512].rearrange("c p t -> p c t"), b_xTb, reads=[S.reg("xT", b)], writes=[b_xTb])
                    S.dma("sp", aTb, aT_d[:, :, t0:t0 + 512].rearrange("c p t -> p c t"), b_aTb, reads=[S.reg("aT", 4 * b + i) for i in range(4)], writes=[b_aTb])
                    S.dma("sp", oTb, oT_d[:, :, t0:t0 + 512].rearrange("c p t -> p c t"), b_oTb, reads=[S.reg("oT", 8 * b + i) for i in range(8)], writes=[b_oTb])
                    for ct in range(8):
                        wt, b_wt = next_wA()
                        load_w(wt[:, 0:8, 0:256], w_branch_a[l][:, ct * 256:(ct + 1) * 256].rearrange("(kc p) n -> p kc n", p=128), b_wt)
                        load_w(wt[:, 8:16, 0:256], w_branch_b[l][:, ct * 256:(ct + 1) * 256].rearrange("(kc p) n -> p kc n", p=128), b_wt)
                        for j in range(2):
                            oc = ct * 2 + j
                            ga_, b_ga = gab[oc % 2]; gb_, b_gb = gbb[oc % 2]
                            S.dma("sp", ga_[:], gaT_d[oc, :, t0:t0 + 512], b_ga, reads=[S.reg("gaT", oc, b)], writes=[b_ga])
                            S.dma("sp", gb_[:], gbT_d[oc, :, t0:t0 + 512], b_gb, reads=[S.reg("gbT", oc, b)], writes=[b_gb])
                            pa, b_pa = bank()
                            pb_, b_pb = bank()
                            for kc in range(8):
                                S.op("pe", lambda e, pa=pa, wt=wt, kc=kc, j=j: e.matmul(pa[:], wt[:, kc, j * 128:(j + 1) * 128], aTb[:, kc, :], start=(kc == 0), stop=(kc == 7)),
                                     reads=[b_wt, b_aTb], writes=[b_pa])
                            for kc in range(8):
                                S.op("pe", lambda e, pb_=pb_, wt=wt, kc=kc, j=j: e.matmul(pb_[:], wt[:, 8 + kc, j * 128:(j + 1) * 128], oTb[:, kc, :], start=(kc == 0), stop=(kc == 7)),
                                     reads=[b_wt, b_oTb], writes=[b_pb])
                            ta, b_ta = t1[oc % 2]; tb_, b_tb = t2[oc % 2]
                            S.op("dve", lambda e, ta=ta, pa=pa, ga_=ga_: e.tensor_tensor(out=ta[:], in0=pa[:], in1=ga_[:], op=ALU.mult), reads=[b_pa, b_ga], writes=[b_ta])
                            S.op("dve", lambda e, tb_=tb_, pb_=pb_, gb_=gb_: e.tensor_tensor(out=tb_[:], in0=pb_[:], in1=gb_[:], op=ALU.mult), reads=[b_pb, b_gb], writes=[b_tb])
                            S.op("pool", lambda e, ta=ta, tb_=tb_, oc=oc: e.tensor_tensor(out=mTb[:, oc, :], in0=ta[:], in1=tb_[:], op=ALU.add), reads=[b_ta, b_tb], writes=[b_mTb])
                    for ct in range(8):
                        wt, b_wt = next_wA()
                        load_w(wt[:, :, 0:256], w_out[l][:, ct * 256:(ct + 1) * 256].rearrange("(kc p) n -> p kc n", p=128), b_wt)
                        for j in range(2):
                            oc = ct * 2 + j
                            pp, b_pp = bank()
                            for kc in range(16):
                                S.op("pe", lambda e, pp=pp, wt=wt, kc=kc, j=j: e.matmul(pp[:], wt[:, kc, j * 128:(j + 1) * 128], mTb[:, kc, :], start=(kc == 0), stop=(kc == 15)),
                                     reads=[b_wt, b_mTb], writes=[b_pp])
                            S.op("dve", lambda e, pp=pp, oc=oc, l=l, g=g: e.scalar_tensor_tensor(out=xTb[:, oc, :], in0=pp[:], scalar=modT[:, l, 32 + oc, g:g + 1], in1=xTb[:, oc, :], op0=ALU.mult, op1=ALU.add),
                                 reads=[b_pp, b_modT, b_xTb], writes=[b_xTb])
                    if is_moe:
                        plg, b_plg = bank()

                        def h32cb(c, tmp, b_tmp, plg=plg, b_plg=b_plg):
                            hl_, b_hl = hlo[c % 2]
                            S.op("dve", lambda e, c=c, tmp=tmp: e.tensor_copy(hT[:, c, :], tmp[:]), reads=[b_tmp], writes=[b_hT])
                            S.op("dve", lambda e, c=c, tmp=tmp, hl_=hl_: e.tensor_tensor(out=hl_[:], in0=tmp[:], in1=hT[:, c, :], op=ALU.subtract), reads=[b_tmp, b_hT], writes=[b_hl])
                            S.op("pe", lambda e, c=c: e.matmul(plg[0:NE, :], rwh[:, c, :], hT[:, c, :], start=(c == 0), stop=False), reads=[b_hT, b_rwh], writes=[b_plg])
                            S.op("pe", lambda e, c=c, hl_=hl_: e.matmul(plg[0:NE, :], rwh[:, c, :], hl_[:], start=False, stop=False), reads=[b_hl, b_rwh], writes=[b_plg])
                            S.op("pe", lambda e, c=c: e.matmul(plg[0:NE, :], rwl[:, c, :], hT[:, c, :], start=False, stop=(c == 15)), reads=[b_hT, b_rwl], writes=[b_plg])
                        norm_mod((sq, rstd, b_rstd, tmps), xTb, b_xTb, hT, b_hT, l, 1, g, h32cb)
                        S.op("dve", lambda e, plg=plg: e.tensor_copy(lgT[:], plg[0:NE, :]), reads=[b_plg], writes=[b_lgT])
                        for ts in range(4):
                            pq, b_pq = bank()
                            S.op("pe", lambda e, pq=pq, ts=ts: e.transpose(pq[:, 0:NE], lgT[:, ts * 128:(ts + 1) * 128], identf[0:NE, 0:NE]), reads=[b_lgT, b_identf], writes=[b_pq])
                            S.op("dve", lambda e, pq=pq: e.tensor_tensor(out=lg[:], in0=pq[:, 0:NE], in1=rbb[:], op=ALU.add), reads=[b_pq, b_rbb], writes=[b_lg])
                            S.op("dve", lambda e: e.tensor_reduce(out=m12[:, 0:1], in_=lg[:], axis=mybir.AxisListType.X, op=ALU.max), reads=[b_lg], writes=[b_m12])
                            S.op("dve", lambda e: e.tensor_scalar(out=mk1[:], in0=lg[:], scalar1=m12[:, 0:1], scalar2=None, op0=ALU.is_equal), reads=[b_lg, b_m12], writes=[b_mk1])
                            S.op("dve", lambda e: e.scalar_tensor_tensor(out=lg[:], in0=mk1[:], scalar=-1e30, in1=lg[:], op0=ALU.mult, op1=ALU.add), reads=[b_mk1, b_lg], writes=[b_lg])
                            S.op("dve", lambda e: e.tensor_reduce(out=m12[:, 1:2], in_=lg[:], axis=mybir.AxisListType.X, op=ALU.max), reads=[b_lg], writes=[b_m12])
                            S.op("dve", lambda e: e.tensor_scalar(out=mk2[:], in0=lg[:], scalar1=m12[:, 1:2], scalar2=None, op0=ALU.is_equal), reads=[b_lg, b_m12], writes=[b_mk2])
                            S.op("dve", lambda e: e.tensor_tensor(out=m12[:, 2:3], in0=m12[:, 1:2], in1=m12[:, 0:1], op=ALU.subtract), reads=[b_m12], writes=[b_m12])
                            S.op("act", lambda e: e.activation(m12[:, 2:3], m12[:, 2:3], AF.Exp), reads=[b_m12], writes=[b_m12])
                            S.op("dve", lambda e: e.tensor_scalar(out=m12[:, 2:3], in0=m12[:, 2:3], scalar1=1.0, scalar2=None, op0=ALU.add), reads=[b_m12], writes=[b_m12])
                            S.op("dve", lambda e: e.reciprocal(m12[:, 2:3], m12[:, 2:3]), reads=[b_m12], writes=[b_m12])
                            S.op("dve", lambda e: e.tensor_scalar(out=m12[:, 3:4], in0=m12[:, 2:3], scalar1=-1.0, scalar2=1.0, op0=ALU.mult, op1=ALU.add), reads=[b_m12], writes=[b_m12])
                            S.op("dve", lambda e: e.tensor_scalar(out=mk1[:], in0=mk1[:], scalar1=m12[:, 2:3], scalar2=None, op0=ALU.mult), reads=[b_mk1, b_m12], writes=[b_mk1])
                            S.op("dve", lambda e: e.scalar_tensor_tensor(out=cmb[:], in0=mk2[:], scalar=m12[:, 3:4], in1=mk1[:], op0=ALU.mult, op1=ALU.add), reads=[b_mk1, b_mk2, b_m12], writes=[b_cmb])
                            pq2, b_pq2 = bank()
                            S.op("pe", lambda e, pq2=pq2: e.transpose(pq2[0:NE, 0:128], cmb[:], identf[:]), reads=[b_cmb, b_identf], writes=[b_pq2])
                            S.op("dve", lambda e, pq2=pq2, ts=ts: e.tensor_copy(cmbT[:, ts * 128:(ts + 1) * 128], pq2[0:NE, 0:128]), reads=[b_pq2], writes=[b_cmbT])
                        S.op("dve", lambda e: e.tensor_copy(cmbTh[:], cmbT[:]), reads=[b_cmbT], writes=[b_cmbTh])
                        experts = [(moe_w1[jl, ex], moe_w3[jl, ex], moe_w2[jl, ex], ex) for ex in range(NE)]
                    else:
                        norm_mod((sq, rstd, b_rstd, tmps), xTb, b_xTb, hT, b_hT, l, 1, g)
                        experts = [(ffn_w1[jl], ffn_w3[jl], ffn_w2[jl], None)]
                    for (W1, W3, W2, ex) in experts:
                        if ex is not None:
                            cbe, b_cbe = cbes[ex % 2]
                            pq3, b_pq3 = bank()
                            S.op("pe", lambda e, pq3=pq3, ex=ex: e.matmul(pq3[:], selEb[:, ex, :], cmbTh[:], start=True, stop=True), reads=[b_selEb, b_cmbTh], writes=[b_pq3])
                            S.op("act", lambda e, pq3=pq3, cbe=cbe: e.activation(cbe[:], pq3[:], AF.Copy), reads=[b_pq3], writes=[b_cbe])
                        for ct in range(NFF // 4):
                            w1t, b_w1t = next_wA()
                            w3t, b_w3t = next_wA()
                            load_w(w1t[:], W1[:, ct * 512:(ct + 1) * 512].rearrange("(kc p) n -> p kc n", p=128), b_w1t)
                            load_w(w3t[:], W3[:, ct * 512:(ct + 1) * 512].rearrange("(kc p) n -> p kc n", p=128), b_w3t)
                            for j in range(4):
                                fc = ct * 4 + j
                                pu1, b_pu1 = bank()
                                pu3, b_pu3 = bank()
                                for kc in range(16):
                                    S.op("pe", lambda e, pu1=pu1, w1t=w1t, kc=kc, j=j: e.matmul(pu1[:], w1t[:, kc, j * 128:(j + 1) * 128], hT[:, kc, :], start=(kc == 0), stop=(kc == 15)),
                                         reads=[b_w1t, b_hT], writes=[b_pu1])
                                for kc in range(16):
                                    S.op("pe", lambda e, pu3=pu3, w3t=w3t, kc=kc, j=j: e.matmul(pu3[:], w3t[:, kc, j * 128:(j + 1) * 128], hT[:, kc, :], start=(kc == 0), stop=(kc == 15)),
                                         reads=[b_w3t, b_hT], writes=[b_pu3])
                                sl, b_sl = sil[fc % 2]
                                S.op("act", lambda e, sl=sl, pu1=pu1: e.activation(sl[:], pu1[:], AF.Silu), reads=[b_pu1], writes=[b_sl])
                                if ex is None:
                                    S.op("dve", lambda e, sl=sl, pu3=pu3, fc=fc: e.tensor_tensor(out=gT[:, fc, :], in0=pu3[:], in1=sl[:], op=ALU.mult), reads=[b_pu3, b_sl], writes=[b_gT])
                                else:
                                    u3_, b_u3 = u3s[fc % 2]
                                    S.op("dve", lambda e, u3_=u3_, pu3=pu3, cbe=cbe: e.tensor_tensor(out=u3_[:], in0=pu3[:], in1=cbe[:], op=ALU.mult), reads=[b_pu3, b_cbe], writes=[b_u3])
                                    S.op("pool", lambda e, u3_=u3_, sl=sl, fc=fc: e.tensor_tensor(out=gT[:, fc, :], in0=u3_[:], in1=sl[:], op=ALU.mult), reads=[b_u3, b_sl], writes=[b_gT])
                        for pq_ in range(4):
                            accs = [bank() for _ in range(4)]
                            for fc2 in range(NFF // 2):
                                w2_, b_w2 = w2t[w2i[0] % 3]
                                w2i[0] += 1
                                load_w(w2_[:], W2[fc2 * 256:(fc2 + 1) * 256, pq_ * 512:(pq_ + 1) * 512].rearrange("(f p) n -> p f n", p=128), b_w2)
                                for f_ in range(2):
                                    fc = fc2 * 2 + f_
                                    for j in range(4):
                                        pp, b_pp = accs[j]
                                        S.op("pe", lambda e, pp=pp, w2_=w2_, fc=fc, f_=f_, j=j: e.matmul(pp[:], w2_[:, f_, j * 128:(j + 1) * 128], gT[:, fc, :], start=(fc == 0), stop=(fc == NFF - 1)),
                                             reads=[b_w2, b_gT], writes=[b_pp])
                            for j in range(4):
                                oc = pq_ * 4 + j
                                pp, b_pp = accs[j]
                                S.op("dve", lambda e, pp=pp, oc=oc, l=l, g=g: e.scalar_tensor_tensor(out=xTb[:, oc, :], in0=pp[:], scalar=modT[:, l, 80 + oc, g:g + 1], in1=xTb[:, oc, :], op0=ALU.mult, op1=ALU.add),
                                     reads=[b_pp, b_modT, b_xTb], writes=[b_xTb])
                    S.dma("sp", xT[:, :, t0:t0 + 512].rearrange("c p t -> p c t"), xTb[:], b_xTb, reads=[b_xTb], writes=[S.reg("xT", b)])
                S.barrier()
                stage("p45_%d" % l)

        with ExitStack() as ph:
            xTb, b_xTb = sbt(ph, "xTb", (128, 16, 512), F32)
            sq = [sbt(ph, "sq%d" % i, (128, 512), BF16) for i in range(2)]
            rstd, b_rstd = sbt(ph, "rstd", (128, 512), F32)
            yo = [sbt(ph, "yo%d" % i, (128, D), F32) for i in range(2)]
            for b in range(NBLK):
                t0 = b * 512
                S.dma("sp", xTb[:], xT[:, :, t0:t0 + 512].rearrange("c p t -> p c t"), b_xTb, reads=[S.reg("xT", b)], writes=[b_xTb])
                pss, b_pss = bank()
                for c in range(16):
                    sqt, b_sq = sq[c % 2]
                    S.op("act", lambda e, c=c, sqt=sqt: e.activation(sqt[:], xTb[:, c, :], AF.Square), reads=[b_xTb], writes=[b_sq])
                    S.op("pe", lambda e, c=c, sqt=sqt, pss=pss: e.matmul(pss[:], onesb[:], sqt[:], start=(c == 0), stop=(c == 15)), reads=[b_sq, b_onesb], writes=[b_pss])
                S.op("act", lambda e, pss=pss: e.activation(rstd[:], pss[:], AF.Sqrt, scale=1.0 / D, bias=epsb[:]), reads=[b_pss, b_epsb], writes=[b_rstd])
                S.op("dve", lambda e: e.reciprocal(rstd[:], rstd[:]), reads=[b_rstd], writes=[b_rstd])
                for c in range(16):
                    S.op("dve", lambda e, c=c: e.scalar_tensor_tensor(out=xTb[:, c, :], in0=xTb[:, c, :], scalar=fgT[:, c:c + 1], in1=rstd[:], op0=ALU.mult, op1=ALU.mult),
                         reads=[b_xTb, b_fgT, b_rstd], writes=[b_xTb])
                for ts in range(4):
                    yo_, b_yo = yo[ts % 2]
                    for q4 in range(4):
                        pt, b_pt = bank()
                        for j in range(4):
                            c = q4 * 4 + j
                            S.op("pe", lambda e, pt=pt, j=j, c=c, ts=ts: e.transpose(pt[:, j * 128:(j + 1) * 128], xTb[:, c, ts * 128:(ts + 1) * 128], identf[:]),
                                 reads=[b_xTb, b_identf], writes=[b_pt])
                        if q4 % 2:
                            S.op("act", lambda e, pt=pt, q4=q4, yo_=yo_: e.activation(yo_[:, q4 * 512:(q4 + 1) * 512], pt[:], AF.Copy), reads=[b_pt], writes=[b_yo])
                        else:
                            S.op("dve", lambda e, pt=pt, q4=q4, yo_=yo_: e.tensor_copy(yo_[:, q4 * 512:(q4 + 1) * 512], pt[:]), reads=[b_pt], writes=[b_yo])
                    tok = t0 + ts * 128
                    dst = ys_out[tok:tok + 128, :] if tok < 2048 else yp_out[tok - 2048:tok - 2048 + 128, :]
                    S.dma("sp", dst, yo_[:], b_yo, reads=[b_yo])
            S.barrier()
        S.emit()
    return nc, es


_CACHE = {}


def kernel(**inputs):
    n = 8
    if "nc" not in _CACHE:
        _CACHE["nc"] = build()
    nc, _es = _CACHE["nc"]
    f = lambda a: np.ascontiguousarray(np.asarray(a, dtype=np.float32))
    xs = f(inputs["x_sample"])
    xp = f(inputs["x_prompt"])
    st = f(inputs["state_gla"])
    c = f(inputs["c"])
    c_ctx = f(inputs["c_ctx"])
    wnames = ["norm1_g", "norm2_g", "w_mod", "b_mod", "w_in", "sgu_ln_g", "sgu_ln_b", "w_spatial", "b_spatial", "gla_a2", "gla_ab",
              "gla_norm_g", "w_branch_a", "w_branch_b", "w_out", "ffn_w1", "ffn_w3", "ffn_w2", "moe_router", "moe_router_b",
              "moe_w1", "moe_w3", "moe_w2", "final_g"]
    wd = {k: f(inputs[k]) for k in wnames}
    in_maps = []
    for i in range(n):
        m = dict(wd)
        m["xs"] = xs[i]
        m["xp"] = np.ascontiguousarray(xp[4 * i:4 * i + 4].reshape(1024, D))
        m["st"] = st[i]
        m["cc"] = np.ascontiguousarray(np.stack([c[i], c_ctx], axis=0))
        in_maps.append(m)
    res = run_bass_kernel_spmd(nc, in_maps, core_ids=list(range(n)))
    R = res.results
    y_sample = np.stack([R[i]["ys"] for i in range(n)], axis=0)
    y_prompt = np.concatenate([R[i]["yp"].reshape(4, 256, D) for i in range(n)], axis=0)
    new_state = np.concatenate([R[i]["ns"] for i in range(n)], axis=0)
    return (y_prompt.astype(np.float32), y_sample.astype(np.float32), new_state.astype(np.float32))
```

```python
import math
import numpy as np
from contextlib import ExitStack
import concourse.bass as bass
import concourse.mybir as mybir
from concourse.bass_utils import run_bass_kernel_spmd

F32 = mybir.dt.float32
BF16 = mybir.dt.bfloat16
I32 = mybir.dt.int32
AF = mybir.ActivationFunctionType
ALU = mybir.AluOpType

D = 2048
DEPTH = 4
T = 3072
NBLK = 6
D_A = 1024
DFF = 5632
NFF = 44
NE = 8
EPS = 1e-6
N_IN = 9248
SEQS = [(0, 2048, 0)] + [(2048 + 256 * i, 256, 1) for i in range(4)]


class SemH:
    def __init__(self, h):
        self.h = h
        self.count = 0


class Buf:
    __slots__ = ("name", "last_w", "readers", "sem")

    def __init__(self, name):
        self.name = name
        self.last_w = None
        self.readers = []
        self.sem = None


class Op:
    __slots__ = ("eng", "fn", "deps", "needed", "is_dma", "tok")

    def __init__(self, eng, fn, is_dma=False):
        self.eng = eng
        self.fn = fn
        self.deps = []
        self.needed = False
        self.is_dma = is_dma
        self.tok = None


class Sched:
    ENGS = ("pe", "act", "dve", "pool", "sp")

    def __init__(self, nc, es):
        self.nc = nc
        self.es = es
        self.ops = {e: [] for e in self.ENGS}
        self.touched = {}
        self.cur_bar = None
        self.free_sems = []
        self.nsem = 0
        self.regs = {}
        self.phase_sems = []
        self.dead = False

    def buf(self, name):
        b = Buf(name)
        b.last_w = self.cur_bar
        return b

    def reg(self, *key):
        b = self.regs.get(key)
        if b is None:
            b = self.buf(str(key))
            self.regs[key] = b
        return b

    def _getsem(self):
        if self.free_sems:
            return self.free_sems.pop(0)
        self.nsem += 1
        return SemH(self.es.enter_context(self.nc.semaphore("ds%d" % self.nsem)))

    def _add(self, op, reads, writes):
        if self.dead:
            return op
        deps = []
        for b in reads:
            if b.last_w is not None:
                deps.append(b.last_w)
        for b in writes:
            deps.extend(b.readers)
            if b.last_w is not None:
                deps.append(b.last_w)
        seen = set()
        for d in deps:
            if d is op or id(d) in seen:
                continue
            seen.add(id(d))
            if d.eng == "pe" and op.eng == "pe" and not d.is_dma and not op.is_dma:
                continue
            op.deps.append(d)
            d.needed = True
        for b in reads:
            b.readers.append(op)
            self.touched[id(b)] = b
        for b in writes:
            b.readers = []
            b.last_w = op
            self.touched[id(b)] = b
        self.ops[op.eng].append(op)
        return op

    def op(self, eng, fn, reads=(), writes=()):
        return self._add(Op(eng, fn), list(reads), list(writes))

    def dma(self, eng, out, in_, sb, reads=(), writes=(), **kw):
        def fn(e):
            return e.dma_start(out=out, in_=in_, **kw)
        o = Op(eng, fn, is_dma=True)
        o.needed = True
        if self.dead:
            return o
        if sb.sem is None:
            sb.sem = self._getsem()
            self.phase_sems.append(sb)
        sb.sem.count += 16
        o.tok = (sb.sem.h, sb.sem.count)
        return self._add(o, list(reads), list(writes))

    def barrier(self):
        if self.dead:
            return None
        allb = list(self.touched.values())
        o = self.op("sp", lambda e: e.nop(), reads=[], writes=allb)
        o.needed = True
        self.cur_bar = o
        self.touched = {}
        for b in self.phase_sems:
            self.free_sems.append(b.sem)
            b.sem = None
        self.phase_sems = []
        return o

    def emit(self):
        nc = self.nc
        engmap = {"pe": "tensor", "act": "scalar", "dve": "vector", "pool": "gpsimd", "sp": "sync"}
        ROT = 30000
        for e in self.ENGS:
            cnt = 0
            sem = None
            k = 0
            for o in self.ops[e]:
                if o.is_dma:
                    continue
                if o.needed:
                    if sem is None or cnt >= ROT:
                        sem = self.es.enter_context(nc.semaphore("es_%s_%d" % (e, k)))
                        k += 1
                        cnt = 0
                    cnt += 1
                    o.tok = (sem, cnt)
        block = self.es.enter_context(nc.Block())
        sched = self

        def make(ename):
            def body(eng):
                waited = {}
                for o in sched.ops[ename]:
                    for d in o.deps:
                        s, v = d.tok
                        k = id(s)
                        if waited.get(k, 0) >= v:
                            continue
                        waited[k] = v
                        eng.wait_ge(s, v)
                    ins = o.fn(eng)
                    if o.is_dma:
                        ins.then_inc(o.tok[0], 16)
                    elif o.needed:
                        ins.then_inc(o.tok[0], 1)
            return body

        for ename in self.ENGS:
            getattr(block, engmap[ename])(make(ename))


class _Stop(Exception):
    pass


def build(n_layers=DEPTH, stop=None, dbg=False):
    nc = bass.Bass("TRN2", target_bir_lowering=False)
    es = ExitStack()
    S = Sched(nc, es)

    def stage(name):
        if stop == name and not S.dead:
            S.barrier()
            S.dead = True

    def din(name, shape):
        return nc.dram_tensor(name, list(shape), F32, kind="ExternalInput").ap()

    def dout(name, shape):
        return nc.dram_tensor(name, list(shape), F32, kind="ExternalOutput").ap()

    def dscr(name, shape, dt):
        if dbg:
            return nc.dram_tensor(name, list(shape), dt, kind="ExternalOutput").ap()
        return nc.dram_tensor(name, list(shape), dt).ap()

    xs_in = din("xs", (2048, D))
    xp_in = din("xp", (1024, D))
    st_in = din("st", (DEPTH, 2, 4, 128, 256))
    cc_in = din("cc", (2, D))
    norm1_g = din("norm1_g", (DEPTH, D))
    norm2_g = din("norm2_g", (DEPTH, D))
    w_mod = din("w_mod", (DEPTH, D, 6 * D))
    b_mod = din("b_mod", (DEPTH, 6 * D))
    w_in = din("w_in", (DEPTH, D, N_IN))
    sgu_ln_g = din("sgu_ln_g", (DEPTH, D_A))
    sgu_ln_b = din("sgu_ln_b", (DEPTH, D_A))
    w_spatial = din("w_spatial", (DEPTH, 8, 128, 128))
    b_spatial = din("b_spatial", (DEPTH, 8, 128))
    gla_a2 = din("gla_a2", (DEPTH, 2, 16, 512))
    gla_ab = din("gla_ab", (DEPTH, 2, 512))
    gla_norm_g = din("gla_norm_g", (DEPTH, 1024))
    w_branch_a = din("w_branch_a", (DEPTH, 1024, D))
    w_branch_b = din("w_branch_b", (DEPTH, 1024, D))
    w_out = din("w_out", (DEPTH, D, D))
    ffn_w1 = din("ffn_w1", (2, D, DFF))
    ffn_w3 = din("ffn_w3", (2, D, DFF))
    ffn_w2 = din("ffn_w2", (2, DFF, D))
    moe_router = din("moe_router", (2, D, NE))
    moe_router_b = din("moe_router_b", (2, NE))
    moe_w1 = din("moe_w1", (2, NE, D, DFF))
    moe_w3 = din("moe_w3", (2, NE, D, DFF))
    moe_w2 = din("moe_w2", (2, NE, DFF, D))
    final_g = din("final_g", (D,))
    ys_out = dout("ys", (2048, D))
    yp_out = dout("yp", (1024, D))
    ns_out = dout("ns", (4, DEPTH, 2, 4, 128, 256))

    xT = dscr("xT", (16, 128, T), F32)
    puT = dscr("puT", (8, 128, T), BF16)
    vn_d = dscr("vn", (T, 1024), BF16)
    qT_d = dscr("qT", (4, 128, T), F32)
    kT_d = dscr("kT", (4, 128, T), F32)
    ktm_d = dscr("ktm", (T, 512), BF16)
    vtm_d = dscr("vtm", (T, 1024), BF16)
    rtm_d = dscr("rtm", (T, 1024), BF16)
    latm_d = dscr("latm", (2, T, 512), F32)
    gaT_d = dscr("gaT", (16, 128, T), BF16)
    gbT_d = dscr("gbT", (16, 128, T), BF16)
    aT_d = dscr("aT", (8, 128, T), BF16)
    oT_d = dscr("oT", (8, 128, T), BF16)
    of_d = dscr("of", (T, 1024), F32)

    _uid = [0]

    def sbt(stack, name, shape, dt):
        _uid[0] += 1
        name = "%s_%d" % (name, _uid[0])
        t = stack.enter_context(nc.sbuf_tensor(name, list(shape), dt))
        return t, S.buf(name)

    pbk = []
    for i in range(8):
        t = es.enter_context(nc.psum_tensor("pb%d" % i, [128, 512], F32))
        pbk.append((t, S.buf("pb%d" % i)))
    bank_i = [0]

    def bank():
        r = pbk[bank_i[0] % 8]
        bank_i[0] += 1
        return r

    identb, b_identb = sbt(es, "identb", (128, 128), BF16)
    identf, b_identf = sbt(es, "identf", (128, 128), F32)
    onesb, b_onesb = sbt(es, "onesb", (128, 128), BF16)
    onesf, b_onesf = sbt(es, "onesf", (128, 128), F32)
    triS, b_triS = sbt(es, "triS", (64, 4, 64), F32)
    triM, b_triM = sbt(es, "triM", (64, 2, 4, 64), F32)
    selE, b_selE = sbt(es, "selE", (8, 8, 128), BF16)
    modT, b_modT = sbt(es, "modT", (128, DEPTH, 96, 2), F32)
    bmodT, b_bmodT = sbt(es, "bmodT", (128, DEPTH, 96), F32)
    nT, b_nT = sbt(es, "nT", (128, 2, DEPTH, 16), F32)
    fgT, b_fgT = sbt(es, "fgT", (128, 16), F32)
    gsc, b_gsc = sbt(es, "gsc", (128, DEPTH, 2, 16, 2), F32)
    scT, b_scT = sbt(es, "scT", (128, 16, 2), BF16)

    def mk_tri(dst, pattern_sign, cm, op, scale_val):
        S.op("pool", lambda e: e.memset(dst, scale_val), writes=[])
        S.op("pool", lambda e: e.affine_select(out=dst, in_=dst, pattern=[[pattern_sign, 64]], compare_op=op,
                                              fill=0.0, base=0, channel_multiplier=cm), reads=[], writes=[])

    def cst(fn, w):
        S.op("pool", fn, reads=w, writes=w)

    cst(lambda e: e.memset(identf[:], 0.0), [b_identf])
    cst(lambda e: e.affine_select(out=identf[:], in_=identf[:], pattern=[[-1, 128]], compare_op=ALU.not_equal,
                                  fill=1.0, base=0, channel_multiplier=1), [b_identf])
    S.op("dve", lambda e: e.tensor_copy(identb[:], identf[:]), reads=[b_identf], writes=[b_identb])
    cst(lambda e: e.memset(onesf[:], 1.0), [b_onesf])
    cst(lambda e: e.memset(onesb[:], 1.0), [b_onesb])
    sc16 = -1.0 / 16.0
    cst(lambda e: e.memset(triS[:], sc16), [b_triS])
    cst(lambda e: e.affine_select(out=triS[:, 0, :], in_=triS[:, 0, :], pattern=[[1, 64]], compare_op=ALU.is_ge, fill=0.0, base=0, channel_multiplier=-1), [b_triS])
    cst(lambda e: e.affine_select(out=triS[:, 1, :], in_=triS[:, 1, :], pattern=[[-1, 64]], compare_op=ALU.is_ge, fill=0.0, base=0, channel_multiplier=1), [b_triS])
    cst(lambda e: e.affine_select(out=triS[:, 2, :], in_=triS[:, 2, :], pattern=[[-1, 64]], compare_op=ALU.is_gt, fill=0.0, base=0, channel_multiplier=1), [b_triS])
    cst(lambda e: e.affine_select(out=triS[:, 3, :], in_=triS[:, 3, :], pattern=[[1, 64]], compare_op=ALU.is_gt, fill=0.0, base=0, channel_multiplier=-1), [b_triS])
    triSb, b_triSb = sbt(es, "triSb", (64, 4, 64), BF16)
    S.op("dve", lambda e: e.tensor_copy(triSb[:], triS[:]), reads=[b_triS], writes=[b_triSb])
    cst(lambda e: e.memset(triM[:], 1.0), [b_triM])
    for h in range(4):
        cst(lambda e, h=h: e.affine_select(out=triM[:, 0, h, :], in_=triM[:, 0, h, :], pattern=[[1, 64]], compare_op=ALU.is_ge, fill=0.0, base=0, channel_multiplier=-1), [b_triM])
        cst(lambda e, h=h: e.affine_select(out=triM[:, 1, h, :], in_=triM[:, 1, h, :], pattern=[[-1, 64]], compare_op=ALU.is_ge, fill=0.0, base=0, channel_multiplier=1), [b_triM])
    cst(lambda e: e.memset(selE[:], 1.0), [b_selE])
    cst(lambda e: e.affine_select(out=selE[:], in_=selE[:], pattern=[[-1, 8], [0, 128]], compare_op=ALU.is_equal, fill=0.0, base=0, channel_multiplier=1), [b_selE])

    wA = [sbt(es, "wA%d" % i, (128, 16, 512), BF16) for i in range(3)]
    wA_i = [0]

    def next_wA():
        r = wA[wA_i[0] % 3]
        wA_i[0] += 1
        return r

    def load_w(dst, src, bufobj):
        return S.dma("pool", dst, src, bufobj, writes=[bufobj])

    with nc.allow_non_contiguous_dma(reason="small one-time parameter layouts"):
        with ExitStack() as ph:
            ccT, b_ccT = sbt(ph, "ccT", (128, 16, 2), F32)
            for g_ in range(2):
                S.dma("sp", ccT[:, :, g_], cc_in[g_].rearrange("(kc p) -> p kc", p=128), b_ccT, writes=[b_ccT])
            S.op("act", lambda e: e.activation(scT[:], ccT[:], AF.Silu), reads=[b_ccT], writes=[b_scT])
            for l_ in range(DEPTH):
                S.dma("sp", bmodT[:, l_, :], b_mod[l_].rearrange("(j p) -> p j", p=128), b_bmodT, writes=[b_bmodT])
                S.dma("sp", nT[:, 0, l_, :], norm1_g[l_].rearrange("(c p) -> p c", p=128), b_nT, writes=[b_nT])
                S.dma("sp", nT[:, 1, l_, :], norm2_g[l_].rearrange("(c p) -> p c", p=128), b_nT, writes=[b_nT])
            S.dma("sp", fgT[:], final_g.rearrange("(c p) -> p c", p=128), b_fgT, writes=[b_fgT])
            for l in range(n_layers):
                pm, b_pm = bank()
                for ct in range(48):
                    wt, b_wt = next_wA()
                    load_w(wt[:, :, 0:256], w_mod[l][:, ct * 256:(ct + 1) * 256].rearrange("(kc p) n -> p kc n", p=128), b_wt)
                    for j in range(2):
                        jj = ct * 2 + j
                        for kc in range(16):
                            S.op("pe", lambda e, wt=wt, j=j, kc=kc, jj=jj, pm=pm: e.matmul(
                                pm[:, jj * 2:jj * 2 + 2], wt[:, kc, j * 128:(j + 1) * 128], scT[:, kc, :],
                                start=(kc == 0), stop=(kc == 15)), reads=[b_wt, b_scT], writes=[b_pm])
                for g in range(2):
                    S.op("dve", lambda e, l=l, g=g, pm=pm: e.tensor_tensor(
                        out=modT[:, l, :, g], in0=pm[:, 0:192].rearrange("p (j g) -> p j g", g=2)[:, :, g],
                        in1=bmodT[:, l, :], op=ALU.add), reads=[b_pm, b_bmodT], writes=[b_modT])
                for which in range(2):
                    for g in range(2):
                        sc0 = 16 + 48 * which
                        S.op("dve", lambda e, l=l, g=g, which=which, sc0=sc0: e.scalar_tensor_tensor(
                            out=gsc[:, l, which, :, g], in0=modT[:, l, sc0:sc0 + 16, g], scalar=1.0,
                            in1=nT[:, which, l, :], op0=ALU.add, op1=ALU.mult),
                            reads=[b_modT, b_nT], writes=[b_gsc])
            if dbg:
                d_modT = nc.dram_tensor("d_modT", [128, DEPTH, 96, 2], F32, kind="ExternalOutput").ap()
                d_gsc = nc.dram_tensor("d_gsc", [128, DEPTH, 2, 16, 2], F32, kind="ExternalOutput").ap()
                S.dma("sp", d_modT, modT[:], b_modT, reads=[b_modT])
                S.dma("sp", d_gsc, gsc[:], b_gsc, reads=[b_gsc])
            S.barrier()
            stage("pro")

        with ExitStack() as ph:
            freq, b_freq = sbt(ph, "freq", (128, 512), F32)
            ii, b_ii = sbt(ph, "ii", (128, 512), I32)
            pidx, b_pidx = sbt(ph, "pidx", (128, 2), I32)
            pv2, b_pv2 = sbt(ph, "pv2", (128, 2), F32)
            rv, b_rv = sbt(ph, "rv", (128, 1), F32)
            posc, b_posc = sbt(ph, "posc", (128, 1024), F32)
            posr, b_posr = sbt(ph, "posr", (128, 1024), F32)
            uu, b_uu = sbt(ph, "uu", (128, 512), F32)
            ki, b_ki = sbt(ph, "ki", (128, 512), I32)
            kf, b_kf = sbt(ph, "kf", (128, 512), F32)
            mm, b_mm = sbt(ph, "mm", (128, 512), F32)
            negpi, b_negpi = sbt(ph, "negpi", (128, 1), F32)
            xin = [sbt(ph, "xin%d" % i, (128, D), F32) for i in range(2)]
            xtt = [sbt(ph, "xtt%d" % i, (128, 16, 128), F32) for i in range(2)]
            inv2pi = 1.0 / (2.0 * math.pi)
            S.op("pool", lambda e: e.memset(negpi[:], -math.pi), writes=[b_negpi])
            S.op("pool", lambda e: e.iota(ii[:], pattern=[[1, 512]], base=0, channel_multiplier=0), writes=[b_ii])
            S.op("pool", lambda e: e.iota(pidx[:, 0:1], pattern=[[0, 1]], base=0, channel_multiplier=1), writes=[b_pidx])
            S.op("dve", lambda e: e.tensor_single_scalar(out=pidx[:, 1:2], in_=pidx[:, 0:1], scalar=6, op=ALU.arith_shift_right), reads=[b_pidx], writes=[b_pidx])
            S.op("dve", lambda e: e.tensor_single_scalar(out=pidx[:, 0:1], in_=pidx[:, 0:1], scalar=63, op=ALU.bitwise_and), reads=[b_pidx], writes=[b_pidx])
            S.op("dve", lambda e: e.tensor_copy(pv2[:], pidx[:]), reads=[b_pidx], writes=[b_pv2])
            S.op("dve", lambda e: e.tensor_copy(freq[:], ii[:]), reads=[b_ii], writes=[b_freq])
            S.op("act", lambda e: e.activation(freq[:], freq[:], AF.Exp, scale=-math.log(10000.0) / 512.0), reads=[b_freq], writes=[b_freq])

            def sincos(dst, b_dst, vcol, b_v, off):
                S.op("dve", lambda e: e.tensor_scalar(out=uu[:], in0=freq[:], scalar1=vcol, scalar2=off, op0=ALU.mult, op1=ALU.add),
                     reads=[b_freq, b_v], writes=[b_uu])
                S.op("dve", lambda e: e.tensor_copy(ki[:], uu[:]), reads=[b_uu], writes=[b_ki])
                S.op("dve", lambda e: e.tensor_copy(kf[:], ki[:]), reads=[b_ki], writes=[b_kf])
                S.op("dve", lambda e: e.tensor_tensor(out=uu[:], in0=uu[:], in1=kf[:], op=ALU.subtract), reads=[b_uu, b_kf], writes=[b_uu])
                S.op("dve", lambda e: e.tensor_single_scalar(out=mm[:], in_=uu[:], scalar=0.0, op=ALU.is_lt), reads=[b_uu], writes=[b_mm])
                S.op("dve", lambda e: e.tensor_tensor(out=uu[:], in0=uu[:], in1=mm[:], op=ALU.add), reads=[b_uu, b_mm], writes=[b_uu])
                S.op("act", lambda e: e.activation(dst, uu[:], AF.Sin, scale=2.0 * math.pi, bias=negpi[:]), reads=[b_uu, b_negpi], writes=[b_dst])

            S.op("dve", lambda e: e.tensor_scalar(out=pv2[:], in0=pv2[:], scalar1=inv2pi, scalar2=None, op0=ALU.mult), reads=[b_pv2], writes=[b_pv2])
            sincos(posc[:, 0:512], b_posc, pv2[:, 0:1], b_pv2, 0.5)
            sincos(posc[:, 512:1024], b_posc, pv2[:, 0:1], b_pv2, 0.75)
            for i in range(24):
                xt_, b_xt = xin[i % 2]
                xo, b_xo = xtt[i % 2]
                src = xs_in[i * 128:(i + 1) * 128, :] if i < 16 else xp_in[(i - 16) * 128:(i - 15) * 128, :]
                S.dma("sp", xt_[:], src, b_xt, writes=[b_xt])
                if i < 16:
                    S.op("dve", lambda e, i=i: e.tensor_scalar(out=rv[:], in0=pv2[:, 1:2], scalar1=2.0 * i * inv2pi, scalar2=None, op0=ALU.add),
                         reads=[b_pv2], writes=[b_rv])
                    sincos(posr[:, 0:512], b_posr, rv[:, 0:1], b_rv, 0.5)
                    sincos(posr[:, 512:1024], b_posr, rv[:, 0:1], b_rv, 0.75)
                    S.op("dve", lambda e, xt_=xt_: e.tensor_tensor(out=xt_[:, 0:1024], in0=xt_[:, 0:1024], in1=posr[:], op=ALU.add), reads=[b_xt, b_posr], writes=[b_xt])
                    S.op("dve", lambda e, xt_=xt_: e.tensor_tensor(out=xt_[:, 1024:2048], in0=xt_[:, 1024:2048], in1=posc[:], op=ALU.add), reads=[b_xt, b_posc], writes=[b_xt])
                for q4 in range(4):
                    pt, b_pt = bank()
                    for j in range(4):
                        c = q4 * 4 + j
                        S.op("pe", lambda e, pt=pt, j=j, c=c, xt_=xt_: e.transpose(pt[:, j * 128:(j + 1) * 128], xt_[:, c * 128:(c + 1) * 128], identf[:]),
                             reads=[b_xt, b_identf], writes=[b_pt])
                    eng = "act" if q4 % 2 else "dve"
                    if eng == "act":
                        S.op("act", lambda e, pt=pt, q4=q4, xo=xo: e.activation(xo[:, q4 * 4:(q4 + 1) * 4, :], pt[:].rearrange("p (j t) -> p j t", t=128), AF.Copy), reads=[b_pt], writes=[b_xo])
                    else:
                        S.op("dve", lambda e, pt=pt, q4=q4, xo=xo: e.tensor_copy(xo[:, q4 * 4:(q4 + 1) * 4, :], pt[:].rearrange("p (j t) -> p j t", t=128)), reads=[b_pt], writes=[b_xo])
                S.dma("sp", xT[:, :, i * 128:(i + 1) * 128].rearrange("c p t -> p c t"), xo[:], b_xo, reads=[b_xo], writes=[S.reg("xT", i // 4)])
            S.barrier()
            stage("x")

        def norm_mod(ph_bufs, xTb, b_xTb, hT, b_hT, l, which, g, h32cb=None):
            sq, rstd, b_rstd, tmps = ph_bufs
            pss, b_pss = bank()
            for c in range(16):
                sqt, b_sq = sq[c % 2]
                S.op("act", lambda e, c=c, sqt=sqt: e.activation(sqt[:], xTb[:, c, :], AF.Square), reads=[b_xTb], writes=[b_sq])
                S.op("pe", lambda e, c=c, sqt=sqt, pss=pss: e.matmul(pss[:], onesb[:], sqt[:], start=(c == 0), stop=(c == 15)),
                     reads=[b_sq, b_onesb], writes=[b_pss])
            S.op("act", lambda e, pss=pss: e.activation(rstd[:], pss[:], AF.Sqrt, scale=1.0 / D, bias=epsb[:]), reads=[b_pss, b_epsb], writes=[b_rstd])
            S.op("dve", lambda e: e.reciprocal(rstd[:], rstd[:]), reads=[b_rstd], writes=[b_rstd])
            sh0 = 0 if which == 0 else 48
            for c in range(16):
                tmp, b_tmp = tmps[c % 2]
                S.op("dve", lambda e, c=c, tmp=tmp: e.tensor_tensor(out=tmp[:], in0=xTb[:, c, :], in1=rstd[:], op=ALU.mult), reads=[b_xTb, b_rstd], writes=[b_tmp])
                if h32cb is None:
                    S.op("act", lambda e, c=c, tmp=tmp, l=l, g=g, which=which: e.activation(hT[:, c, :], tmp[:], AF.Identity, scale=gsc[:, l, which, c, g:g + 1],
                                                                     bias=modT[:, l, sh0 + c, g:g + 1]), reads=[b_tmp, b_gsc, b_modT], writes=[b_hT])
                else:
                    S.op("act", lambda e, c=c, tmp=tmp, l=l, g=g, which=which: e.activation(tmp[:], tmp[:], AF.Identity, scale=gsc[:, l, which, c, g:g + 1],
                                                                     bias=modT[:, l, sh0 + c, g:g + 1]), reads=[b_tmp, b_gsc, b_modT], writes=[b_tmp])
                    h32cb(c, tmp, b_tmp)

        epsb, b_epsb = sbt(es, "epsb", (128, 1), F32)
        S.op("pool", lambda e: e.memset(epsb[:], EPS), writes=[b_epsb])

        for l in range(n_layers):
            with ExitStack() as ph:
                xTb, b_xTb = sbt(ph, "xTb", (128, 16, 512), F32)
                hT, b_hT = sbt(ph, "hT", (128, 16, 512), BF16)
                sq = [sbt(ph, "sq%d" % i, (128, 512), BF16) for i in range(2)]
                rstd, b_rstd = sbt(ph, "rstd", (128, 512), F32)
                tmps = [sbt(ph, "tmp%d" % i, (128, 512), F32) for i in range(2)]
                stb = [sbt(ph, "stb%d" % i, (128, 512), BF16) for i in range(3)]
                stf = [sbt(ph, "stf%d" % i, (128, 512), F32) for i in range(3)]
                stt = [sbt(ph, "stt%d" % i, (128, 256), BF16) for i in range(3)]
                pvs = [sbt(ph, "pvs%d" % i, (128, 1024), F32) for i in range(4)]
                vnb = [sbt(ph, "vnb%d" % i, (128, 1024), BF16) for i in range(2)]
                lng, b_lng = sbt(ph, "lng", (128, 1024), F32)
                lnb, b_lnb = sbt(ph, "lnb", (128, 1024), F32)
                bst, b_bst = sbt(ph, "bst", (128, 2, 6), F32)
                bag, b_bag = sbt(ph, "bag", (128, 2), F32)
                lfh, b_lfh = sbt(ph, "lfh", (32, 512), BF16)
                lfl, b_lfl = sbt(ph, "lfl", (32, 512), BF16)
                a2s, b_a2s = sbt(ph, "a2s", (32, 2, 512), F32)
                a2hi, b_a2hi = sbt(ph, "a2hi", (32, 2, 512), BF16)
                a2lo, b_a2lo = sbt(ph, "a2lo", (32, 2, 512), BF16)
                abb, b_abb = sbt(ph, "abb", (128, 2, 512), F32)
                lt = [sbt(ph, "lt%d" % i, (128, 512), F32) for i in range(4)]
                wlf, b_wlf = sbt(ph, "wlf", (128, 16, 32), BF16)
                cnt = {"b": 0, "f": 0, "t": 0}

                S.dma("sp", lng[:], sgu_ln_g[l].partition_broadcast(128), b_lng, writes=[b_lng])
                S.dma("sp", lnb[:], sgu_ln_b[l].partition_broadcast(128), b_lnb, writes=[b_lnb])
                S.op("pool", lambda e: e.memset(a2s[:], 0.0), writes=[b_a2s])
                S.dma("sp", a2s[0:16, 0, :], gla_a2[l, 0], b_a2s, writes=[b_a2s])
                S.dma("sp", a2s[16:32, 1, :], gla_a2[l, 1], b_a2s, writes=[b_a2s])
                S.op("dve", lambda e: e.tensor_copy(a2hi[:], a2s[:]), reads=[b_a2s], writes=[b_a2hi])
                S.op("dve", lambda e: e.tensor_tensor(out=a2lo[:], in0=a2s[:], in1=a2hi[:], op=ALU.subtract), reads=[b_a2s, b_a2hi], writes=[b_a2lo])
                for d_ in range(2):
                    S.dma("sp", abb[:, d_, :], gla_ab[l, d_].partition_broadcast(128), b_abb, writes=[b_abb])

                for b in range(NBLK):
                    g = 0 if b < 4 else 1
                    t0 = b * 512
                    S.dma("sp", xTb[:], xT[:, :, t0:t0 + 512].rearrange("c p t -> p c t"), b_xTb, reads=[S.reg("xT", b)], writes=[b_xTb])
                    norm_mod((sq, rstd, b_rstd, tmps), xTb, b_xTb, hT, b_hT, l, 0, g)
                    stage("p1a")

                    def fm_group(col0, ncols, dst, name, func, scale=1.0, fp32=False):
                        for ct in range(ncols // 256):
                            wt, b_wt = next_wA()
                            load_w(wt[:, :, 0:256], w_in[l][:, col0 + ct * 256:col0 + (ct + 1) * 256].rearrange("(kc p) n -> p kc n", p=128), b_wt)
                            for j in range(2):
                                ch = ct * 2 + j
                                pp, b_pp = bank()
                                for kc in range(16):
                                    S.op("pe", lambda e, wt=wt, j=j, kc=kc, pp=pp: e.matmul(pp[:], wt[:, kc, j * 128:(j + 1) * 128], hT[:, kc, :],
                                                                                         start=(kc == 0), stop=(kc == 15)), reads=[b_wt, b_hT], writes=[b_pp])
                                if fp32:
                                    st_, b_st = stf[cnt["f"] % 3]; cnt["f"] += 1
                                else:
                                    st_, b_st = stb[cnt["b"] % 3]; cnt["b"] += 1
                                S.op("act", lambda e, st_=st_, pp=pp: e.activation(st_[:], pp[:], func, scale=scale), reads=[b_pp], writes=[b_st])
                                S.dma("sp", dst[ch, :, t0:t0 + 512], st_[:], b_st, reads=[b_st], writes=[S.reg(name, ch, b)])

                    def tm_group(col0, ncols, evac):
                        for ct in range(ncols // 256):
                            wt, b_wt = next_wA()
                            load_w(wt[:, :, 0:256], w_in[l][:, col0 + ct * 256:col0 + (ct + 1) * 256].rearrange("(kc p) n -> p kc n", p=128), b_wt)
                            for ts in range(4):
                                pp, b_pp = bank()
                                for kc in range(16):
                                    S.op("pe", lambda e, wt=wt, ts=ts, kc=kc, pp=pp: e.matmul(pp[:, 0:256], hT[:, kc, ts * 128:(ts + 1) * 128], wt[:, kc, 0:256],
                                                                                          start=(kc == 0), stop=(kc == 15)), reads=[b_wt, b_hT], writes=[b_pp])
                                evac(ct, ts, pp, b_pp)

                    def tm_store(dst, name, func):
                        def evac(ct, ts, pp, b_pp):
                            st_, b_st = stt[cnt["t"] % 3]; cnt["t"] += 1
                            S.op("act", lambda e, st_=st_, pp=pp: e.activation(st_[:], pp[:, 0:256], func), reads=[b_pp], writes=[b_st])
                            S.dma("sp", dst[t0 + ts * 128:t0 + (ts + 1) * 128, ct * 256:(ct + 1) * 256], st_[:], b_st, reads=[b_st],
                                  writes=[S.reg(name, ct, b * 4 + ts)])
                        return evac

                    fm_group(0, 1024, puT, "puT", AF.Gelu_apprx_tanh)
                    stage("p1b")

                    def evac_pv(ct, ts, pp, b_pp):
                        pvt, b_pvt = pvs[ts]
                        S.op("act", lambda e, pvt=pvt, pp=pp, ct=ct: e.activation(pvt[:, ct * 256:(ct + 1) * 256], pp[:, 0:256], AF.Gelu_apprx_tanh), reads=[b_pp], writes=[b_pvt])
                    tm_group(1024, 1024, evac_pv)
                    for ts in range(4):
                        pvt, b_pvt = pvs[ts]
                        vb_, b_vb = vnb[ts % 2]
                        for hh in range(2):
                            S.op("dve", lambda e, pvt=pvt, hh=hh: e.bn_stats(bst[:, hh, :], pvt[:, hh * 512:(hh + 1) * 512]), reads=[b_pvt], writes=[b_bst])
                        S.op("dve", lambda e: e.bn_aggr(bag[:], bst[:].rearrange("p a s -> p (a s)")), reads=[b_bst], writes=[b_bag])
                        S.op("act", lambda e: e.activation(bag[:, 1:2], bag[:, 1:2], AF.Sqrt, bias=epsb[:]), reads=[b_bag, b_epsb], writes=[b_bag])
                        S.op("dve", lambda e: e.reciprocal(bag[:, 1:2], bag[:, 1:2]), reads=[b_bag], writes=[b_bag])
                        S.op("dve", lambda e, pvt=pvt: e.tensor_scalar(out=pvt[:], in0=pvt[:], scalar1=bag[:, 0:1], scalar2=bag[:, 1:2], op0=ALU.subtract, op1=ALU.mult),
                             reads=[b_pvt, b_bag], writes=[b_pvt])
                        S.op("dve", lambda e, pvt=pvt: e.tensor_tensor(out=pvt[:], in0=pvt[:], in1=lng[:], op=ALU.mult), reads=[b_pvt, b_lng], writes=[b_pvt])
                        S.op("dve", lambda e, pvt=pvt, vb_=vb_: e.tensor_tensor(out=vb_[:], in0=pvt[:], in1=lnb[:], op=ALU.add), reads=[b_pvt, b_lnb], writes=[b_vb])
                        S.dma("sp", vn_d[t0 + ts * 128:t0 + (ts + 1) * 128, :], vb_[:], b_vb, reads=[b_vb], writes=[S.reg("vn", b * 4 + ts)])

                    stage("p1c")
                    fm_group(2048, 512, qT_d, "qT", AF.Copy, scale=128.0 ** -0.5, fp32=True)
                    fm_group(2560, 512, kT_d, "kT", AF.Copy, fp32=True)
                    tm_group(2560, 512, tm_store(ktm_d, "ktm", AF.Copy))
                    tm_group(3072, 1024, tm_store(vtm_d, "vtm", AF.Copy))
                    tm_group(4096, 1024, tm_store(rtm_d, "rtm", AF.Silu))

                    stage("p1d")
                    load_w(wlf[:], w_in[l][:, 5120:5152].rearrange("(kc p) n -> p kc n", p=128), b_wlf)
                    pp, b_pp = bank()
                    for kc in range(16):
                        S.op("pe", lambda e, kc=kc, pp=pp: e.matmul(pp[0:32, :], wlf[:, kc, :], hT[:, kc, :], start=(kc == 0), stop=(kc == 15)),
                             reads=[b_wlf, b_hT], writes=[b_pp])
                    S.op("dve", lambda e, pp=pp: e.tensor_copy(lfh[:], pp[0:32, :]), reads=[b_pp], writes=[b_lfh])
                    S.op("dve", lambda e, pp=pp: e.tensor_tensor(out=lfl[:], in0=pp[0:32, :], in1=lfh[:], op=ALU.subtract), reads=[b_pp, b_lfh], writes=[b_lfl])
                    for d_ in range(2):
                        for ts in range(4):
                            pp2, b_pp2 = bank()
                            S.op("pe", lambda e, pp2=pp2, ts=ts, d_=d_: e.matmul(pp2[:], lfh[:, ts * 128:(ts + 1) * 128], a2hi[:, d_, :], start=True, stop=False),
                                 reads=[b_lfh, b_a2hi], writes=[b_pp2])
                            S.op("pe", lambda e, pp2=pp2, ts=ts, d_=d_: e.matmul(pp2[:], lfl[:, ts * 128:(ts + 1) * 128], a2hi[:, d_, :], start=False, stop=False),
                                 reads=[b_lfl, b_a2hi], writes=[b_pp2])
                            S.op("pe", lambda e, pp2=pp2, ts=ts, d_=d_: e.matmul(pp2[:], lfh[:, ts * 128:(ts + 1) * 128], a2lo[:, d_, :], start=False, stop=True),
                                 reads=[b_lfh, b_a2lo], writes=[b_pp2])
                            l0, b_l0 = lt[(d_ * 4 + ts) % 2]
                            l2, b_l2 = lt[2 + (d_ * 4 + ts) % 2]
                            S.op("dve", lambda e, pp2=pp2, l0=l0, d_=d_: e.tensor_tensor(out=l0[:], in0=pp2[:], in1=abb[:, d_, :], op=ALU.add), reads=[b_pp2, b_abb], writes=[b_l0])
                            S.op("act", lambda e, l0=l0: e.activation(l0[:], l0[:], AF.Exp, scale=-1.0), reads=[b_l0], writes=[b_l0])
                            S.op("act", lambda e, l0=l0, l2=l2: e.activation(l2[:], l0[:], AF.Ln, bias=onesf[:, 0:1]), reads=[b_l0, b_onesf], writes=[b_l2])
                            S.dma("sp", latm_d[d_, t0 + ts * 128:t0 + (ts + 1) * 128, :], l2[:], b_l2, reads=[b_l2], writes=[S.reg("latm", d_, b * 4 + ts)])

                    stage("p1e")
                    fm_group(5152, 2048, gaT_d, "gaT", AF.Sigmoid)
                    fm_group(7200, 2048, gbT_d, "gbT", AF.Sigmoid)
                S.barrier()
                stage("p1_%d" % l)

            with ExitStack() as ph:
                wsr, b_wsr = sbt(ph, "wsr", (128, 8, 128), F32)
                wsT, b_wsT = sbt(ph, "wsT", (128, 8, 128), BF16)
                bsf, b_bsf = sbt(ph, "bsf", (128, 1024), F32)
                ftm = [sbt(ph, "ftm%d" % i, (128, 4, 128), F32) for i in range(2)]
                vnc = [sbt(ph, "vnc%d" % i, (128, 1024), BF16) for i in range(2)]
                puc = [sbt(ph, "puc%d" % i, (128, 8, 128), BF16) for i in range(2)]
                atc = [sbt(ph, "atc%d" % i, (128, 8, 128), BF16) for i in range(2)]
                S.dma("sp", wsr[:], w_spatial[l].rearrange("g p q -> p g q"), b_wsr, writes=[b_wsr])
                S.dma("sp", bsf[:], b_spatial[l].rearrange("g p -> (g p)").partition_broadcast(128), b_bsf, writes=[b_bsf])
                for g2 in range(2):
                    pt, b_pt = bank()
                    for j in range(4):
                        gg = g2 * 4 + j
                        S.op("pe", lambda e, pt=pt, j=j, gg=gg: e.transpose(pt[:, j * 128:(j + 1) * 128], wsr[:, gg, :], identf[:]), reads=[b_wsr, b_identf], writes=[b_pt])
                    S.op("dve", lambda e, pt=pt, g2=g2: e.tensor_copy(wsT[:, g2 * 4:(g2 + 1) * 4, :], pt[:].rearrange("p (j t) -> p j t", t=128)), reads=[b_pt], writes=[b_wsT])
                for i in range(24):
                    vc, b_vc = vnc[i % 2]
                    pc, b_pc = puc[i % 2]
                    ac, b_ac = atc[i % 2]
                    S.dma("sp", vc[:], vn_d[i * 128:(i + 1) * 128, :], b_vc, reads=[S.reg("vn", i)], writes=[b_vc])
                    S.dma("sp", pc[:], puT[:, :, i * 128:(i + 1) * 128].rearrange("c p t -> p c t"), b_pc, reads=[S.reg("puT", c, i // 4) for c in range(8)], writes=[b_pc])
                    for g2 in range(2):
                        pp, b_pp = bank()
                        for j in range(4):
                            gg = g2 * 4 + j
                            S.op("pe", lambda e, pp=pp, j=j, gg=gg, vc=vc: e.matmul(pp[:, j * 128:(j + 1) * 128], vc[:, gg * 128:(gg + 1) * 128], wsT[:, gg, :], start=True, stop=True),
                                 reads=[b_vc, b_wsT], writes=[b_pp])
                        ft_, b_ft = ftm[g2]
                        S.op("dve", lambda e, pp=pp, g2=g2, ft_=ft_: e.tensor_tensor(out=ft_[:], in0=pp[:].rearrange("p (j t) -> p j t", t=128),
                                                                             in1=bsf[:, g2 * 512:(g2 + 1) * 512].rearrange("p (j t) -> p j t", t=128), op=ALU.add), reads=[b_pp, b_bsf], writes=[b_ft])
                        S.op("pool", lambda e, g2=g2, pc=pc, ac=ac, ft_=ft_: e.tensor_tensor(out=ac[:, g2 * 4:(g2 + 1) * 4, :], in0=ft_[:],
                                                                                    in1=pc[:, g2 * 4:(g2 + 1) * 4, :], op=ALU.mult), reads=[b_ft, b_pc], writes=[b_ac])
                    S.dma("sp", aT_d[:, :, i * 128:(i + 1) * 128].rearrange("c p t -> p c t"), ac[:], b_ac, reads=[b_ac], writes=[S.reg("aT", i)])
                S.barrier()
                stage("p2_%d" % l)

            with ExitStack() as ph:
                Sf, b_Sf = sbt(ph, "Sf", (128, 4, 256), F32)
                Sb, b_Sb = sbt(ph, "Sb", (128, 4, 256), BF16)
                gng, b_gng = sbt(ph, "gng", (64, 1024), F32)
                qTc = [sbt(ph, "qTc%d" % i, (128, 4, 64), F32) for i in range(2)]
                kTc = [sbt(ph, "kTc%d" % i, (128, 4, 64), F32) for i in range(2)]
                ktc = [sbt(ph, "ktc%d" % i, (64, 512), BF16) for i in range(2)]
                vtc = [sbt(ph, "vtc%d" % i, (64, 1024), BF16) for i in range(2)]
                lac = [sbt(ph, "lac%d" % i, (64, 512), F32) for i in range(2)]
                lah = [sbt(ph, "lah%d" % i, (64, 512), BF16) for i in range(2)]
                lal = [sbt(ph, "lal%d" % i, (64, 512), BF16) for i in range(2)]
                rtc = [sbt(ph, "rtc%d" % i, (64, 1024), BF16) for i in range(2)]
                ofc = [sbt(ph, "ofc%d" % i, (64, 1024), F32) for i in range(2)]
                E1 = [sbt(ph, "E1%d" % i, (128, 4, 64), F32) for i in range(2)]
                E2 = [sbt(ph, "E2%d" % i, (128, 4, 64), F32) for i in range(2)]
                qd = [sbt(ph, "qd%d" % i, (128, 4, 64), BF16) for i in range(2)]
                kd = [sbt(ph, "kd%d" % i, (128, 4, 64), BF16) for i in range(2)]
                EK = [sbt(ph, "EK%d" % i, (64, 512), F32) for i in range(2)]
                kend = [sbt(ph, "kend%d" % i, (64, 512), BF16) for i in range(2)]
                attm = [sbt(ph, "attm%d" % i, (64, 4, 64), BF16) for i in range(2)]
                osb = [sbt(ph, "osb%d" % i, (64, 1024), F32) for i in range(2)]
                ot2 = [sbt(ph, "ot2%d" % i, (64, 1024), F32) for i in range(2)]
                ogb = [sbt(ph, "ogb%d" % i, (64, 1024), BF16) for i in range(2)]
                oTc = [sbt(ph, "oTc%d" % i, (128, 8, 64), BF16) for i in range(2)]
                ssq = [sbt(ph, "ssq%d" % i, (64, 4), F32) for i in range(2)]
                junk, b_junk = sbt(ph, "junk", (64, 256), BF16)
                S.dma("sp", gng[:], gla_norm_g[l].partition_broadcast(64), b_gng, writes=[b_gng])
                it = [0]
                for si, (s0, L, grp) in enumerate(SEQS):
                    N = L // 64
                    for dr in range(2):
                        if grp == 0:
                            S.dma("sp", Sf[:], st_in[l, dr].rearrange("h d e -> d h e"), b_Sf, writes=[b_Sf])
                        else:
                            S.op("pool", lambda e: e.memset(Sf[:], 0.0), writes=[b_Sf])
                        S.op("act", lambda e: e.activation(Sb[:], Sf[:], AF.Copy), reads=[b_Sf], writes=[b_Sb])
                        order = range(N) if dr == 0 else range(N - 1, -1, -1)
                        for n in order:
                            k_ = it[0] % 2
                            it[0] += 1
                            tk = s0 + n * 64
                            c64 = tk // 64
                            b5 = tk // 512
                            t128 = tk // 128
                            q_, b_q = qTc[k_]; kk_, b_k = kTc[k_]; kt_, b_kt = ktc[k_]; vt_, b_vt = vtc[k_]; la_, b_la = lac[k_]
                            S.dma("sp", q_[:], qT_d[:, :, tk:tk + 64].rearrange("h p t -> p h t"), b_q, reads=[S.reg("qT", h, b5) for h in range(4)], writes=[b_q])
                            S.dma("sp", kk_[:], kT_d[:, :, tk:tk + 64].rearrange("h p t -> p h t"), b_k, reads=[S.reg("kT", h, b5) for h in range(4)], writes=[b_k])
                            S.dma("sp", kt_[:], ktm_d[tk:tk + 64, :], b_kt, reads=[S.reg("ktm", c, t128) for c in range(2)], writes=[b_kt])
                            S.dma("sp", vt_[:], vtm_d[tk:tk + 64, :], b_vt, reads=[S.reg("vtm", c, t128) for c in range(4)], writes=[b_vt])
                            S.dma("sp", la_[:], latm_d[dr, tk:tk + 64, :], b_la, reads=[S.reg("latm", dr, t128)], writes=[b_la])
                            lah_, b_lah = lah[k_]; lal_, b_lal = lal[k_]
                            S.op("dve", lambda e, lah_=lah_, la_=la_: e.tensor_copy(lah_[:], la_[:]), reads=[b_la], writes=[b_lah])
                            S.op("pool", lambda e, lal_=lal_, lah_=lah_, la_=la_: e.tensor_tensor(out=lal_[:], in0=la_[:], in1=lah_[:], op=ALU.subtract), reads=[b_la, b_lah], writes=[b_lal])
                            p1, b_p1 = bank()
                            for h in range(4):
                                S.op("pe", lambda e, p1=p1, h=h, lah_=lah_, dr=dr: e.matmul(p1[:, h * 64:(h + 1) * 64], lah_[:, h * 128:(h + 1) * 128], triSb[:, dr, :], start=True, stop=False),
                                     reads=[b_lah, b_triSb], writes=[b_p1])
                                S.op("pe", lambda e, p1=p1, h=h, lal_=lal_, dr=dr: e.matmul(p1[:, h * 64:(h + 1) * 64], lal_[:, h * 128:(h + 1) * 128], triSb[:, dr, :], start=False, stop=True),
                                     reads=[b_lal, b_triSb], writes=[b_p1])
                            p2, b_p2 = bank()
                            S.op("pe", lambda e, p2=p2, lah_=lah_, dr=dr: e.matmul(p2[0:64, :], triSb[:, 2 + dr, :], lah_[:], start=True, stop=False), reads=[b_lah, b_triSb], writes=[b_p2])
                            S.op("pe", lambda e, p2=p2, lal_=lal_, dr=dr: e.matmul(p2[0:64, :], triSb[:, 2 + dr, :], lal_[:], start=False, stop=True), reads=[b_lal, b_triSb], writes=[b_p2])
                            e1, b_e1 = E1[k_]; e2, b_e2 = E2[k_]; ek, b_ek = EK[k_]
                            S.op("act", lambda e, p1=p1, e1=e1: e.activation(e1[:], p1[:, 0:256].rearrange("p (h c) -> p h c", c=64), AF.Exp), reads=[b_p1], writes=[b_e1])
                            S.op("act", lambda e, p1=p1, e2=e2: e.activation(e2[:], p1[:, 0:256].rearrange("p (h c) -> p h c", c=64), AF.Exp, scale=-1.0), reads=[b_p1], writes=[b_e2])
                            S.op("act", lambda e, p2=p2, ek=ek: e.activation(ek[:], p2[0:64, :], AF.Exp), reads=[b_p2], writes=[b_ek])
                            qd_, b_qd = qd[k_]; kd_, b_kd = kd[k_]; ke_, b_ke = kend[k_]
                            S.op("dve", lambda e, qd_=qd_, q_=q_, e1=e1: e.tensor_tensor(out=qd_[:], in0=q_[:], in1=e1[:], op=ALU.mult), reads=[b_q, b_e1], writes=[b_qd])
                            S.op("dve", lambda e, kd_=kd_, kk_=kk_, e2=e2: e.tensor_tensor(out=kd_[:], in0=kk_[:], in1=e2[:], op=ALU.mult), reads=[b_k, b_e2], writes=[b_kd])
                            S.op("pool", lambda e, ke_=ke_, kt_=kt_, ek=ek: e.tensor_tensor(out=ke_[:], in0=kt_[:], in1=ek[:], op=ALU.mult), reads=[b_kt, b_ek], writes=[b_ke])
                            p3, b_p3 = bank()
                            for h in range(4):
                                S.op("pe", lambda e, p3=p3, h=h, kd_=kd_, qd_=qd_: e.matmul(p3[0:64, h * 64:(h + 1) * 64], kd_[:, h, :], qd_[:, h, :], start=True, stop=True),
                                     reads=[b_kd, b_qd], writes=[b_p3])
                            am_, b_am = attm[k_]
                            S.op("dve", lambda e, p3=p3, am_=am_, dr=dr: e.tensor_tensor(out=am_[:], in0=p3[0:64, 0:256].rearrange("p (h c) -> p h c", c=64), in1=triM[:, dr, :, :], op=ALU.mult),
                                 reads=[b_p3, b_triM], writes=[b_am])
                            po = [bank(), bank()]
                            for h in range(4):
                                pt_, b_pt_ = po[h // 2]
                                hh = h % 2
                                S.op("pe", lambda e, pt_=pt_, hh=hh, h=h, am_=am_, vt_=vt_: e.matmul(pt_[0:64, hh * 256:(hh + 1) * 256], am_[:, h, :], vt_[:, h * 256:(h + 1) * 256], start=True, stop=False),
                                     reads=[b_am, b_vt], writes=[b_pt_])
                                S.op("pe", lambda e, pt_=pt_, hh=hh, h=h, qd_=qd_: e.matmul(pt_[0:64, hh * 256:(hh + 1) * 256], qd_[:, h, :], Sb[:, h, :], start=False, stop=True),
                                     reads=[b_qd, b_Sb], writes=[b_pt_])
                            pd = [bank(), bank()]
                            for h in range(4):
                                pt_, b_pt_ = pd[h // 2]
                                hh = h % 2
                                S.op("pe", lambda e, pt_=pt_, hh=hh, h=h, ke_=ke_, vt_=vt_: e.matmul(pt_[:, hh * 256:(hh + 1) * 256], ke_[:, h * 128:(h + 1) * 128], vt_[:, h * 256:(h + 1) * 256], start=True, stop=True),
                                     reads=[b_ke, b_vt], writes=[b_pt_])
                            gcol = 63 if dr == 0 else 0
                            for h in range(4):
                                pt_, b_pt_ = pd[h // 2]
                                hh = h % 2
                                S.op("dve", lambda e, pt_=pt_, hh=hh, h=h, e1=e1, gcol=gcol: e.scalar_tensor_tensor(out=Sf[:, h, :], in0=Sf[:, h, :], scalar=e1[:, h, gcol:gcol + 1],
                                                                                                     in1=pt_[:, hh * 256:(hh + 1) * 256], op0=ALU.mult, op1=ALU.add),
                                     reads=[b_Sf, b_e1, b_pt_], writes=[b_Sf])
                            S.op("act", lambda e: e.activation(Sb[:], Sf[:], AF.Copy), reads=[b_Sf], writes=[b_Sb])
                            if dbg and it[0] == 1 and l == 0:
                                for nm, t_, b_, shp, dt_ in (("g_e1", e1, b_e1, [128, 4, 64], F32), ("g_ek", ek, b_ek, [64, 512], F32), ("g_kend", ke_, b_ke, [64, 512], BF16),
                                                             ("g_attm", am_, b_am, [64, 4, 64], BF16), ("g_qd", qd_, b_qd, [128, 4, 64], BF16), ("g_kd", kd_, b_kd, [128, 4, 64], BF16),
                                                             ("g_S", Sf, b_Sf, [128, 4, 256], F32), ("g_la", la_, b_la, [64, 512], F32),
                                                             ("g_tri", triS, b_triS, [64, 4, 64], F32), ("g_trib", triSb, b_triSb, [64, 4, 64], BF16),
                                                             ("g_lah", lah_, b_lah, [64, 512], BF16), ("g_lal", lal_, b_lal, [64, 512], BF16), ("g_triM", triM, b_triM, [64, 2, 4, 64], F32)):
                                    dd = nc.dram_tensor(nm, shp, dt_, kind="ExternalOutput").ap()
                                    S.dma("sp", dd, t_[:], b_, reads=[b_])
                            if dr == 0:
                                os_, b_os = osb[k_]
                                for hb in range(2):
                                    pt_, b_pt_ = po[hb]
                                    S.op("act" if hb else "dve", (lambda e, pt_=pt_, hb=hb, os_=os_: e.activation(os_[:, hb * 512:(hb + 1) * 512], pt_[0:64, :], AF.Copy)) if hb else
                                         (lambda e, pt_=pt_, hb=hb, os_=os_: e.tensor_copy(os_[:, hb * 512:(hb + 1) * 512], pt_[0:64, :])), reads=[b_pt_], writes=[b_os])
                                S.dma("sp", of_d[tk:tk + 64, :], os_[:], b_os, reads=[b_os], writes=[S.reg("of", c64)])
                            else:
                                of_, b_of = ofc[k_]; rt_, b_rt = rtc[k_]
                                S.dma("sp", of_[:], of_d[tk:tk + 64, :], b_of, reads=[S.reg("of", c64)], writes=[b_of])
                                S.dma("sp", rt_[:], rtm_d[tk:tk + 64, :], b_rt, reads=[S.reg("rtm", c, t128) for c in range(4)], writes=[b_rt])
                                os_, b_os = osb[k_]
                                for hb in range(2):
                                    pt_, b_pt_ = po[hb]
                                    S.op("dve", lambda e, pt_=pt_, hb=hb, os_=os_, of_=of_: e.tensor_tensor(out=os_[:, hb * 512:(hb + 1) * 512], in0=pt_[0:64, :], in1=of_[:, hb * 512:(hb + 1) * 512], op=ALU.add),
                                         reads=[b_pt_, b_of], writes=[b_os])
                                ss_, b_ss = ssq[k_]
                                for h in range(4):
                                    S.op("act", lambda e, os_=os_, h=h, ss_=ss_: e.activation(junk[:], os_[:, h * 256:(h + 1) * 256], AF.Square, accum_out=ss_[:, h:h + 1]),
                                         reads=[b_os], writes=[b_junk, b_ss])
                                S.op("act", lambda e, ss_=ss_: e.activation(ss_[:], ss_[:], AF.Sqrt, scale=1.0 / 256.0, bias=epsb[0:64, :]), reads=[b_ss, b_epsb], writes=[b_ss])
                                S.op("dve", lambda e, ss_=ss_: e.reciprocal(ss_[:], ss_[:]), reads=[b_ss], writes=[b_ss])
                                o2_, b_o2 = ot2[k_]
                                for h in range(4):
                                    S.op("dve", lambda e, os_=os_, h=h, ss_=ss_, o2_=o2_: e.scalar_tensor_tensor(out=o2_[:, h * 256:(h + 1) * 256], in0=os_[:, h * 256:(h + 1) * 256], scalar=ss_[:, h:h + 1],
                                                                                                         in1=gng[:, h * 256:(h + 1) * 256], op0=ALU.mult, op1=ALU.mult),
                                         reads=[b_os, b_ss, b_gng], writes=[b_o2])
                                og_, b_og = ogb[k_]
                                S.op("pool", lambda e, og_=og_, o2_=o2_, rt_=rt_: e.tensor_tensor(out=og_[:], in0=o2_[:], in1=rt_[:], op=ALU.mult), reads=[b_o2, b_rt], writes=[b_og])
                                ptr, b_ptr = bank()
                                ptb = ptr[:].bitcast(BF16)
                                for c in range(8):
                                    S.op("pe", lambda e, ptb=ptb, c=c, og_=og_: e.transpose(ptb[:, c * 64:(c + 1) * 64], og_[:, c * 128:(c + 1) * 128], identb[0:64, 0:64]),
                                         reads=[b_og, b_identb], writes=[b_ptr])
                                oc_, b_oc = oTc[k_]
                                S.op("dve", lambda e, ptb=ptb, oc_=oc_: e.tensor_copy(oc_[:], ptb[:, 0:512].rearrange("p (c t) -> p c t", t=64)), reads=[b_ptr], writes=[b_oc])
                                S.dma("sp", oT_d[:, :, tk:tk + 64].rearrange("c p t -> p c t"), oc_[:], b_oc, reads=[b_oc], writes=[S.reg("oT", c64)])
                        if grp == 1:
                            S.dma("sp", ns_out[si - 1, l, dr].rearrange("h d e -> d h e"), Sf[:], b_Sf, reads=[b_Sf])
                S.barrier()
                stage("p3_%d" % l)

            with ExitStack() as ph:
                is_moe = (l % 2 == 1)
                jl = l // 2
                xTb, b_xTb = sbt(ph, "xTb", (128, 16, 512), F32)
                hT, b_hT = sbt(ph, "hT", (128, 16, 512), BF16)
                gT, b_gT = sbt(ph, "gT", (128, NFF, 512), BF16)
                mTb, b_mTb = gT, b_gT
                aTb, b_aTb = hT[:, 0:8, :], b_hT
                oTb, b_oTb = hT[:, 8:16, :], b_hT
                gab = [sbt(ph, "gab%d" % i, (128, 512), BF16) for i in range(2)]
                gbb = [sbt(ph, "gbb%d" % i, (128, 512), BF16) for i in range(2)]
                sq = [sbt(ph, "sq%d" % i, (128, 512), BF16) for i in range(2)]
                rstd, b_rstd = sbt(ph, "rstd", (128, 512), F32)
                tmps = [sbt(ph, "tmp%d" % i, (128, 512), F32) for i in range(2)]
                t1 = [sbt(ph, "t1%d" % i, (128, 512), BF16) for i in range(2)]
                t2 = [sbt(ph, "t2%d" % i, (128, 512), BF16) for i in range(2)]
                sil = [sbt(ph, "sil%d" % i, (128, 512), BF16) for i in range(2)]
                u3s = [sbt(ph, "u3s%d" % i, (128, 512), BF16) for i in range(2)]
                w2t = [sbt(ph, "w2t%d" % i, (128, 2, 512), BF16) for i in range(6)]
                if is_moe:
                    rwf, b_rwf = sbt(ph, "rwf", (128, 16, NE), F32)
                    rwh, b_rwh = sbt(ph, "rwh", (128, 16, NE), BF16)
                    rwl, b_rwl = sbt(ph, "rwl", (128, 16, NE), BF16)
                    hlo = [sbt(ph, "hlo%d" % i, (128, 512), BF16) for i in range(2)]
                    selEb, b_selEb = selE, b_selE
                    cmbTh, b_cmbTh = sbt(ph, "cmbTh", (NE, 512), BF16)
                    rbb, b_rbb = sbt(ph, "rbb", (128, NE), F32)
                    lgT, b_lgT = sbt(ph, "lgT", (NE, 512), F32)
                    lg, b_lg = sbt(ph, "lg", (128, NE), F32)
                    mk1, b_mk1 = sbt(ph, "mk1", (128, NE), F32)
                    mk2, b_mk2 = sbt(ph, "mk2", (128, NE), F32)
                    m12, b_m12 = sbt(ph, "m12", (128, 4), F32)
                    cmb, b_cmb = sbt(ph, "cmb", (128, NE), F32)
                    cmbT, b_cmbT = sbt(ph, "cmbT", (NE, 512), F32)
                    cbes = [sbt(ph, "cbe%d" % i, (128, 512), BF16) for i in range(2)]
                    S.dma("sp", rwf[:], moe_router[jl].rearrange("(kc p) e -> p kc e", p=128), b_rwf, writes=[b_rwf])
                    S.dma("sp", rbb[:], moe_router_b[jl].partition_broadcast(128), b_rbb, writes=[b_rbb])
                    S.op("dve", lambda e: e.tensor_copy(rwh[:], rwf[:]), reads=[b_rwf], writes=[b_rwh])
                    S.op("dve", lambda e: e.tensor_tensor(out=rwl[:], in0=rwf[:], in1=rwh[:], op=ALU.subtract), reads=[b_rwf, b_rwh], writes=[b_rwl])

                w2i = [0]
                for b in range(NBLK):
                    g = 0 if b < 4 else 1
                    t0 = b * 512
                    S.dma("sp", xTb[:], xT[:, :, t0:t0 + 512].rearrange("c p t -> p c t"), b_xTb, reads=[S.reg("xT", b)], writes=[b_xTb])
                    S.dma("sp", aTb, aT_d[:, :, t0:t0 + 512].rearrange("c p t -> p c t"), b_aTb, reads=[S.reg("aT", 4 * b + i) for i in range(4)], writes=[b_aTb])
                    S.dma("sp", oTb, oT_d[:, :, t0:t0 + 512].rearrange("c p t -> p c t"), b_oTb, reads=[S.reg("oT", 8 * b + i) for i in range(8)], writes=[b_oTb])
                    for ct in range(8):
                        wt, b_wt = next_wA()
                        load_w(wt[:, 0:8, 0:256], w_branch_a[l][:, ct * 256:(ct + 1) * 256].rearrange("(kc p) n -> p kc n", p=128), b_wt)
                        load_w(wt[:, 8:16, 0:256], w_branch_b[l][:, ct * 256:(ct + 1) * 256].rearrange("(kc p) n -> p kc n", p=128), b_wt)
                        for j in range(2):
                            oc = ct * 2 + j
                            ga_, b_ga = gab[oc % 2]; gb_, b_gb = gbb[oc % 2]
                            S.dma("sp", ga_[:], gaT_d[oc, :, t0:t0 + 512], b_ga, reads=[S.reg("gaT", oc, b)], writes=[b_ga])
                            S.dma("sp", gb_[:], gbT_d[oc, :, t0:t0 + 512], b_gb, reads=[S.reg("gbT", oc, b)], writes=[b_gb])
                            pa, b_pa = bank()
                            pb_, b_pb = bank()
                            for kc in range(8):
                                S.op("pe", lambda e, pa=pa, wt=wt, kc=kc, j=j: e.matmul(pa[:], wt[:, kc, j * 128:(j + 1) * 128], aTb[:, kc, :], start=(kc == 0), stop=(kc == 7)),
                                     reads=[b_wt, b_aTb], writes=[b_pa])
                            for kc in range(8):
                                S.op("pe", lambda e, pb_=pb_, wt=wt, kc=kc, j=j: e.matmul(pb_[:], wt[:, 8 + kc, j * 128:(j + 1) * 128], oTb[:, kc, :], start=(kc == 0), stop=(kc == 7)),
                                     reads=[b_wt, b_oTb], writes=[b_pb])
                            ta, b_ta = t1[oc % 2]; tb_, b_tb = t2[oc % 2]
                            S.op("dve", lambda e, ta=ta, pa=pa, ga_=ga_: e.tensor_tensor(out=ta[:], in0=pa[:], in1=ga_[:], op=ALU.mult), reads=[b_pa, b_ga], writes=[b_ta])
                            S.op("dve", lambda e, tb_=tb_, pb_=pb_, gb_=gb_: e.tensor_tensor(out=tb_[:], in0=pb_[:], in1=gb_[:], op=ALU.mult), reads=[b_pb, b_gb], writes=[b_tb])
                            S.op("pool", lambda e, ta=ta, tb_=tb_, oc=oc: e.tensor_tensor(out=mTb[:, oc, :], in0=ta[:], in1=tb_[:], op=ALU.add), reads=[b_ta, b_tb], writes=[b_mTb])
                    for ct in range(8):
                        wt, b_wt = next_wA()
                        load_w(wt[:, :, 0:256], w_out[l][:, ct * 256:(ct + 1) * 256].rearrange("(kc p) n -> p kc n", p=128), b_wt)
                        for j in range(2):
                            oc = ct * 2 + j
                            pp, b_pp = bank()
                            for kc in range(16):
                                S.op("pe", lambda e, pp=pp, wt=wt, kc=kc, j=j: e.matmul(pp[:], wt[:, kc, j * 128:(j + 1) * 128], mTb[:, kc, :], start=(kc == 0), stop=(kc == 15)),
                                     reads=[b_wt, b_mTb], writes=[b_pp])
                            S.op("dve", lambda e, pp=pp, oc=oc, l=l, g=g: e.scalar_tensor_tensor(out=xTb[:, oc, :], in0=pp[:], scalar=modT[:, l, 32 + oc, g:g + 1], in1=xTb[:, oc, :], op0=ALU.mult, op1=ALU.add),
                                 reads=[b_pp, b_modT, b_xTb], writes=[b_xTb])
                    if is_moe:
                        plg, b_plg = bank()

                        def h32cb(c, tmp, b_tmp, plg=plg, b_plg=b_plg):
                            hl_, b_hl = hlo[c % 2]
                            S.op("dve", lambda e, c=c, tmp=tmp: e.tensor_copy(hT[:, c, :], tmp[:]), reads=[b_tmp], writes=[b_hT])
                            S.op("dve", lambda e, c=c, tmp=tmp, hl_=hl_: e.tensor_tensor(out=hl_[:], in0=tmp[:], in1=hT[:, c, :], op=ALU.subtract), reads=[b_tmp, b_hT], writes=[b_hl])
                            S.op("pe", lambda e, c=c: e.matmul(plg[0:NE, :], rwh[:, c, :], hT[:, c, :], start=(c == 0), stop=False), reads=[b_hT, b_rwh], writes=[b_plg])
                            S.op("pe", lambda e, c=c, hl_=hl_: e.matmul(plg[0:NE, :], rwh[:, c, :], hl_[:], start=False, stop=False), reads=[b_hl, b_rwh], writes=[b_plg])
                            S.op("pe", lambda e, c=c: e.matmul(plg[0:NE, :], rwl[:, c, :], hT[:, c, :], start=False, stop=(c == 15)), reads=[b_hT, b_rwl], writes=[b_plg])
                        norm_mod((sq, rstd, b_rstd, tmps), xTb, b_xTb, hT, b_hT, l, 1, g, h32cb)
                        S.op("dve", lambda e, plg=plg: e.tensor_copy(lgT[:], plg[0:NE, :]), reads=[b_plg], writes=[b_lgT])
                        for ts in range(4):
                            pq, b_pq = bank()
                            S.op("pe", lambda e, pq=pq, ts=ts: e.transpose(pq[:, 0:NE], lgT[:, ts * 128:(ts + 1) * 128], identf[0:NE, 0:NE]), reads=[b_lgT, b_identf], writes=[b_pq])
                            S.op("dve", lambda e, pq=pq: e.tensor_tensor(out=lg[:], in0=pq[:, 0:NE], in1=rbb[:], op=ALU.add), reads=[b_pq, b_rbb], writes=[b_lg])
                            S.op("dve", lambda e: e.tensor_reduce(out=m12[:, 0:1], in_=lg[:], axis=mybir.AxisListType.X, op=ALU.max), reads=[b_lg], writes=[b_m12])
                            S.op("dve", lambda e: e.tensor_scalar(out=mk1[:], in0=lg[:], scalar1=m12[:, 0:1], scalar2=None, op0=ALU.is_equal), reads=[b_lg, b_m12], writes=[b_mk1])
                            S.op("dve", lambda e: e.scalar_tensor_tensor(out=lg[:], in0=mk1[:], scalar=-1e30, in1=lg[:], op0=ALU.mult, op1=ALU.add), reads=[b_mk1, b_lg], writes=[b_lg])
                            S.op("dve", lambda e: e.tensor_reduce(out=m12[:, 1:2], in_=lg[:], axis=mybir.AxisListType.X, op=ALU.max), reads=[b_lg], writes=[b_m12])
                            S.op("dve", lambda e: e.tensor_scalar(out=mk2[:], in0=lg[:], scalar1=m12[:, 1:2], scalar2=None, op0=ALU.is_equal), reads=[b_lg, b_m12], writes=[b_mk2])
                            S.op("dve", lambda e: e.tensor_tensor(out=m12[:, 2:3], in0=m12[:, 1:2], in1=m12[:, 0:1], op=ALU.subtract), reads=[b_m12], writes=[b_m12])
                            S.op("act", lambda e: e.activation(m12[:, 2:3], m12[:, 2:3], AF.Exp), reads=[b_m12], writes=[b_m12])
                            S.op("dve", lambda e: e.tensor_scalar(out=m12[:, 2:3], in0=m12[:, 2:3], scalar1=1.0, scalar2=None, op0=ALU.add), reads=[b_m12], writes=[b_m12])
                            S.op("dve", lambda e: e.reciprocal(m12[:, 2:3], m12[:, 2:3]), reads=[b_m12], writes=[b_m12])
                            S.op("dve", lambda e: e.tensor_scalar(out=m12[:, 3:4], in0=m12[:, 2:3], scalar1=-1.0, scalar2=1.0, op0=ALU.mult, op1=ALU.add), reads=[b_m12], writes=[b_m12])
                            S.op("dve", lambda e: e.tensor_scalar(out=mk1[:], in0=mk1[:], scalar1=m12[:, 2:3], scalar2=None, op0=ALU.mult), reads=[b_mk1, b_m12], writes=[b_mk1])
                            S.op("dve", lambda e: e.scalar_tensor_tensor(out=cmb[:], in0=mk2[:], scalar=m12[:, 3:4], in1=mk1[:], op0=ALU.mult, op1=ALU.add), reads=[b_mk1, b_mk2, b_m12], writes=[b_cmb])
                            pq2, b_pq2 = bank()
                            S.op("pe", lambda e, pq2=pq2: e.transpose(pq2[0:NE, 0:128], cmb[:], identf[:]), reads=[b_cmb, b_identf], writes=[b_pq2])
                            S.op("dve", lambda e, pq2=pq2, ts=ts: e.tensor_copy(cmbT[:, ts * 128:(ts + 1) * 128], pq2[0:NE, 0:128]), reads=[b_pq2], writes=[b_cmbT])
                        S.op("dve", lambda e: e.tensor_copy(cmbTh[:], cmbT[:]), reads=[b_cmbT], writes=[b_cmbTh])
                        experts = [(moe_w1[jl, ex], moe_w3[jl, ex], moe_w2[jl, ex], ex) for ex in range(NE)]
                    else:
                        norm_mod((sq, rstd, b_rstd, tmps), xTb, b_xTb, hT, b_hT, l, 1, g)
                        experts = [(ffn_w1[jl], ffn_w3[jl], ffn_w2[jl], None)]
                    for (W1, W3, W2, ex) in experts:
                        if ex is not None:
                            cbe, b_cbe = cbes[ex % 2]
                            pq3, b_pq3 = bank()
                            S.op("pe", lambda e, pq3=pq3, ex=ex: e.matmul(pq3[:], selEb[:, ex, :], cmbTh[:], start=True, stop=True), reads=[b_selEb, b_cmbTh], writes=[b_pq3])
                            S.op("act", lambda e, pq3=pq3, cbe=cbe: e.activation(cbe[:], pq3[:], AF.Copy), reads=[b_pq3], writes=[b_cbe])
                        for ct in range(NFF // 4):
                            w1t, b_w1t = next_wA()
                            w3t, b_w3t = next_wA()
                            load_w(w1t[:], W1[:, ct * 512:(ct + 1) * 512].rearrange("(kc p) n -> p kc n", p=128), b_w1t)
                            load_w(w3t[:], W3[:, ct * 512:(ct + 1) * 512].rearrange("(kc p) n -> p kc n", p=128), b_w3t)
                            for j in range(4):
                                fc = ct * 4 + j
                                pu1, b_pu1 = bank()
                                pu3, b_pu3 = bank()
                                for kc in range(16):
                                    S.op("pe", lambda e, pu1=pu1, w1t=w1t, kc=kc, j=j: e.matmul(pu1[:], w1t[:, kc, j * 128:(j + 1) * 128], hT[:, kc, :], start=(kc == 0), stop=(kc == 15)),
                                         reads=[b_w1t, b_hT], writes=[b_pu1])
                                for kc in range(16):
                                    S.op("pe", lambda e, pu3=pu3, w3t=w3t, kc=kc, j=j: e.matmul(pu3[:], w3t[:, kc, j * 128:(j + 1) * 128], hT[:, kc, :], start=(kc == 0), stop=(kc == 15)),
                                         reads=[b_w3t, b_hT], writes=[b_pu3])
                                sl, b_sl = sil[fc % 2]
                                S.op("act", lambda e, sl=sl, pu1=pu1: e.activation(sl[:], pu1[:], AF.Silu), reads=[b_pu1], writes=[b_sl])
                                if ex is None:
                                    S.op("dve", lambda e, sl=sl, pu3=pu3, fc=fc: e.tensor_tensor(out=gT[:, fc, :], in0=pu3[:], in1=sl[:], op=ALU.mult), reads=[b_pu3, b_sl], writes=[b_gT])
                                else:
                                    u3_, b_u3 = u3s[fc % 2]
                                    S.op("dve", lambda e, u3_=u3_, pu3=pu3, cbe=cbe: e.tensor_tensor(out=u3_[:], in0=pu3[:], in1=cbe[:], op=ALU.mult), reads=[b_pu3, b_cbe], writes=[b_u3])
                                    S.op("pool", lambda e, u3_=u3_, sl=sl, fc=fc: e.tensor_tensor(out=gT[:, fc, :], in0=u3_[:], in1=sl[:], op=ALU.mult), reads=[b_u3, b_sl], writes=[b_gT])
                        for pq_ in range(4):
                            accs = [bank() for _ in range(4)]
                            for fc2 in range(NFF // 2):
                                w2_, b_w2 = w2t[w2i[0] % 6]
                                w2i[0] += 1
                                load_w(w2_[:], W2[fc2 * 256:(fc2 + 1) * 256, pq_ * 512:(pq_ + 1) * 512].rearrange("(f p) n -> p f n", p=128), b_w2)
                                for f_ in range(2):
                                    fc = fc2 * 2 + f_
                                    for j in range(4):
                                        pp, b_pp = accs[j]
                                        S.op("pe", lambda e, pp=pp, w2_=w2_, fc=fc, f_=f_, j=j: e.matmul(pp[:], w2_[:, f_, j * 128:(j + 1) * 128], gT[:, fc, :], start=(fc == 0), stop=(fc == NFF - 1)),
                                             reads=[b_w2, b_gT], writes=[b_pp])
                            for j in range(4):
                                oc = pq_ * 4 + j
                                pp, b_pp = accs[j]
                                S.op("dve", lambda e, pp=pp, oc=oc, l=l, g=g: e.scalar_tensor_tensor(out=xTb[:, oc, :], in0=pp[:], scalar=modT[:, l, 80 + oc, g:g + 1], in1=xTb[:, oc, :], op0=ALU.mult, op1=ALU.add),
                                     reads=[b_pp, b_modT, b_xTb], writes=[b_xTb])
                    S.dma("sp", xT[:, :, t0:t0 + 512].rearrange("c p t -> p c t"), xTb[:], b_xTb, reads=[b_xTb], writes=[S.reg("xT", b)])
                S.barrier()
                stage("p45_%d" % l)

        with ExitStack() as ph:
            xTb, b_xTb = sbt(ph, "xTb", (128, 16, 512), F32)
            sq = [sbt(ph, "sq%d" % i, (128, 512), BF16) for i in range(2)]
            rstd, b_rstd = sbt(ph, "rstd", (128, 512), F32)
            yo = [sbt(ph, "yo%d" % i, (128, D), F32) for i in range(2)]
            for b in range(NBLK):
                t0 = b * 512
                S.dma("sp", xTb[:], xT[:, :, t0:t0 + 512].rearrange("c p t -> p c t"), b_xTb, reads=[S.reg("xT", b)], writes=[b_xTb])
                pss, b_pss = bank()
                for c in range(16):
                    sqt, b_sq = sq[c % 2]
                    S.op("act", lambda e, c=c, sqt=sqt: e.activation(sqt[:], xTb[:, c, :], AF.Square), reads=[b_xTb], writes=[b_sq])
                    S.op("pe", lambda e, c=c, sqt=sqt, pss=pss: e.matmul(pss[:], onesb[:], sqt[:], start=(c == 0), stop=(c == 15)), reads=[b_sq, b_onesb], writes=[b_pss])
                S.op("act", lambda e, pss=pss: e.activation(rstd[:], pss[:], AF.Sqrt, scale=1.0 / D, bias=epsb[:]), reads=[b_pss, b_epsb], writes=[b_rstd])
                S.op("dve", lambda e: e.reciprocal(rstd[:], rstd[:]), reads=[b_rstd], writes=[b_rstd])
                for c in range(16):
                    S.op("dve", lambda e, c=c: e.scalar_tensor_tensor(out=xTb[:, c, :], in0=xTb[:, c, :], scalar=fgT[:, c:c + 1], in1=rstd[:], op0=ALU.mult, op1=ALU.mult),
                         reads=[b_xTb, b_fgT, b_rstd], writes=[b_xTb])
                for ts in range(4):
                    yo_, b_yo = yo[ts % 2]
                    for q4 in range(4):
                        pt, b_pt = bank()
                        for j in range(4):
                            c = q4 * 4 + j
                            S.op("pe", lambda e, pt=pt, j=j, c=c, ts=ts: e.transpose(pt[:, j * 128:(j + 1) * 128], xTb[:, c, ts * 128:(ts + 1) * 128], identf[:]),
                                 reads=[b_xTb, b_identf], writes=[b_pt])
                        if q4 % 2:
                            S.op("act", lambda e, pt=pt, q4=q4, yo_=yo_: e.activation(yo_[:, q4 * 512:(q4 + 1) * 512], pt[:], AF.Copy), reads=[b_pt], writes=[b_yo])
                        else:
                            S.op("dve", lambda e, pt=pt, q4=q4, yo_=yo_: e.tensor_copy(yo_[:, q4 * 512:(q4 + 1) * 512], pt[:]), reads=[b_pt], writes=[b_yo])
                    tok = t0 + ts * 128
                    dst = ys_out[tok:tok + 128, :] if tok < 2048 else yp_out[tok - 2048:tok - 2048 + 128, :]
                    S.dma("sp", dst, yo_[:], b_yo, reads=[b_yo])
            S.barrier()
        S.emit()
    return nc, es


_CACHE = {}


def kernel(**inputs):
    n = 8
    if "nc" not in _CACHE:
        _CACHE["nc"] = build()
    nc, _es = _CACHE["nc"]
    f = lambda a: np.ascontiguousarray(np.asarray(a, dtype=np.float32))
    xs = f(inputs["x_sample"])
    xp = f(inputs["x_prompt"])
    st = f(inputs["state_gla"])
    c = f(inputs["c"])
    c_ctx = f(inputs["c_ctx"])
    wnames = ["norm1_g", "norm2_g", "w_mod", "b_mod", "w_in", "sgu_ln_g", "sgu_ln_b", "w_spatial", "b_spatial", "gla_a2", "gla_ab",
              "gla_norm_g", "w_branch_a", "w_branch_b", "w_out", "ffn_w1", "ffn_w3", "ffn_w2", "moe_router", "moe_router_b",
              "moe_w1", "moe_w3", "moe_w2", "final_g"]
    wd = {k: f(inputs[k]) for k in wnames}
    in_maps = []
    for i in range(n):
        m = dict(wd)
        m["xs"] = xs[i]
        m["xp"] = np.ascontiguousarray(xp[4 * i:4 * i + 4].reshape(1024, D))
        m["st"] = st[i]
        m["cc"] = np.ascontiguousarray(np.stack([c[i], c_ctx], axis=0))
        in_maps.append(m)
    res = run_bass_kernel_spmd(nc, in_maps, core_ids=list(range(n)))
    R = res.results
    y_sample = np.stack([R[i]["ys"] for i in range(n)], axis=0)
    y_prompt = np.concatenate([R[i]["yp"].reshape(4, 256, D) for i in range(n)], axis=0)
    new_state = np.concatenate([R[i]["ns"] for i in range(n)], axis=0)
    return (y_prompt.astype(np.float32), y_sample.astype(np.float32), new_state.astype(np.float32))
```
